# Optimizing a Trainium2 kernel written in Bass

```python
import math
import jax, jax.numpy as jnp
from jax import lax
import numpy as np

D_MODEL = 1024
BATCH = 8
SEQ = 2048
DEPTH = 4

MIX_WIDTH = D_MODEL
N_MIXERS = 4
GROUP_WIDTH = MIX_WIDTH // N_MIXERS
ATT_HEADS = 4
ATT_V_DIM = GROUP_WIDTH // ATT_HEADS
ATT_QK_DIM = ATT_V_DIM // 2
Q_BLOCK = 128
NUM_BUCKETS = 32
MAX_DISTANCE = 128
CONF_WIDTH = 31
SCONV_WIDTH = 3
SGU_HEADS = 4
SGU_HEAD_DIM = GROUP_WIDTH // SGU_HEADS
SGU_CHUNK = 128
PEER_HEADS = 8
PEER_NKEYS = 128
PEER_EXPERTS = PEER_NKEYS * PEER_NKEYS
PEER_TOPK = 16
PEER_QDIM = 256
PEER_HALF = PEER_QDIM // 2
PEER_TOK_BLOCK = 128
N_IN_SLICES = 10
MIX_IN = N_IN_SLICES * GROUP_WIDTH
N_MOD = 6
EPS = 1e-6
NEG_INF = -1e30

kernel_name = 'hybrid_diffattn_conv_sgu_peer'


def rms_norm(x, g):
    xf = x.astype(jnp.float32)
    y = xf * lax.rsqrt(jnp.mean(xf * xf, axis=-1, keepdims=True) + EPS)
    return (y * g.astype(jnp.float32)).astype(x.dtype)


def layer_norm(x, g, b):
    xf = x.astype(jnp.float32)
    mu = jnp.mean(xf, axis=-1, keepdims=True)
    var = jnp.mean(jnp.square(xf - mu), axis=-1, keepdims=True)
    y = (xf - mu) * lax.rsqrt(var + EPS)
    return (y * g.astype(jnp.float32) + b.astype(jnp.float32)).astype(x.dtype)


def causal_depthwise_conv(x, w):
    k, ch = w.shape
    return lax.conv_general_dilated(
        x, w[:, None, :].astype(x.dtype), window_strides=(1,), padding=[(k - 1, 0)],
        dimension_numbers=('NWC', 'WIO', 'NWC'), feature_group_count=ch)


def rel_bucket(rel):
    n = jnp.maximum(rel, 0)
    max_exact = NUM_BUCKETS // 2
    ratio = jnp.log(jnp.maximum(n, 1).astype(jnp.float32) / max_exact) / math.log(MAX_DISTANCE / max_exact)
    large = jnp.minimum(max_exact + (ratio * (NUM_BUCKETS - max_exact)).astype(jnp.int32), NUM_BUCKETS - 1)
    return jnp.where(n < max_exact, n, large)


def diff_attention(q, k, v, rel_bias, lam, subln_g, lam_init):
    b, s = q.shape[:2]
    scale = ATT_QK_DIM ** -0.5
    outs = []
    for i in range(s // Q_BLOCK):
        q0 = i * Q_BLOCK
        end = q0 + Q_BLOCK
        logits = jnp.einsum('bqhmd,bkhmd->bhmqk', q[:, q0:end], k[:, :end]).astype(jnp.float32) * scale
        rel = (q0 + jnp.arange(Q_BLOCK))[:, None] - jnp.arange(end)[None, :]
        bias = rel_bias[rel_bucket(rel)].astype(jnp.float32)
        logits = logits + jnp.transpose(bias, (2, 0, 1))[None, :, None]
        logits = jnp.where(rel >= 0, logits, NEG_INF)
        p = jax.nn.softmax(logits, axis=-1)
        w = (p[:, :, 0] - lam * p[:, :, 1]).astype(v.dtype)
        outs.append(jnp.einsum('bhqk,bkhd->bqhd', w, v[:, :end]))
    o = jnp.concatenate(outs, axis=1)
    o = rms_norm(o, subln_g) * (1.0 - lam_init)
    return o.reshape(b, s, ATT_HEADS * ATT_V_DIM)


def spatial_gating(u, v, ln_g, ln_b, w_s, b_s):
    b, s, _ = v.shape
    v = layer_norm(v, ln_g, ln_b).reshape(b, s // SGU_CHUNK, SGU_CHUNK, SGU_HEADS, SGU_HEAD_DIM)
    w = jnp.tril(w_s)
    z = jnp.einsum('hts,bcshd->bcthd', w, v) + jnp.transpose(b_s)[:, :, None]
    return u * z.reshape(b, s, GROUP_WIDTH)


def hybrid_mixer(h, w_in, w_out, lam_par, subln_g, conf_dw, conf_ln_g, conf_ln_b, sconv_w,
                 sgu_ln_g, sgu_ln_b, sgu_w, sgu_b, rel_bias, lam_init):
    b, s, _ = h.shape
    q, k, v, ga, gb, cb, cc, ch, su, sv = jnp.split(h @ w_in, N_IN_SLICES, axis=-1)
    lp = lam_par.astype(jnp.float32)
    lam = jnp.exp(jnp.sum(lp[0] * lp[1])) - jnp.exp(jnp.sum(lp[2] * lp[3])) + lam_init
    y_a = diff_attention(q.reshape(b, s, ATT_HEADS, 2, ATT_QK_DIM),
                         k.reshape(b, s, ATT_HEADS, 2, ATT_QK_DIM),
                         v.reshape(b, s, ATT_HEADS, ATT_V_DIM), rel_bias, lam, subln_g, lam_init)
    z = causal_depthwise_conv(ga * jax.nn.sigmoid(gb), conf_dw)
    y_b = jax.nn.silu(layer_norm(z, conf_ln_g, conf_ln_b))
    y_c = cb * causal_depthwise_conv(cc * ch, sconv_w)
    y_d = spatial_gating(jax.nn.gelu(su, approximate=False), jax.nn.gelu(sv, approximate=False),
                         sgu_ln_g, sgu_ln_b, sgu_w, sgu_b)
    return jnp.concatenate([y_a, y_b, y_c, y_d], axis=-1) @ w_out


def peer_ffn(h, wq, keys, u_tab, v_tab):
    b, s, d = h.shape
    q = (h @ wq).reshape(b, s, PEER_HEADS, 2, PEER_HALF)
    sc = jnp.einsum('bshpd,hpnd->bshpn', q, keys).astype(jnp.float32)
    half_s, half_i = lax.top_k(sc, PEER_TOPK)
    cand = (half_s[..., 0, :, None] + half_s[..., 1, None, :]).reshape(b, s, PEER_HEADS, PEER_TOPK * PEER_TOPK)
    top_s, top_c = lax.top_k(cand, PEER_TOPK)
    i1 = jnp.take_along_axis(half_i[..., 0, :], top_c // PEER_TOPK, axis=-1)
    i2 = jnp.take_along_axis(half_i[..., 1, :], top_c % PEER_TOPK, axis=-1)
    expert = i1 * PEER_NKEYS + i2
    gate = jax.nn.softmax(top_s, axis=-1).astype(h.dtype)
    n_blk = (b * s) // PEER_TOK_BLOCK

    def block(args):
        hb, eb, gb = args
        act = jax.nn.gelu(jnp.einsum('td,thkd->thk', hb, u_tab[eb]), approximate=False)
        return jnp.einsum('thk,thkd->td', gb * act, v_tab[eb])

    y = lax.map(block, (h.reshape(n_blk, PEER_TOK_BLOCK, d),
                        expert.reshape(n_blk, PEER_TOK_BLOCK, PEER_HEADS, PEER_TOPK),
                        gate.reshape(n_blk, PEER_TOK_BLOCK, PEER_HEADS, PEER_TOPK)))
    return y.reshape(b, s, d)


def setup_inputs(seed: int = 0) -> dict:
    key = jax.random.key(seed)
    ks = jax.random.split(key, 26)
    f32 = jnp.float32

    def nrm(k, shape, std):
        return jax.random.normal(k, shape, f32) * std

    L, D, G = DEPTH, D_MODEL, GROUP_WIDTH
    return {
        'x': nrm(ks[0], (BATCH, SEQ, D), 1.0),
        'c': nrm(ks[1], (BATCH, D), 1.0),
        'rel_bias': nrm(ks[2], (NUM_BUCKETS, ATT_HEADS), 0.5),
        'w_mod': nrm(ks[3], (L, D, N_MOD * D), 0.5 * D ** -0.5),
        'b_mod': nrm(ks[4], (L, N_MOD * D), 0.01),
        'norm1_g': 1.0 + nrm(ks[5], (L, D), 0.02),
        'norm2_g': 1.0 + nrm(ks[6], (L, D), 0.02),
        'w_in': nrm(ks[7], (L, D, MIX_IN), D ** -0.5),
        'w_out': nrm(ks[8], (L, MIX_WIDTH, D), MIX_WIDTH ** -0.5),
        'diff_lambda': nrm(ks[9], (L, 4, ATT_QK_DIM), 0.1),
        'subln_g': 1.0 + nrm(ks[10], (L, ATT_V_DIM), 0.02),
        'conf_dw': nrm(ks[11], (L, CONF_WIDTH, G), CONF_WIDTH ** -0.5),
        'conf_ln_g': 1.0 + nrm(ks[12], (L, G), 0.02),
        'conf_ln_b': nrm(ks[13], (L, G), 0.01),
        'sconv_w': nrm(ks[14], (L, SCONV_WIDTH, G), SCONV_WIDTH ** -0.5),
        'sgu_ln_g': 1.0 + nrm(ks[15], (L, G), 0.02),
        'sgu_ln_b': nrm(ks[16], (L, G), 0.01),
        'sgu_w': nrm(ks[17], (L, SGU_HEADS, SGU_CHUNK, SGU_CHUNK), SGU_CHUNK ** -0.5),
        'sgu_b': 1.0 + nrm(ks[18], (L, SGU_HEADS, SGU_CHUNK), 0.01),
        'peer_wq': nrm(ks[19], (L, D, PEER_HEADS * PEER_QDIM), D ** -0.5),
        'peer_keys': nrm(ks[20], (L, PEER_HEADS, 2, PEER_NKEYS, PEER_HALF), PEER_HALF ** -0.5),
        'peer_u': nrm(ks[21], (L, PEER_EXPERTS, D), D ** -0.5),
        'peer_v': nrm(ks[22], (L, PEER_EXPERTS, D), PEER_HEADS ** -0.5),
        'final_g': 1.0 + nrm(ks[23], (D,), 0.02),
    }


def reference(x, c, rel_bias, w_mod, b_mod, norm1_g, norm2_g, w_in, w_out, diff_lambda, subln_g,
              conf_dw, conf_ln_g, conf_ln_b, sconv_w, sgu_ln_g, sgu_ln_b, sgu_w, sgu_b,
              peer_wq, peer_keys, peer_u, peer_v, final_g):
    cond = jax.nn.silu(c)
    for l in range(DEPTH):
        lam_init = 0.8 - 0.6 * math.exp(-0.3 * l)
        mod = cond @ w_mod[l] + b_mod[l]
        sh1, sc1, g1, sh2, sc2, g2 = [m[:, None, :] for m in jnp.split(mod, N_MOD, axis=-1)]
        h = rms_norm(x, norm1_g[l]) * (1.0 + sc1) + sh1
        x = x + g1 * hybrid_mixer(h, w_in[l], w_out[l], diff_lambda[l], subln_g[l], conf_dw[l],
                                  conf_ln_g[l], conf_ln_b[l], sconv_w[l], sgu_ln_g[l], sgu_ln_b[l],
                                  sgu_w[l], sgu_b[l], rel_bias, lam_init)
        h = rms_norm(x, norm2_g[l]) * (1.0 + sc2) + sh2
        x = x + g2 * peer_ffn(h, peer_wq[l], peer_keys[l], peer_u[l], peer_v[l])
    return rms_norm(x, final_g)
```

```python
import contextlib
import math
import numpy as np
import concourse.bass as bass
import concourse.mybir as mybir
from concourse.bass_utils import run_bass_kernel_spmd

F32 = mybir.dt.float32
BF16 = mybir.dt.bfloat16
U32 = mybir.dt.uint32
AF = mybir.ActivationFunctionType
ALU = mybir.AluOpType
AX = mybir.AxisListType

D = 1024
EPS = 1e-6
NEG = -1.0e30
NDMASEM = 8


class Sched:
    ENG = ('pe', 'dve', 'act', 'pool', 'sp')

    def __init__(self, nc, st):
        self.nc = nc
        self.e = {'pe': nc.tensor, 'dve': nc.vector, 'act': nc.scalar, 'pool': nc.gpsimd, 'sp': nc.sync}
        self.sem = {}
        for e in ('pe', 'dve', 'act', 'pool'):
            self.sem[e] = st.enter_context(nc.semaphore('s_' + e))
        for q in ('sp', 'pool'):
            for j in range(NDMASEM):
                self.sem[('dma', q, j)] = st.enter_context(nc.semaphore('d_%s%d' % (q, j)))
        self.cnt = {e: 0 for e in self.ENG}
        self.dcnt = {'sp': 0, 'pool': 0}
        self.waited = {e: {} for e in self.ENG}
        self.last_w = {}
        self.readers = {}
        self.nops = 0
        self.psi = 0

    def _deps(self, reads, writes):
        deps = set()
        for r in reads:
            lw = self.last_w.get(r)
            if lw is not None:
                deps.add(lw)
        for w in writes:
            lw = self.last_w.get(w)
            if lw is not None:
                deps.add(lw)
            rd = self.readers.get(w)
            if rd:
                deps.update(rd.values())
        return deps

    def _emit_waits(self, eng, deps):
        w = self.waited[eng]
        eo = self.e[eng]
        for (k, v) in deps:
            if k == 'pe' and eng == 'pe':
                continue
            if w.get(k, 0) >= v:
                continue
            w[k] = v
            eo.wait_ge(self.sem[k], v)

    def _record(self, tok, reads, writes):
        k = tok[0]
        for r in reads:
            self.readers.setdefault(r, {})[k] = tok
        for wr in writes:
            self.last_w[wr] = tok
            self.readers[wr] = {}

    def op(self, eng, fn, reads=(), writes=()):
        deps = self._deps(reads, writes)
        self._emit_waits(eng, deps)
        self.cnt[eng] += 1
        tok = (eng, self.cnt[eng])
        fn(self.e[eng]).then_inc(self.sem[eng], 1)
        self._record(tok, reads, writes)
        self.nops += 1
        return tok

    def dma(self, out, in_, reads=(), writes=(), q='sp', **kw):
        deps = self._deps(reads, writes)
        i = self.dcnt[q]
        self.dcnt[q] += 1
        key = ('dma', q, i % NDMASEM)
        val = 16 * (i // NDMASEM + 1)
        if i >= NDMASEM:
            deps.add((key, val - 16))
        self._emit_waits(q, deps)
        self.e[q].dma_start(out=out, in_=in_, **kw).then_inc(self.sem[key], 16)
        tok = (key, val)
        self._record(tok, reads, writes)
        self.nops += 1
        return tok

    def barrier(self):
        toks = set(self.last_w.values())
        for rd in self.readers.values():
            toks.update(rd.values())
        mx = {}
        for (k, v) in toks:
            mx[k] = max(mx.get(k, 0), v)
        for eng in self.ENG:
            self._emit_waits(eng, set(mx.items()))
        self.last_w = {}
        self.readers = {}

    def wait_all(self, eng, toks):
        self._emit_waits(eng, set(toks))


def build(T, L, with_peer=True, dbg=False):
    nc = bass.Bass("TRN2", target_bir_lowering=False)
    NT = T // 128
    NB = T // 512

    def din(name, shape, dt=F32):
        return nc.dram_tensor(name, list(shape), dt, kind="ExternalInput").ap()

    x_d = din('x', [T, D])
    c_fm = din('c_fm', [128, 8])
    relb = din('rel_bias', [1, 128])
    w_mod = din('w_mod', [L, D, 6 * D])
    b_mod = din('b_mod_fm', [L, 128, 48])
    n1g = din('n1g_fm', [L, 128, 8])
    n2g = din('n2g_fm', [L, 128, 8])
    fing = din('fing_fm', [128, 8])
    w_in = din('w_in', [L, D, 2560])
    w_out = din('w_out', [L, D, D])
    dlam = din('diff_lambda', [L, 128])
    sublg = din('subln_g', [L, 64])
    cdw = din('conf_dw_fm', [L, 128, 2 * 31])
    clng = din('conf_lng_fm', [L, 128, 2])
    clnb = din('conf_lnb_fm', [L, 128, 2])
    scw = din('sconv_fm', [L, 128, 2 * 3])
    slng = din('sgu_ln_g', [L, 256])
    slnb = din('sgu_ln_b', [L, 256])
    sguwT = din('sgu_wT', [L, 128, 4 * 128])
    sgub = din('sgu_b', [L, 512])
    wq = din('peer_wq', [L, D, 2048])
    keysT = din('peer_keysT', [L, 128, 16 * 128])
    uT = din('peer_uT', [L, 128, 128, 1024])
    vr = din('peer_vr', [L, 128, 128, 1024])
    ident_d = din('ident', [128, 128])
    trilT_d = din('trilT', [128, 128])
    bk_d = din('bk', [128, 256])
    mk_d = din('mk', [128, 256])
    maskq_d = din('maskq', [128, 2])
    iota_d = din('iota', [128, 128])
    out_d = nc.dram_tensor('out', [T, D], F32, kind="ExternalOutput").ap()
    dbg_d = nc.dram_tensor('dbg', [T, D], F32, kind="ExternalOutput").ap() if dbg else None

    wind = nc.dram_tensor('wind', [L, 128, 8 * 2560], BF16, kind="Internal").ap()
    woutd = nc.dram_tensor('woutd', [L, 128, 8 * 1024], BF16, kind="Internal").ap()
    h2d = nc.dram_tensor('h2d', [128, 8 * T], BF16, kind="Internal").ap()
    Gd = nc.dram_tensor('Gd', [NT, 128, 128 * 128], BF16, kind="Internal").ap()

    with contextlib.ExitStack() as st:
        S = Sched(nc, st)

        sbn = [0]

        def sb(name, shape, dt=F32, stack=None):
            sbn[0] += 1
            return (stack or st).enter_context(nc.sbuf_tensor('sb%d_%s' % (sbn[0], name), list(shape), dt))

        xT = sb('xT', [128, 8, T])
        ident_f = sb('ident_f', [128, 128])
        ident_b = sb('ident_b', [128, 128], BF16)
        ones_f = sb('ones_f', [128, 128])
        ones_b = sb('ones_b', [128, 128], BF16)
        eps_t = sb('eps_t', [128, 1])
        modv = sb('modv', [128, L, 48])
        gs1 = sb('gs1', [128, L, 8])
        gs2 = sb('gs2', [128, L, 8])
        TB = sb('TB', [128, 4, 256])
        rb_bc = sb('rb_bc', [128, 128])
        trilT = sb('trilT', [128, 128])
        iota_f = sb('iota_f', [128, 128])
        fing_t = sb('fing_t', [128, 8])
        maskq = sb('maskq', [128, 2])
        cond = sb('cond', [128, 8])
        bmt = sb('bmt', [128, L, 48])
        ngt = sb('ngt', [128, 2, L, 8])
        iota_rep = sb('iota_rep', [128, 128, 8], BF16)
        ps = [st.enter_context(nc.psum_tensor('ps%d' % i, [128, 512], F32)) for i in range(8)]

        def xk(k, b):
            return ('xT', k, b)

        def psn(pool=(0, 1, 2, 3, 4, 5)):
            S.psi += 1
            return pool[S.psi % len(pool)]

        def pk(i):
            return ('ps', i)

        with contextlib.ExitStack() as ph:
            S.dma(ident_f[:], ident_d, writes=['ident_f'])
            S.dma(trilT[:], trilT_d, writes=['trilT'])
            S.dma(iota_f[:], iota_d, writes=['iota_f'])
            S.dma(fing_t[:], fing, writes=['fing'])
            S.dma(maskq[:], maskq_d, writes=['maskq'])
            S.dma(rb_bc[:].unsqueeze(1), relb[0:1, :].partition_broadcast(128), writes=['rb_bc'])
            S.op('dve', lambda e: e.tensor_copy(out=ident_b[:], in_=ident_f[:]), reads=['ident_f'], writes=['ident_b'])
            S.op('pool', lambda e: e.memset(ones_f[:], 1.0), writes=['ones_f'])
            S.op('dve', lambda e: e.tensor_copy(out=iota_rep[:], in_=iota_f[:].unsqueeze(2).to_broadcast([128, 128, 8])), reads=['iota_f'], writes=['iota_rep'])
            S.op('pool', lambda e: e.memset(ones_b[:], 1.0), writes=['ones_b'])
            S.op('pool', lambda e: e.memset(eps_t[:], EPS), writes=['eps'])
            xin = [sb('xin%d' % i, [128, D], stack=ph) for i in range(2)]
            for tt in range(NT):
                xi = xin[tt % 2]
                S.dma(xi[:], x_d[tt * 128:(tt + 1) * 128, :], writes=[('xin', tt % 2)])
                for half in range(2):
                    bnk = psn()
                    for j in range(4):
                        k = half * 4 + j
                        S.op('pe', lambda e, bnk=bnk, j=j, k=k, xi=xi: e.transpose(
                            out=ps[bnk][:, j * 128:(j + 1) * 128], in_=xi[:, k * 128:(k + 1) * 128], identity=ident_f[:]),
                            reads=[('xin', tt % 2), 'ident_f'], writes=[pk(bnk)])
                    eng = 'act' if half == 0 else 'dve'
                    dst = xT[:, half * 4:half * 4 + 4, tt * 128:(tt + 1) * 128]
                    src = ps[bnk][:, :].rearrange("p (j t) -> p j t", j=4)
                    if eng == 'act':
                        S.op('act', lambda e, dst=dst, src=src: e.copy(out=dst, in_=src), reads=[pk(bnk)],
                             writes=[xk(k, tt // 4) for k in range(half * 4, half * 4 + 4)])
                    else:
                        S.op('dve', lambda e, dst=dst, src=src: e.tensor_copy(out=dst, in_=src), reads=[pk(bnk)],
                             writes=[xk(k, tt // 4) for k in range(half * 4, half * 4 + 4)])
            S.dma(cond[:], c_fm, writes=['cond'])
            S.dma(bmt[:], b_mod.rearrange("l p j -> p l j"), writes=['bmt'])
            S.dma(ngt[:, 0], n1g.rearrange("l p j -> p l j"), writes=['ngt0'])
            S.dma(ngt[:, 1], n2g.rearrange("l p j -> p l j"), writes=['ngt1'])
            S.op('act', lambda e: e.activation(out=cond[:], in_=cond[:], func=AF.Silu), reads=['cond'], writes=['cond'])
            wmt = [sb('wmt%d' % i, [128, 8, 512], stack=ph) for i in range(2)]
            wi = 0
            for l in range(1):
                mb = psn()
                for pc in range(12):
                    wt = wmt[wi % 2]
                    wkey = ('wmt', wi % 2)
                    wi += 1
                    S.dma(wt[:], w_mod[l, :, pc * 512:(pc + 1) * 512].rearrange("(k p) n -> p k n", p=128), writes=[wkey])
                    for oc in range(4):
                        col = pc * 4 + oc
                        for k in range(8):
                            S.op('pe', lambda e, wt=wt, oc=oc, k=k, col=col, mb=mb: e.matmul(
                                ps[mb][:, col:col + 1], lhsT=wt[:, k, oc * 128:(oc + 1) * 128], rhs=cond[:, k:k + 1],
                                start=(k == 0), stop=(k == 7)), reads=[wkey, 'cond'], writes=[pk(mb)])
                S.op('dve', lambda e, l=l, mb=mb: e.tensor_tensor(out=modv[:, l, :], in0=ps[mb][:, 0:48], in1=bmt[:, l, :], op=ALU.add),
                     reads=[pk(mb), 'bmt'], writes=[('modv', l)])
                S.op('dve', lambda e, l=l: e.scalar_tensor_tensor(out=gs1[:, l, :], in0=modv[:, l, 8:16], scalar=1.0, in1=ngt[:, 0, l, :],
                                                                  op0=ALU.add, op1=ALU.mult), reads=[('modv', l), 'ngt0'], writes=[('gs1', l)])
                S.op('dve', lambda e, l=l: e.scalar_tensor_tensor(out=gs2[:, l, :], in0=modv[:, l, 32:40], scalar=1.0, in1=ngt[:, 1, l, :],
                                                                  op0=ALU.add, op1=ALU.mult), reads=[('modv', l), 'ngt1'], writes=[('gs2', l)])
            bkt = sb('bkt', [128, 256], stack=ph)
            mkt = sb('mkt', [128, 256], stack=ph)
            tbt = sb('tbt', [128, 256], stack=ph)
            S.dma(bkt[:], bk_d, writes=['bkt'])
            S.dma(mkt[:], mk_d, writes=['mkt'])
            for h in range(4):
                S.op('pool', lambda e, h=h: e.tensor_copy(out=TB[:, h, :], in_=mkt[:]), reads=['mkt'], writes=[('TB', h)])
                for bq in range(32):
                    S.op('dve', lambda e, h=h, bq=bq: e.tensor_scalar(out=tbt[:], in0=bkt[:], scalar1=float(bq), scalar2=rb_bc[:, bq * 4 + h:bq * 4 + h + 1],
                                                                    op0=ALU.is_equal, op1=ALU.mult), reads=['bkt', 'rb_bc'], writes=['tbt'])
                    S.op('dve', lambda e, h=h: e.tensor_tensor(out=TB[:, h, :], in0=TB[:, h, :], in1=tbt[:], op=ALU.add),
                         reads=['tbt', ('TB', h)], writes=[('TB', h)])
            stf = [sb('stf%d' % i, [128, 8, 256], stack=ph) for i in range(2)]
            stb = [sb('stb%d' % i, [128, 8, 256], BF16, stack=ph) for i in range(2)]
            ci = 0
            for l in range(L):
                for (src, dstd, ncol) in ((w_in, wind, 2560), (w_out, woutd, 1024)):
                    for pc in range(ncol // 256):
                        sf, sbf = stf[ci % 2], stb[ci % 2]
                        kf, kb_ = ('stf', ci % 2), ('stb', ci % 2)
                        S.dma(sf[:], src[l, :, pc * 256:(pc + 1) * 256].rearrange("(k p) n -> p k n", p=128), writes=[kf])
                        eng = ('dve', 'pool')[ci % 2]
                        S.op(eng, lambda e, sf=sf, sbf=sbf: e.tensor_copy(out=sbf[:], in_=sf[:]), reads=[kf], writes=[kb_])
                        S.dma(dstd[l].rearrange("p (k n) -> p k n", k=8)[:, :, pc * 256:(pc + 1) * 256], sbf[:], reads=[kb_],
                              writes=[('wd', id(dstd), l, pc)])
                        ci += 1
            S.barrier()

        for l in range(L):
            lam_init = 0.8 - 0.6 * math.exp(-0.3 * l)
            with contextlib.ExitStack() as ph:
                dlt = sb('dlt', [128, 128], stack=ph)
                sgb = sb('sgb', [128, 64], stack=ph)
                cdwt = sb('cdwt', [128, 62], stack=ph)
                clg = sb('clg', [128, 2], stack=ph)
                clb = sb('clb', [128, 2], stack=ph)
                scwt = sb('scwt', [128, 6], stack=ph)
                slg = sb('slg', [128, 256], stack=ph)
                slb = sb('slb', [128, 256], stack=ph)
                wtf = sb('wtf', [128, 512], stack=ph)
                WTm = sb('WTm', [128, 512], BF16, stack=ph)
                sbf_ = sb('sbf_', [1, 512], stack=ph)
                sbb = sb('sbb', [1, 512], BF16, stack=ph)
                lamv = sb('lamv', [128, 8], stack=ph)
                junk = sb('junk', [128, 64], stack=ph)
                S.dma(dlt[:].unsqueeze(1), dlam[l:l + 1, :].partition_broadcast(128), writes=['dlt'])
                S.dma(sgb[:].unsqueeze(1), sublg[l:l + 1, :].partition_broadcast(128), writes=['sgb'])
                S.dma(cdwt[:], cdw[l], writes=['cdwt'])
                S.dma(clg[:], clng[l], writes=['clg'])
                S.dma(clb[:], clnb[l], writes=['clb'])
                S.dma(scwt[:], scw[l], writes=['scwt'])
                S.dma(slg[:].unsqueeze(1), slng[l:l + 1, :].partition_broadcast(128), writes=['slg'])
                S.dma(slb[:].unsqueeze(1), slnb[l:l + 1, :].partition_broadcast(128), writes=['slb'])
                S.dma(wtf[:], sguwT[l], writes=['wtf'])
                S.dma(sbf_[:], sgub[l:l + 1, :], writes=['sbf'])
                S.op('dve', lambda e: e.tensor_tensor(out=WTm[:].rearrange("p (h t) -> p h t", h=4), in0=wtf[:].rearrange("p (h t) -> p h t", h=4),
                                                      in1=trilT[:].unsqueeze(1).to_broadcast([128, 4, 128]), op=ALU.mult),
                     reads=['wtf', 'trilT'], writes=['WTm'])
                S.op('dve', lambda e: e.tensor_copy(out=sbb[:], in_=sbf_[:]), reads=['sbf'], writes=['sbb'])
                S.op('dve', lambda e: e.tensor_scalar(out=sgb[:], in0=sgb[:], scalar1=float(1.0 - lam_init), scalar2=None, op0=ALU.mult),
                     reads=['sgb'], writes=['sgb'])
                S.op('dve', lambda e: e.tensor_tensor(out=junk[:, 0:32], in0=dlt[:, 0:32], in1=dlt[:, 32:64], op=ALU.mult), reads=['dlt'], writes=['junk'])
                S.op('dve', lambda e: e.tensor_reduce(out=lamv[:, 0:1], in_=junk[:, 0:32], axis=AX.X, op=ALU.add), reads=['junk'], writes=['lam0'])
                S.op('dve', lambda e: e.tensor_tensor(out=junk[:, 32:64], in0=dlt[:, 64:96], in1=dlt[:, 96:128], op=ALU.mult), reads=['dlt'], writes=['junk'])
                S.op('dve', lambda e: e.tensor_reduce(out=lamv[:, 1:2], in_=junk[:, 32:64], axis=AX.X, op=ALU.add), reads=['junk'], writes=['lam1'])
                S.op('act', lambda e: e.activation(out=lamv[:, 2:4], in_=lamv[:, 0:2], func=AF.Exp), reads=['lam0', 'lam1'], writes=['lam2'])
                S.op('dve', lambda e: e.tensor_tensor(out=lamv[:, 4:5], in0=lamv[:, 2:3], in1=lamv[:, 3:4], op=ALU.subtract), reads=['lam2'], writes=['lam4'])
                S.op('dve', lambda e: e.tensor_scalar(out=lamv[:, 5:6], in0=lamv[:, 4:5], scalar1=float(lam_init), scalar2=-1.0, op0=ALU.add, op1=ALU.mult),
                     reads=['lam4'], writes=['neglam'])

                hT = sb('hT', [128, 8, 512], BF16, stack=ph)
                yT = sb('yT', [128, 8, 512], BF16, stack=ph)
                kT = sb('kT', [128, 2, T], BF16, stack=ph)
                qT = sb('qT', [128, 2, 2, 512], BF16, stack=ph)
                vaug = sb('vaug', [128, NT, 4, 66], BF16, stack=ph)
                wsl = [sb('wsl%d' % i, [128, 8, 256], BF16, stack=ph) for i in range(3)]
                wol = [sb('wol%d' % i, [128, 8, 128], BF16, stack=ph) for i in range(2)]
                wkf = [sb('wkf%d' % i, [128, 512], stack=ph) for i in range(6)]
                GL = sb('GL', [128, 2, 30 + 512], BF16, stack=ph)
                dgt = sb('dgt', [128, 62, 128], BF16, stack=ph)
                zc = sb('zc', [128, 2, 512], stack=ph)
                MM = sb('MM', [128, 2, 2 + 512], stack=ph)
                uTt = sb('uTt', [128, 2, 512], stack=ph)
                Ebuf = [sb('E%d' % i, [128, 16, 128], BF16, stack=ph) for i in range(2)]
                tmpn = [sb('tmpn%d' % i, [128, 256], stack=ph) for i in range(2)]
                yat = sb('yat', [128, 256], BF16, stack=ph)
                att = sb('att', [128, 4, 80], stack=ph)
                gv = [sb('gv%d' % i, [128, 256], stack=ph) for i in range(4)]
                vn = [sb('vn%d' % i, [128, 256], BF16, stack=ph) for i in range(2)]
                bst = sb('bst', [128, 64], stack=ph)
                rst = sb('rst', [128, 512], stack=ph)
                rkey = 'rst'
                S.op('pool', lambda e: e.memset(vaug[:], 1.0), writes=['vaug_all'])
                for idx_ in range(62):
                    S.op('dve' if idx_ % 2 == 0 else 'pool', lambda e, idx_=idx_: e.tensor_scalar(out=dgt[:, idx_, :], in0=ident_f[:], scalar1=cdwt[:, idx_:idx_ + 1], scalar2=None, op0=ALU.mult),
                         reads=['cdwt', 'ident_f'], writes=['dgt'])
                S.op('pool', lambda e: e.memset(GL[:, :, 0:30], 0.0), writes=['GLh0', 'GLh1'])
                S.op('pool', lambda e: e.memset(MM[:, :, 0:2], 0.0), writes=['MMh0', 'MMh1'])
                wsi = [0]
                wki = [0]

                def wk():
                    wki[0] += 1
                    i = wki[0] % 6
                    return wkf[i], ('wkf', i)

                def load_slice(s):
                    i = wsi[0] % 3
                    wsi[0] += 1
                    S.dma(wsl[i][:], wind[l].rearrange("p (k n) -> p k n", k=8)[:, :, s * 256:(s + 1) * 256], writes=[('wsl', i)],
                          reads=[('wd', id(wind), l, s)])
                    return wsl[i], ('wsl', i)

                def fm_chunk(w, wkey, c, bnk):
                    for k in range(8):
                        S.op('pe', lambda e, k=k: e.matmul(ps[bnk][:, :], lhsT=w[:, k, c * 128:(c + 1) * 128], rhs=hT[:, k, :],
                                                           start=(k == 0), stop=(k == 7)), reads=[wkey, 'hT'], writes=[pk(bnk)])

                def tm_tile(w, wkey, tt, bnk):
                    for k in range(8):
                        S.op('pe', lambda e, k=k: e.matmul(ps[bnk][:, 0:256], lhsT=hT[:, k, tt * 128:(tt + 1) * 128], rhs=w[:, k, :],
                                                           start=(k == 0), stop=(k == 7)), reads=[wkey, 'hT'], writes=[pk(bnk)])

                for b in range(NB):
                    bs = slice(b * 512, (b + 1) * 512)
                    sbank = psn()
                    for k in range(8):
                        w_, wkey_ = wk()
                        S.op('act', lambda e, k=k, w_=w_: e.activation(out=w_[:], in_=xT[:, k, bs], func=AF.Square), reads=[xk(k, b)], writes=[wkey_])
                        S.op('pe', lambda e, k=k, w_=w_: e.matmul(ps[sbank][:, :], lhsT=ones_f[:], rhs=w_[:], start=(k == 0), stop=(k == 7)),
                             reads=[wkey_, 'ones_f'], writes=[pk(sbank)])
                    S.op('act', lambda e: e.activation(out=rst[:], in_=ps[sbank][:, :], func=AF.Ln, bias=eps_t[:], scale=1.0 / D),
                         reads=[pk(sbank), 'eps'], writes=[rkey])
                    S.op('act', lambda e: e.activation(out=rst[:], in_=rst[:], func=AF.Exp, scale=-0.5), reads=[rkey], writes=[rkey])
                    for k in range(8):
                        w_, wkey_ = wk()
                        S.op('dve', lambda e, k=k, w_=w_: e.scalar_tensor_tensor(out=w_[:], in0=xT[:, k, bs], scalar=gs1[:, l, k:k + 1], in1=rst[:],
                                                                                 op0=ALU.mult, op1=ALU.mult), reads=[xk(k, b), rkey, ('gs1', l)], writes=[wkey_])
                        S.op('act', lambda e, k=k, w_=w_: e.activation(out=hT[:, k, :], in_=w_[:], func=AF.Identity, bias=modv[:, l, k:k + 1], scale=1.0),
                             reads=[wkey_, ('modv', l)], writes=['hT'])
                    w, wkey = load_slice(1)
                    for c in range(2):
                        bnk = psn()
                        fm_chunk(w, wkey, c, bnk)
                        S.op('act', lambda e, c=c, bnk=bnk: e.copy(out=kT[:, c, bs], in_=ps[bnk][:, :]), reads=[pk(bnk)], writes=[('kT', c, b)])
                    w, wkey = load_slice(2)
                    for i in range(4):
                        gi = b * 4 + i
                        bnk = psn()
                        tm_tile(w, wkey, i, bnk)
                        S.op('dve', lambda e, gi=gi, bnk=bnk: e.tensor_copy(out=vaug[:, gi, :, 0:64], in_=ps[bnk][:, 0:256].rearrange("p (h d) -> p h d", h=4)),
                             reads=[pk(bnk), 'vaug_all'], writes=[('vaug', gi)])
                    w, wkey = load_slice(0)
                    for c in range(2):
                        bnk = psn()
                        fm_chunk(w, wkey, c, bnk)
                        for m in range(2):
                            S.op('act', lambda e, c=c, bnk=bnk, m=m: e.activation(out=qT[:, m, c, :], in_=ps[bnk][:, :], func=AF.Copy, scale=maskq[:, m:m + 1]),
                                 reads=[pk(bnk), 'maskq'], writes=[('qT', c)])
                    PA, PR = (0, 1, 2, 6), (3, 4, 5)

                    def gen_attn():
                        for i in range(4):
                            gi = b * 4 + i
                            for h in range(4):
                                c = h // 2
                                acc = 7
                                ao = ((gi * 4 + h) % 2) * 256
                                for m in range(2):
                                    E = Ebuf[m]
                                    ekey = ('E', m)
                                    pb = (h % 2) * 64
                                    bnk = psn(PA)
                                    nears = [gi] if gi == 0 else [gi - 1, gi]
                                    for kb in nears:
                                        slot = kb - (gi - 1)
                                        S.op('pe', lambda e, kb=kb, slot=slot, bnk=bnk, pb=pb: e.matmul(
                                            ps[bnk][:, slot * 128:(slot + 1) * 128], lhsT=kT[pb:pb + 64, c, kb * 128:(kb + 1) * 128],
                                            rhs=qT[pb:pb + 64, m, c, i * 128:(i + 1) * 128], start=True, stop=True),
                                            reads=[('kT', c, kb // 4), ('qT', c)], writes=[pk(bnk)])
                                    s0 = 1 if gi == 0 else 0
                                    tn = tmpn[m]
                                    S.op('dve', lambda e, bnk=bnk, s0=s0, tn=tn: e.tensor_tensor(out=tn[:, s0 * 128:256], in0=ps[bnk][:, s0 * 128:256],
                                                                                                 in1=TB[:, h, s0 * 128:256], op=ALU.add),
                                         reads=[pk(bnk), ('TB', h)], writes=[('tmpn', m)])
                                    S.op('act', lambda e, s0=s0, tn=tn, E=E: e.activation(
                                        out=E[:, gi - 1 + s0:gi + 1, :], in_=tn[:, s0 * 128:256].rearrange("p (s q) -> p s q", q=128), func=AF.Exp),
                                        reads=[('tmpn', m)], writes=[ekey])
                                    for g0 in range(0, gi - 1, 4):
                                        g1 = min(g0 + 4, gi - 1)
                                        bnk = psn(PA)
                                        for kb in range(g0, g1):
                                            S.op('pe', lambda e, kb=kb, bnk=bnk, pb=pb: e.matmul(
                                                ps[bnk][:, (kb - g0) * 128:(kb - g0 + 1) * 128], lhsT=kT[pb:pb + 64, c, kb * 128:(kb + 1) * 128],
                                                rhs=qT[pb:pb + 64, m, c, i * 128:(i + 1) * 128], start=True, stop=True),
                                                reads=[('kT', c, kb // 4), ('qT', c)], writes=[pk(bnk)])
                                        S.op('act', lambda e, g0=g0, g1=g1, bnk=bnk, E=E: e.activation(
                                            out=E[:, g0:g1, :], in_=ps[bnk][:, 0:(g1 - g0) * 128].rearrange("p (s q) -> p s q", q=128), func=AF.Exp,
                                            bias=rb_bc[:, 124 + h:125 + h], scale=1.0), reads=[pk(bnk), 'rb_bc'], writes=[ekey])
                                    for kb in range(gi + 1):
                                        S.op('pe', lambda e, kb=kb, E=E: e.matmul(ps[acc][:, ao + m * 128:ao + m * 128 + 65], lhsT=E[:, kb, :], rhs=vaug[:, kb, h, 0:65],
                                                                               start=(kb == 0), stop=(kb == gi)),
                                             reads=[ekey, ('vaug', kb)], writes=[('acc', ao)])
                                    yield
                                a = att[:, h, :]
                                akey = ('att', h)
                                S.op('dve', lambda e, a=a, acc=acc: e.reciprocal(out=a[:, 0:2], in_=ps[acc][:, ao:ao + 256].rearrange("p (m x) -> p m x", m=2)[:, :, 64]),
                                     reads=[('acc', ao)], writes=[akey])
                                S.op('dve', lambda e, a=a: e.tensor_tensor(out=a[:, 2:3], in0=a[:, 1:2], in1=lamv[:, 5:6], op=ALU.mult),
                                     reads=[akey, 'neglam'], writes=[akey])
                                S.op('dve', lambda e, a=a, acc=acc: e.tensor_scalar(out=a[:, 8:72], in0=ps[acc][:, ao:ao + 64], scalar1=a[:, 0:1], scalar2=None, op0=ALU.mult),
                                     reads=[('acc', ao), akey], writes=[akey])
                                S.op('dve', lambda e, a=a, acc=acc: e.scalar_tensor_tensor(out=a[:, 8:72], in0=ps[acc][:, ao + 128:ao + 192], scalar=a[:, 2:3], in1=a[:, 8:72],
                                                                                           op0=ALU.mult, op1=ALU.add), reads=[('acc', ao), akey], writes=[akey])
                                S.op('act', lambda e, a=a: e.activation(out=junk[:, 0:64], in_=a[:, 8:72], func=AF.Square, accum_out=a[:, 3:4]),
                                     reads=[akey], writes=[akey, 'junk'])
                                S.op('act', lambda e, a=a: e.activation(out=a[:, 4:5], in_=a[:, 3:4], func=AF.Ln, bias=eps_t[:], scale=1.0 / 64),
                                     reads=[akey, 'eps'], writes=[akey])
                                S.op('act', lambda e, a=a: e.activation(out=a[:, 4:5], in_=a[:, 4:5], func=AF.Exp, scale=-0.5), reads=[akey], writes=[akey])
                                S.op('dve', lambda e, a=a, h=h: e.scalar_tensor_tensor(out=yat[:, h * 64:(h + 1) * 64], in0=a[:, 8:72], scalar=a[:, 4:5], in1=sgb[:],
                                                                                       op0=ALU.mult, op1=ALU.mult), reads=[akey, 'sgb'], writes=[('yat', h)])
                                yield
                            bnk = psn(PA)
                            pbf = ps[bnk][:, 0:128].bitcast(BF16)
                            for c in range(2):
                                S.op('pe', lambda e, c=c, pbf=pbf: e.transpose(out=pbf[:, c * 128:(c + 1) * 128], in_=yat[:, c * 128:(c + 1) * 128], identity=ident_b[:]),
                                     reads=[('yat', 2 * c), ('yat', 2 * c + 1), 'ident_b'], writes=[pk(bnk)])
                            S.op('act', lambda e, pbf=pbf, i=i: e.copy(out=yT[:, 0:2, i * 128:(i + 1) * 128], in_=pbf.rearrange("p (c t) -> p c t", c=2)),
                                 reads=[pk(bnk)], writes=[('yT', 0, i), ('yT', 1, i)])

                    def gen_rest():
                        wgb, kgb = load_slice(4)
                        wga, kga = load_slice(3)
                        for c in range(2):
                            bnk = psn(PR)
                            yield
                            fm_chunk(wgb, kgb, c, bnk)
                            sg, sgk = wk()
                            S.op('act', lambda e, bnk=bnk, sg=sg: e.activation(out=sg[:], in_=ps[bnk][:, :], func=AF.Sigmoid), reads=[pk(bnk)], writes=[sgk])
                            bnk2 = psn(PR)
                            yield
                            fm_chunk(wga, kga, c, bnk2)
                            S.op('dve', lambda e, bnk2=bnk2, sg=sg, c=c: e.tensor_tensor(out=GL[:, c, 30:542], in0=ps[bnk2][:, :], in1=sg[:], op=ALU.mult),
                                 reads=[pk(bnk2), sgk], writes=[('GL', c)])
                            cbk = psn(PR)
                            for j in range(31):
                                if j % 8 == 7:
                                    yield
                                S.op('pe', lambda e, c=c, j=j, cbk=cbk: e.matmul(ps[cbk][:, :], lhsT=dgt[:, c * 31 + j, :], rhs=GL[:, c, j:j + 512],
                                                                               start=(j == 0), stop=(j == 30)), reads=[('GL', c), 'GLh%d' % c, 'dgt'], writes=[pk(cbk)])
                            S.op('act', lambda e, c=c, cbk=cbk: e.copy(out=zc[:, c, :], in_=ps[cbk][:, :]), reads=[pk(cbk)], writes=[('zc', c)])
                            S.op('pool', lambda e, c=c: e.tensor_copy(out=GL[:, c, 0:30], in_=GL[:, c, 512:542]), reads=[('GL', c)], writes=['GLh%d' % c])
                        yield
                        mub, msb = psn(PR), psn(PR)
                        for c in range(2):
                            S.op('pe', lambda e, c=c: e.matmul(ps[mub][:, :], lhsT=ones_f[:], rhs=zc[:, c, :], start=(c == 0), stop=(c == 1)),
                                 reads=[('zc', c), 'ones_f'], writes=[pk(mub)])
                        for c in range(2):
                            w_, wkey_ = wk()
                            S.op('act', lambda e, c=c, w_=w_: e.activation(out=w_[:], in_=zc[:, c, :], func=AF.Square), reads=[('zc', c)], writes=[wkey_])
                            S.op('pe', lambda e, c=c, w_=w_: e.matmul(ps[msb][:, :], lhsT=ones_f[:], rhs=w_[:], start=(c == 0), stop=(c == 1)),
                                 reads=[wkey_, 'ones_f'], writes=[pk(msb)])
                        mu, muk = wk()
                        var, vark = wk()
                        S.op('act', lambda e: e.activation(out=mu[:], in_=ps[mub][:, :], func=AF.Copy, scale=1.0 / 256), reads=[pk(mub)], writes=[muk])
                        S.op('dve', lambda e: e.tensor_tensor(out=var[:], in0=mu[:], in1=mu[:], op=ALU.mult), reads=[muk], writes=[vark])
                        S.op('dve', lambda e: e.scalar_tensor_tensor(out=var[:], in0=ps[msb][:, :], scalar=1.0 / 256, in1=var[:], op0=ALU.mult, op1=ALU.subtract),
                             reads=[pk(msb), vark], writes=[vark])
                        S.op('dve', lambda e: e.tensor_scalar(out=var[:], in0=var[:], scalar1=0.0, scalar2=None, op0=ALU.max), reads=[vark], writes=[vark])
                        S.op('act', lambda e: e.activation(out=var[:], in_=var[:], func=AF.Ln, bias=eps_t[:], scale=1.0), reads=[vark, 'eps'], writes=[vark])
                        S.op('act', lambda e: e.activation(out=var[:], in_=var[:], func=AF.Exp, scale=-0.5), reads=[vark], writes=[vark])
                        for c in range(2):
                            w_, wkey_ = wk()
                            S.op('dve', lambda e, c=c, w_=w_: e.tensor_tensor(out=w_[:], in0=zc[:, c, :], in1=mu[:], op=ALU.subtract), reads=[('zc', c), muk], writes=[wkey_])
                            S.op('dve', lambda e, w_=w_: e.tensor_tensor(out=w_[:], in0=w_[:], in1=var[:], op=ALU.mult), reads=[wkey_, vark], writes=[wkey_])
                            S.op('act', lambda e, c=c, w_=w_: e.activation(out=yT[:, 2 + c, :], in_=w_[:], func=AF.Silu, bias=clb[:, c:c + 1], scale=clg[:, c:c + 1]),
                                 reads=[wkey_, 'clg', 'clb'], writes=[('yT', 2 + c, i) for i in range(4)])
                        wcc, kcc = load_slice(6)
                        wch, kch = load_slice(7)
                        for c in range(2):
                            bnk = psn(PR)
                            yield
                            fm_chunk(wcc, kcc, c, bnk)
                            w_, wkey_ = wk()
                            S.op('act', lambda e, bnk=bnk, w_=w_: e.copy(out=w_[:], in_=ps[bnk][:, :]), reads=[pk(bnk)], writes=[wkey_])
                            bnk2 = psn(PR)
                            yield
                            fm_chunk(wch, kch, c, bnk2)
                            S.op('dve', lambda e, bnk2=bnk2, w_=w_, c=c: e.tensor_tensor(out=MM[:, c, 2:514], in0=ps[bnk2][:, :], in1=w_[:], op=ALU.mult),
                                 reads=[pk(bnk2), wkey_], writes=[('MM', c)])
                        wcb, kcb = load_slice(5)
                        for c in range(2):
                            cv, cvk = wk()
                            S.op('dve', lambda e, c=c, cv=cv: e.tensor_scalar(out=cv[:], in0=MM[:, c, 0:512], scalar1=scwt[:, c * 3:c * 3 + 1], scalar2=None, op0=ALU.mult),
                                 reads=[('MM', c), 'MMh%d' % c, 'scwt'], writes=[cvk])
                            for j in (1, 2):
                                S.op('dve', lambda e, c=c, j=j, cv=cv: e.scalar_tensor_tensor(out=cv[:], in0=MM[:, c, j:j + 512], scalar=scwt[:, c * 3 + j:c * 3 + j + 1], in1=cv[:],
                                                                                              op0=ALU.mult, op1=ALU.add), reads=[('MM', c), 'MMh%d' % c, cvk], writes=[cvk])
                            S.op('pool', lambda e, c=c: e.tensor_copy(out=MM[:, c, 0:2], in_=MM[:, c, 512:514]), reads=[('MM', c)], writes=['MMh%d' % c])
                            bnk = psn(PR)
                            yield
                            fm_chunk(wcb, kcb, c, bnk)
                            S.op('dve', lambda e, c=c, cv=cv, bnk=bnk: e.tensor_tensor(out=yT[:, 4 + c, :], in0=ps[bnk][:, :], in1=cv[:], op=ALU.mult),
                                 reads=[pk(bnk), cvk], writes=[('yT', 4 + c, i) for i in range(4)])
                        wsu, ksu = load_slice(8)
                        for c in range(2):
                            bnk = psn(PR)
                            yield
                            fm_chunk(wsu, ksu, c, bnk)
                            S.op('act', lambda e, c=c, bnk=bnk: e.activation(out=uTt[:, c, :], in_=ps[bnk][:, :], func=AF.Gelu), reads=[pk(bnk)], writes=[('uTt', c)])
                        wsv, ksv = load_slice(9)
                        zb = [3, 4]
                        for i in range(4):
                            bnk = 5
                            yield
                            tm_tile(wsv, ksv, i, bnk)
                            S.op('act', lambda e, bnk=bnk, i=i: e.activation(out=gv[i][:], in_=ps[bnk][:, 0:256], func=AF.Gelu), reads=[pk(bnk)], writes=[('gv', i)])
                        for i in range(4):
                            yield
                            g_ = gv[i]
                            gk = ('gv', i)
                            v_ = vn[i % 2]
                            vk_ = ('vn', i % 2)
                            bs_ = bst[:, i * 16:(i + 1) * 16]
                            bk_ = ('bst', i)
                            S.op('dve', lambda e, g_=g_, bs_=bs_: e.bn_stats(out=bs_[:, 0:6], in_=g_[:]), reads=[gk], writes=[bk_])
                            S.op('dve', lambda e, bs_=bs_: e.bn_aggr(out=bs_[:, 8:10], in_=bs_[:, 0:6]), reads=[bk_], writes=[bk_])
                            S.op('act', lambda e, bs_=bs_: e.activation(out=bs_[:, 10:11], in_=bs_[:, 9:10], func=AF.Ln, bias=eps_t[:], scale=1.0), reads=[bk_, 'eps'], writes=[bk_])
                            S.op('act', lambda e, bs_=bs_: e.activation(out=bs_[:, 10:11], in_=bs_[:, 10:11], func=AF.Exp, scale=-0.5), reads=[bk_], writes=[bk_])
                            S.op('dve', lambda e, g_=g_, bs_=bs_: e.tensor_scalar(out=g_[:], in0=g_[:], scalar1=bs_[:, 8:9], scalar2=bs_[:, 10:11], op0=ALU.subtract, op1=ALU.mult),
                                 reads=[gk, bk_], writes=[gk])
                            S.op('dve', lambda e, g_=g_: e.tensor_tensor(out=g_[:], in0=g_[:], in1=slg[:], op=ALU.mult), reads=[gk, 'slg'], writes=[gk])
                            S.op('dve', lambda e, g_=g_, v_=v_: e.tensor_tensor(out=v_[:], in0=g_[:], in1=slb[:], op=ALU.add), reads=[gk, 'slb'], writes=[vk_])
                            for h in range(4):
                                c = h // 2
                                po = (h % 2) * 64
                                S.op('pe', lambda e, h=h, c=c, po=po, v_=v_, i=i: e.matmul(ps[zb[c]][po:po + 64, i * 128:(i + 1) * 128], lhsT=v_[:, h * 64:(h + 1) * 64],
                                                                                     rhs=WTm[:, h * 128:(h + 1) * 128], start=True, stop=False),
                                     reads=[vk_, 'WTm'], writes=[pk(zb[c])])
                                S.op('pe', lambda e, h=h, c=c, po=po, i=i: e.matmul(ps[zb[c]][po:po + 64, i * 128:(i + 1) * 128], lhsT=ones_b[0:1, 0:64],
                                                                              rhs=sbb[0:1, h * 128:(h + 1) * 128], start=False, stop=True),
                                     reads=['ones_b', 'sbb'], writes=[pk(zb[c])])
                        for c in range(2):
                            S.op('dve', lambda e, c=c: e.tensor_tensor(out=yT[:, 6 + c, :], in0=ps[zb[c]][:, :], in1=uTt[:, c, :], op=ALU.mult),
                                 reads=[pk(zb[c]), ('uTt', c)], writes=[('yT', 6 + c, i) for i in range(4)])

                    ga_, gr_ = gen_attn(), gen_rest()
                    alive = [True, True]
                    while alive[0] or alive[1]:
                        if alive[0]:
                            try:
                                next(ga_)
                            except StopIteration:
                                alive[0] = False
                        for _ in range(2):
                            if alive[1]:
                                try:
                                    next(gr_)
                                except StopIteration:
                                    alive[1] = False
                    ykeys = [('yT', k, i) for k in range(8) for i in range(4)]
                    for oc in range(8):
                        wo = wol[oc % 2]
                        wok = ('wol', oc % 2)
                        S.dma(wo[:], woutd[l].rearrange("p (k n) -> p k n", k=8)[:, :, oc * 128:(oc + 1) * 128], writes=[wok],
                              reads=[('wd', id(woutd), l, oc // 2)])
                        bnk = psn()
                        for k in range(8):
                            S.op('pe', lambda e, k=k, wo=wo, bnk=bnk: e.matmul(ps[bnk][:, :], lhsT=wo[:, k, :], rhs=yT[:, k, :], start=(k == 0), stop=(k == 7)),
                                 reads=[wok] + ykeys, writes=[pk(bnk)])
                        S.op('dve', lambda e, oc=oc, bnk=bnk: e.scalar_tensor_tensor(out=xT[:, oc, bs], in0=ps[bnk][:, :], scalar=modv[:, l, 16 + oc:17 + oc], in1=xT[:, oc, bs],
                                                                                     op0=ALU.mult, op1=ALU.add), reads=[pk(bnk), xk(oc, b), ('modv', l)], writes=[xk(oc, b)])
                S.barrier()
            if with_peer:
                peer_layer(nc, S, sb, ps, psn, pk, xk, l, T, NT, NB, locals())

        with contextlib.ExitStack() as ph:
            wkf = [sb('fwk%d' % i, [128, 512], stack=ph) for i in range(4)]
            osb = [sb('osb%d' % i, [128, D], stack=ph) for i in range(2)]
            toks = []
            wi = 0
            for b in range(NB):
                bs = slice(b * 512, (b + 1) * 512)
                sbank = psn()
                for k in range(8):
                    w_ = wkf[wi % 2]
                    wkey_ = ('fwk', wi % 2)
                    wi += 1
                    S.op('act', lambda e, k=k, w_=w_: e.activation(out=w_[:], in_=xT[:, k, bs], func=AF.Square), reads=[xk(k, b)], writes=[wkey_])
                    S.op('pe', lambda e, k=k, w_=w_: e.matmul(ps[sbank][:, :], lhsT=ones_f[:], rhs=w_[:], start=(k == 0), stop=(k == 7)),
                         reads=[wkey_, 'ones_f'], writes=[pk(sbank)])
                rst = wkf[2]
                S.op('act', lambda e: e.activation(out=rst[:], in_=ps[sbank][:, :], func=AF.Ln, bias=eps_t[:], scale=1.0 / D),
                     reads=[pk(sbank), 'eps'], writes=['frst'])
                S.op('act', lambda e: e.activation(out=rst[:], in_=rst[:], func=AF.Exp, scale=-0.5), reads=['frst'], writes=['frst'])
                for k in range(8):
                    S.op('dve', lambda e, k=k: e.scalar_tensor_tensor(out=xT[:, k, bs], in0=xT[:, k, bs], scalar=fing_t[:, k:k + 1], in1=rst[:],
                                                                      op0=ALU.mult, op1=ALU.mult), reads=[xk(k, b), 'frst', 'fing'], writes=[xk(k, b)])
                for i in range(4):
                    tt = b * 4 + i
                    ob = osb[tt % 2]
                    okey = ('osb', tt % 2)
                    for half in range(2):
                        bnk = psn()
                        for j in range(4):
                            k = half * 4 + j
                            S.op('pe', lambda e, bnk=bnk, j=j, k=k, tt=tt: e.transpose(out=ps[bnk][:, j * 128:(j + 1) * 128], in_=xT[:, k, tt * 128:(tt + 1) * 128],
                                                                                    identity=ident_f[:]), reads=[xk(k, b), 'ident_f'], writes=[pk(bnk)])
                        if half == 0:
                            S.op('act', lambda e, bnk=bnk, ob=ob: e.copy(out=ob[:, 0:512], in_=ps[bnk][:, :]), reads=[pk(bnk)], writes=[okey + (0,)])
                        else:
                            S.op('dve', lambda e, bnk=bnk, ob=ob: e.tensor_copy(out=ob[:, 512:1024], in_=ps[bnk][:, :]), reads=[pk(bnk)], writes=[okey + (1,)])
                    toks.append(S.dma(out_d[tt * 128:(tt + 1) * 128, :], ob[:], reads=[okey + (0,), okey + (1,)], writes=[('out', tt)]))
            S.wait_all('sp', toks)
    print('instructions emitted:', S.nops)
    return nc


def peer_layer(nc, S, sb, ps, psn, pk, xk, l, T, NT, NB, env):
    xT, modv, gs2, ones_f, eps_t, ident_f, iota_f, iota_rep = (env[k] for k in ('xT', 'modv', 'gs2', 'ones_f', 'eps_t', 'ident_f', 'iota_f', 'iota_rep'))
    cond, bmt, ngt, gs1, w_mod, L = (env[k] for k in ('cond', 'bmt', 'ngt', 'gs1', 'w_mod', 'L'))
    wq, keysT, uT, vr, h2d, Gd = (env[k] for k in ('wq', 'keysT', 'uT', 'vr', 'h2d', 'Gd'))
    ALLB = tuple(range(8))
    h2dv = h2d.rearrange("p (k t) -> p k t", k=8)
    with contextlib.ExitStack() as ph:
        keyt = sb('keyt', [128, 16, 128], stack=ph)
        S.dma(keyt[:], keysT[l].rearrange("d (g n) -> d g n", g=16), writes=['keyt'])
        h2f = sb('h2f', [128, 8, 512], stack=ph)
        rst2 = sb('rst2', [128, 512], stack=ph)
        sqw = [sb('sqw%d' % i, [128, 512], stack=ph) for i in range(2)]
        h2b = [sb('h2b%d' % i, [128, 512], BF16, stack=ph) for i in range(1)]
        qpT = [sb('qpT%d' % i, [128, 16, 256], stack=ph) for i in range(1)]
        wqs = [sb('wqs%d' % i, [128, 8, 128], stack=ph) for i in range(2)]
        sc0s = [sb('sc0_%d' % i, [128, 16, 128], stack=ph) for i in range(2)]
        tv = sb('tv', [128, 16, 16], stack=ph)
        ti = sb('ti', [128, 16, 16], U32, stack=ph)
        tif = sb('tif', [128, 16, 16], stack=ph)
        cand = sb('cand', [128, 8, 256], stack=ph)
        tsv = sb('tsv', [128, 8, 16], stack=ph)
        tcv = sb('tcv', [128, 8, 16], U32, stack=ph)
        tab = sb('tab', [128, 2, 128], U32, stack=ph)
        tabf = sb('tabf', [128, 2, 128], stack=ph)
        i12g = sb('i12g', [128, 3, 128], stack=ph)
        zs = sb('zs', [128, 8], stack=ph)
        sT = sb('sT', [128, 3, 128], stack=ph)
        PT = [sb('PT%d' % i, [128, 128, 8], BF16, stack=ph) for i in range(3)]
        QT = [sb('QT%d' % i, [128, 128, 8], BF16, stack=ph) for i in range(3)]
        sTb = sb('sTb', [128, 2, 128], BF16, stack=ph)
        ohi = [0]
        GT = sb('GT', [128, 128, 128], BF16, stack=ph)
        tvv = tv[:].rearrange("p (h w) k -> p h w k", w=2)
        tifv = tif[:].rearrange("p (h w) k -> p h w k", w=2)
        cand4 = cand[:].rearrange("p h (a b) -> p h a b", a=16)
        wqc = [0]

        def emit_n2(b):
            bs = slice(b * 512, (b + 1) * 512)
            sbank = psn(ALLB)
            for k in range(8):
                w_ = sqw[k % 2]
                wkey_ = ('sqw', k % 2)
                S.op('act', lambda e, k=k, w_=w_: e.activation(out=w_[:], in_=xT[:, k, bs], func=AF.Square), reads=[xk(k, b)], writes=[wkey_])
                S.op('pe', lambda e, k=k, w_=w_: e.matmul(ps[sbank][:, :], lhsT=ones_f[:], rhs=w_[:], start=(k == 0), stop=(k == 7)),
                     reads=[wkey_, 'ones_f'], writes=[pk(sbank)])
            S.op('act', lambda e: e.activation(out=rst2[:], in_=ps[sbank][:, :], func=AF.Ln, bias=eps_t[:], scale=1.0 / D),
                 reads=[pk(sbank), 'eps'], writes=['rst2'])
            S.op('act', lambda e: e.activation(out=rst2[:], in_=rst2[:], func=AF.Exp, scale=-0.5), reads=['rst2'], writes=['rst2'])
            for k in range(8):
                w_ = sqw[k % 2]
                wkey_ = ('sqw', k % 2)
                hb_ = h2b[0]
                hkey = ('h2b', 0)
                S.op('dve', lambda e, k=k, w_=w_: e.scalar_tensor_tensor(out=w_[:], in0=xT[:, k, bs], scalar=gs2[:, l, k:k + 1], in1=rst2[:],
                                                                         op0=ALU.mult, op1=ALU.mult), reads=[xk(k, b), 'rst2', ('gs2', l)], writes=[wkey_])
                S.op('act', lambda e, k=k, w_=w_: e.activation(out=h2f[:, k, :], in_=w_[:], func=AF.Identity, bias=modv[:, l, 24 + k:25 + k], scale=1.0),
                     reads=[wkey_, ('modv', l)], writes=[('h2f', k)])
                S.op('pool', lambda e, k=k, hb_=hb_: e.tensor_copy(out=hb_[:], in_=h2f[:, k, :]), reads=[('h2f', k)], writes=[hkey])
                S.dma(h2dv[:, k, bs], hb_[:], reads=[hkey], writes=[('h2d', k, b)])

        def emit_qproj(b, half, qi, glist=None):
            hs = slice(half * 256, (half + 1) * 256)
            for g in (glist if glist is not None else range(16)):
                wt = wqs[wqc[0] % 2]
                wkey = ('wqs', wqc[0] % 2)
                wqc[0] += 1
                S.dma(wt[:], wq[l, :, g * 128:(g + 1) * 128].rearrange("(k p) n -> p k n", p=128), writes=[wkey])
                bnk = psn(ALLB)
                for k in range(8):
                    S.op('pe', lambda e, k=k, wt=wt, bnk=bnk: e.matmul(ps[bnk][:, 0:256], lhsT=wt[:, k, :], rhs=h2f[:, k, hs], start=(k == 0), stop=(k == 7)),
                         reads=[wkey, ('h2f', k)], writes=[pk(bnk)])
                S.op('act', lambda e, g=g, bnk=bnk: e.copy(out=qpT[qi][:, g, :], in_=ps[bnk][:, 0:256]), reads=[pk(bnk)], writes=[('qpT', qi, g)])

        def emit_scores(tl, qi):
            tsl = slice(tl * 128, (tl + 1) * 128)
            for gq in range(4):
                bnk = psn(ALLB)
                for gg in range(4):
                    g = gq * 4 + gg
                    S.op('pe', lambda e, g=g, gg=gg, bnk=bnk: e.matmul(ps[bnk][:, gg * 128:(gg + 1) * 128], lhsT=qpT[qi][:, g, tsl], rhs=keyt[:, g, :],
                                                                       start=True, stop=True), reads=[('qpT', qi, g), 'keyt'], writes=[pk(bnk)])
                S.op('act', lambda e, gq=gq, bnk=bnk: e.copy(out=sc0s[tl][:, gq * 4:(gq + 1) * 4, :], in_=ps[bnk][:, :].rearrange("p (g n) -> p g n", g=4)),
                     reads=[pk(bnk)], writes=[('sc0', tl, gq * 4 + j) for j in range(4)])

        def part1a(tl):
            yield
            for g in range(16):
                S.op('dve', lambda e, g=g: e.max(out=tv[:, g, 0:8], in_=sc0s[tl][:, g, :]), reads=[('sc0', tl, g)], writes=[('tv', g)])
            yield
            for g in range(16):
                S.op('dve', lambda e, g=g: e.max_index(out=ti[:, g, 0:8], in_max=tv[:, g, 0:8], in_values=sc0s[tl][:, g, :]),
                     reads=[('sc0', tl, g), ('tv', g)], writes=[('ti', g)])
            yield
            for g in range(16):
                S.op('dve', lambda e, g=g: e.match_replace(out=sc0s[tl][:, g, :], in_to_replace=tv[:, g, 0:8], in_values=sc0s[tl][:, g, :], imm_value=NEG),
                     reads=[('sc0', tl, g), ('tv', g)], writes=[('sc0', tl, g)])
            yield
            for g in range(16):
                S.op('dve', lambda e, g=g: e.max(out=tv[:, g, 8:16], in_=sc0s[tl][:, g, :]), reads=[('sc0', tl, g)], writes=[('tv', g)])
            yield
            for g in range(16):
                S.op('dve', lambda e, g=g: e.max_index(out=ti[:, g, 8:16], in_max=tv[:, g, 8:16], in_values=sc0s[tl][:, g, :]),
                     reads=[('sc0', tl, g), ('tv', g)], writes=[('ti', g)])
            tvk = [('tv', g) for g in range(16)]
            tik = [('ti', g) for g in range(16)]
            S.op('dve', lambda e: e.tensor_tensor(out=cand4, in0=tvv[:, :, 0, :].unsqueeze(3).to_broadcast([128, 8, 16, 16]),
                                                  in1=tvv[:, :, 1, :].unsqueeze(2).to_broadcast([128, 8, 16, 16]), op=ALU.add),
                 reads=tvk, writes=[('cand', h) for h in range(8)])
            yield
            for h in range(8):
                S.op('dve', lambda e, h=h: e.max(out=tsv[:, h, 0:8], in_=cand[:, h, :]), reads=[('cand', h)], writes=[('tsv', h)])
            yield
            for h in range(8):
                S.op('dve', lambda e, h=h: e.max_index(out=tcv[:, h, 0:8], in_max=tsv[:, h, 0:8], in_values=cand[:, h, :]),
                     reads=[('cand', h), ('tsv', h)], writes=[('tcv', h)])
            yield
            for h in range(8):
                S.op('dve', lambda e, h=h: e.match_replace(out=cand[:, h, :], in_to_replace=tsv[:, h, 0:8], in_values=cand[:, h, :], imm_value=NEG),
                     reads=[('cand', h), ('tsv', h)], writes=[('cand', h)])
            yield
            for h in range(8):
                S.op('dve', lambda e, h=h: e.max(out=tsv[:, h, 8:16], in_=cand[:, h, :]), reads=[('cand', h)], writes=[('tsv', h)])
            yield
            for h in range(8):
                S.op('dve', lambda e, h=h: e.max_index(out=tcv[:, h, 8:16], in_max=tsv[:, h, 8:16], in_values=cand[:, h, :]),
                     reads=[('cand', h), ('tsv', h)], writes=[('tcv', h)])
            tsk = [('tsv', h) for h in range(8)]
            tck = [('tcv', h) for h in range(8)]
            ck = [('cand', h) for h in range(8)]
            g3 = i12g[:, 2, :].rearrange("p (h k) -> p h k", h=8)
            S.op('dve', lambda e: e.tensor_tensor(out=g3, in0=tsv[:], in1=tsv[:, :, 0:1].to_broadcast([128, 8, 16]), op=ALU.subtract),
                 reads=tsk, writes=['gate'])
            S.op('act', lambda e: e.activation(out=g3, in_=g3, func=AF.Exp), reads=['gate'], writes=['gate'])
            S.op('dve', lambda e: e.tensor_reduce(out=zs[:], in_=g3, axis=AX.X, op=ALU.add), reads=['gate'], writes=['zs'])
            S.op('dve', lambda e: e.reciprocal(out=zs[:], in_=zs[:]), reads=['zs'], writes=['zs'])
            S.op('dve', lambda e: e.tensor_tensor(out=g3, in0=g3, in1=zs[:].unsqueeze(2).to_broadcast([128, 8, 16]), op=ALU.mult),
                 reads=['gate', 'zs'], writes=['gate'])
            tcf = tcv[:].rearrange("p h k -> p (h k)")
            S.op('dve', lambda e: e.tensor_single_scalar(out=tab[:, 0, :], in_=tcf, scalar=4, op=ALU.logical_shift_right), reads=tck, writes=['tab0'])
            S.op('dve', lambda e: e.tensor_single_scalar(out=tab[:, 1, :], in_=tcf, scalar=15, op=ALU.bitwise_and), reads=tck, writes=['tab1'])
            S.op('dve', lambda e: e.tensor_copy(out=tabf[:], in_=tab[:]), reads=['tab0', 'tab1'], writes=['tabf'])
            S.op('dve', lambda e: e.tensor_copy(out=tif[:], in_=ti[:]), reads=tik, writes=['tif'])
            yield
            for w in range(2):
                af = tabf[:, w, :].rearrange("p (h k) -> p h k", h=8)
                S.op('dve', lambda e, af=af: e.tensor_tensor(out=cand4, in0=af.unsqueeze(3).to_broadcast([128, 8, 16, 16]),
                                                            in1=iota_f[:, 0:16].unsqueeze(1).unsqueeze(1).to_broadcast([128, 8, 16, 16]), op=ALU.is_equal),
                     reads=['tabf', 'iota_f'], writes=ck)
                S.op('dve', lambda e, w=w: e.tensor_tensor(out=cand4, in0=cand4, in1=tifv[:, :, w, :].unsqueeze(2).to_broadcast([128, 8, 16, 16]), op=ALU.mult),
                     reads=ck + ['tif'], writes=ck)
                S.op('dve', lambda e, w=w: e.tensor_reduce(out=i12g[:, w, :].rearrange("p (h k) -> p h k", h=8), in_=cand4, axis=AX.X, op=ALU.add),
                     reads=ck, writes=['i12_%d' % w])
            yield

        def part1b():
            bnk = psn(ALLB)
            for w in range(3):
                S.op('pe', lambda e, w=w, bnk=bnk: e.transpose(out=ps[bnk][:, w * 128:(w + 1) * 128], in_=i12g[:, w, :], identity=ident_f[:]),
                     reads=['i12_0', 'i12_1', 'gate', 'ident_f'], writes=[pk(bnk)])
            S.op('act', lambda e, bnk=bnk: e.copy(out=sT[:], in_=ps[bnk][:, 0:384].rearrange("p (w t) -> p w t", w=3)), reads=[pk(bnk)], writes=['sT'])
            S.op('act', lambda e, bnk=bnk: e.copy(out=sTb[:], in_=ps[bnk][:, 0:256].rearrange("p (w t) -> p w t", w=2)), reads=[pk(bnk)], writes=['sTb'])

        def part2(tt, after_group=None):
            iota_bc = iota_f[:].unsqueeze(1).to_broadcast([128, 8, 128])
            iota_bc4 = iota_f[:].unsqueeze(1).to_broadcast([128, 4, 128])

            def onehots(t8):
                t0 = t8 * 8
                bf = ohi[0] % 3
                ohi[0] += 1
                pkey, qkey = ('PT', bf), ('QT', bf)
                S.op('dve', lambda e: e.tensor_tensor(out=PT[bf][:], in0=iota_rep[:], in1=sTb[:, 0, t0:t0 + 8].unsqueeze(1).to_broadcast([128, 128, 8]),
                                                      op=ALU.is_equal), reads=['sTb', 'iota_rep'], writes=[pkey])
                S.op('pool', lambda e: e.tensor_tensor(out=PT[bf][:], in0=PT[bf][:], in1=sT[:, 2, t0:t0 + 8].unsqueeze(1).to_broadcast([128, 128, 8]),
                                                       op=ALU.mult), reads=['sT', pkey], writes=[pkey])
                S.op('dve', lambda e: e.tensor_tensor(out=QT[bf][:], in0=iota_rep[:], in1=sTb[:, 1, t0:t0 + 8].unsqueeze(1).to_broadcast([128, 128, 8]),
                                                      op=ALU.is_equal), reads=['sTb', 'iota_rep'], writes=[(qkey, 0), (qkey, 1)])
                return bf

            bfs = {0: onehots(0)}
            for t8 in range(16):
                t0 = t8 * 8
                if t8 + 1 < 16:
                    bfs[t8 + 1] = onehots(t8 + 1)
                bf = bfs[t8]
                pkey, qkey = ('PT', bf), ('QT', bf)
                for hq in range(2):
                    bnk = psn(ALLB)
                    for q in range(4):
                        sl = hq * 4 + q
                        S.op('pe', lambda e, q=q, bf=bf, sl=sl, bnk=bnk: e.matmul(ps[bnk][:, :].rearrange("p (j q) -> p q j", q=4)[:, q, :], lhsT=PT[bf][:, :, sl],
                                                                               rhs=QT[bf][:, :, sl], start=True, stop=True), reads=[pkey, (qkey, hq)], writes=[pk(bnk)])
                    tq = t0 + hq * 4
                    dst = GT[:, :, tq:tq + 4]
                    src = ps[bnk][:, :].rearrange("p (j q) -> p j q", q=4)
                    S.op('act', lambda e, dst=dst, src=src: e.copy(out=dst, in_=src), reads=[pk(bnk)], writes=[('GT', tq // 4)])
                if after_group is not None:
                    after_group(t8)
            S.dma(Gd[tt], GT[:].rearrange("p j t -> p (j t)"), reads=[('GT', q) for q in range(32)], writes=[('Gd', tt)])

        def exhaust(g):
            if g is not None:
                for _ in g:
                    pass

        def qproj_gen(nb_, nh_):
            for g in range(16):
                emit_qproj(nb_, nh_, 0, glist=[g])
                yield

        def chain(*gens):
            for g in gens:
                if g is not None:
                    for _ in g:
                        yield

        def stepper(g, n):
            def f(t8):
                for _ in range(n):
                    try:
                        next(g)
                    except StopIteration:
                        return
            return f

        seq = [(b, half) for b in range(NB) for half in range(2)]
        emit_n2(0)
        emit_qproj(0, 0, 0)
        emit_scores(0, 0)
        emit_scores(1, 0)
        exhaust(part1a(0))
        for idx, (b, half) in enumerate(seq):
            nxt = seq[idx + 1] if idx + 1 < len(seq) else None
            part1b()
            if nxt is not None and nxt[1] == 0:
                emit_n2(nxt[0])
            bg = chain(part1a(1), qproj_gen(*nxt) if nxt is not None else None)
            part2(b * 4 + half * 2 + 0, stepper(bg, 2))
            exhaust(bg)
            part1b()
            bg = None
            if nxt is not None:
                emit_scores(0, 0)
                emit_scores(1, 0)
                bg = part1a(0)
            part2(b * 4 + half * 2 + 1, stepper(bg, 1) if bg is not None else None)
            exhaust(bg)
        S.barrier()
    with contextlib.ExitStack() as ph:
        h2a = sb('h2a', [128, 8, T], BF16, stack=ph)
        for k in range(8):
            S.dma(h2a[:, k, :], h2dv[:, k, :], writes=[('h2a', k)])
        h2k = [('h2a', k) for k in range(8)]
        US = [sb('US%d' % i, [128, 8, 1024], BF16, stack=ph) for i in range(2)]
        VS = [sb('VS%d' % i, [128, 8, 1024], BF16, stack=ph) for i in range(2)]
        stg = [sb('stg%d' % i, [128, 1024], stack=ph) for i in range(2)]
        Gt = [sb('Gt%d' % i, [128, 8, 2, 128], BF16, stack=ph) for i in range(2)]
        Wt = [sb('Wt%d' % i, [128, 8, 256], BF16, stack=ph) for i in range(2)]
        ge = [sb('ge%d' % i, [128, 256], BF16, stack=ph) for i in range(3)]
        NS = 16
        NG = T // 256
        sti = [0]
        ALLC = tuple(range(7))
        wmc = [sb('wmc%d' % i, [128, 8, 128], stack=ph) for i in range(2)]

        def modgen(ln):
            def mm(pc):
                wt = wmc[pc % 2]
                for k in range(8):
                    S.op('pe', lambda e, k=k, wt=wt, pc=pc: e.matmul(ps[7][:, pc:pc + 1], lhsT=wt[:, k, :], rhs=cond[:, k:k + 1], start=(k == 0), stop=(k == 7)),
                         reads=[('wmc', pc % 2), 'cond'], writes=[pk(7)])
            for pc in range(48):
                S.dma(wmc[pc % 2][:], w_mod[ln, :, pc * 128:(pc + 1) * 128].rearrange("(k p) n -> p k n", p=128), writes=[('wmc', pc % 2)])
                yield
                if pc >= 1:
                    mm(pc - 1)
            yield
            mm(47)
            S.op('dve', lambda e: e.tensor_tensor(out=modv[:, ln, :], in0=ps[7][:, 0:48], in1=bmt[:, ln, :], op=ALU.add),
                 reads=[pk(7), 'bmt'], writes=[('modv', ln)])
            S.op('dve', lambda e: e.scalar_tensor_tensor(out=gs1[:, ln, :], in0=modv[:, ln, 8:16], scalar=1.0, in1=ngt[:, 0, ln, :],
                                                         op0=ALU.add, op1=ALU.mult), reads=[('modv', ln), 'ngt0'], writes=[('gs1', ln)])
            S.op('dve', lambda e: e.scalar_tensor_tensor(out=gs2[:, ln, :], in0=modv[:, ln, 32:40], scalar=1.0, in1=ngt[:, 1, ln, :],
                                                         op0=ALU.add, op1=ALU.mult), reads=[('modv', ln), 'ngt1'], writes=[('gs2', ln)])

        mg = modgen(l + 1) if l + 1 < L else None

        def load_uv(Sx, jj):
            slot = Sx % 2
            j = Sx * 8 + jj
            for which, src, dstb, eng in (('U', uT, US, 'act'), ('V', vr, VS, 'pool')):
                i = sti[0] % 2
                sti[0] += 1
                S.dma(stg[i][:], src[l, j], writes=[('stg', i)])
                if eng == 'act':
                    S.op('act', lambda e, i=i, dstb=dstb: e.copy(out=dstb[slot][:, jj, :], in_=stg[i][:]), reads=[('stg', i)], writes=[(which, slot, jj)])
                else:
                    S.op('pool', lambda e, i=i, dstb=dstb: e.tensor_copy(out=dstb[slot][:, jj, :], in_=stg[i][:]), reads=[('stg', i)], writes=[(which, slot, jj)])

        def load_g(it):
            Sx, tg = divmod(it, NG)
            gt = Gt[it % 2]
            for hh in range(2):
                tt = tg * 2 + hh
                S.dma(gt[:, :, hh, :], Gd[tt].rearrange("p (j t) -> p j t", j=128)[:, Sx * 8:(Sx + 1) * 8, :], writes=[('Gt', it % 2, hh)])

        for jj in range(8):
            load_uv(0, jj)
        load_g(0)
        for it in range(NS * NG):
            Sx, tg = divmod(it, NG)
            slot = Sx % 2
            if it + 1 < NS * NG:
                load_g(it + 1)
            gt = Gt[it % 2]
            wt = Wt[it % 2]
            tsl = slice(tg * 256, (tg + 1) * 256)
            for jj in range(8):
                bnk = psn(ALLC)
                for dk in range(8):
                    S.op('pe', lambda e, dk=dk, jj=jj, bnk=bnk: e.matmul(ps[bnk][:, 0:256], lhsT=US[slot][:, jj, dk * 128:(dk + 1) * 128], rhs=h2a[:, dk, tsl],
                                                                       start=(dk == 0), stop=(dk == 7)), reads=[('U', slot, jj), ('h2a', dk)], writes=[pk(bnk)])
                gi_ = (it * 8 + jj) % 3
                S.op('act', lambda e, bnk=bnk, gi_=gi_: e.activation(out=ge[gi_][:], in_=ps[bnk][:, 0:256], func=AF.Gelu), reads=[pk(bnk)], writes=[('ge', gi_)])
                eng = 'dve' if jj % 2 == 0 else 'pool'
                S.op(eng, lambda e, jj=jj, gi_=gi_, gt=gt, wt=wt: e.tensor_tensor(out=wt[:, jj, :], in0=ge[gi_][:], in1=gt[:, jj].rearrange("p h t -> p (h t)"), op=ALU.mult),
                     reads=[('ge', gi_), ('Gt', it % 2, 0), ('Gt', it % 2, 1)], writes=[('Wt', it % 2, jj)])
            for dk in range(8):
                bnk = psn(ALLC)
                for jj in range(8):
                    S.op('pe', lambda e, dk=dk, jj=jj, bnk=bnk, wt=wt: e.matmul(ps[bnk][:, 0:256], lhsT=VS[slot][:, jj, dk * 128:(dk + 1) * 128], rhs=wt[:, jj, :],
                                                                              start=(jj == 0), stop=(jj == 7)), reads=[('V', slot, jj), ('Wt', it % 2, jj)], writes=[pk(bnk)])
                S.op('dve', lambda e, dk=dk, bnk=bnk: e.scalar_tensor_tensor(out=xT[:, dk, tsl], in0=ps[bnk][:, 0:256], scalar=modv[:, l, 40 + dk:41 + dk], in1=xT[:, dk, tsl],
                                                                             op0=ALU.mult, op1=ALU.add), reads=[pk(bnk), xk(dk, tg // 2), ('modv', l)], writes=[xk(dk, tg // 2)])
            if mg is not None:
                try:
                    next(mg)
                except StopIteration:
                    mg = None
            if Sx + 1 < NS and tg < 8:
                load_uv(Sx + 1, tg)
            if Sx + 1 < NS and NG < 8 and tg == NG - 1:
                for jj in range(NG, 8):
                    load_uv(Sx + 1, jj)
        if mg is not None:
            for _ in mg:
                pass
        S.barrier()


def prep_inputs(inp, T, L, b):
    f = np.float32

    def fm(v):
        return np.ascontiguousarray(v.reshape(v.shape[:-1] + (8, 128)).swapaxes(-1, -2))

    m = {}
    m['x'] = np.ascontiguousarray(inp['x'][b, :T])
    m['c_fm'] = fm(inp['c'][b])
    m['rel_bias'] = np.ascontiguousarray(inp['rel_bias'].reshape(1, 128))
    m['w_mod'] = inp['w_mod'][:L]
    bm = inp['b_mod'][:L].reshape(L, 6, 8, 128)
    m['b_mod_fm'] = np.ascontiguousarray(bm.transpose(0, 3, 1, 2).reshape(L, 128, 48))
    m['n1g_fm'] = fm(inp['norm1_g'][:L])
    m['n2g_fm'] = fm(inp['norm2_g'][:L])
    m['fing_fm'] = fm(inp['final_g'])
    m['w_in'] = inp['w_in'][:L]
    m['w_out'] = inp['w_out'][:L]
    m['diff_lambda'] = np.ascontiguousarray(inp['diff_lambda'][:L].reshape(L, 128))
    m['subln_g'] = inp['subln_g'][:L]
    cd = inp['conf_dw'][:L].reshape(L, 31, 2, 128)
    m['conf_dw_fm'] = np.ascontiguousarray(cd.transpose(0, 3, 2, 1).reshape(L, 128, 62))
    m['conf_lng_fm'] = np.ascontiguousarray(inp['conf_ln_g'][:L].reshape(L, 2, 128).transpose(0, 2, 1))
    m['conf_lnb_fm'] = np.ascontiguousarray(inp['conf_ln_b'][:L].reshape(L, 2, 128).transpose(0, 2, 1))
    sc = inp['sconv_w'][:L].reshape(L, 3, 2, 128)
    m['sconv_fm'] = np.ascontiguousarray(sc.transpose(0, 3, 2, 1).reshape(L, 128, 6))
    m['sgu_ln_g'] = inp['sgu_ln_g'][:L]
    m['sgu_ln_b'] = inp['sgu_ln_b'][:L]
    m['sgu_wT'] = np.ascontiguousarray(inp['sgu_w'][:L].transpose(0, 3, 1, 2).reshape(L, 128, 512))
    m['sgu_b'] = np.ascontiguousarray(inp['sgu_b'][:L].reshape(L, 512))
    m['peer_wq'] = inp['peer_wq'][:L]
    kk = inp['peer_keys'][:L].reshape(L, 16, 128, 128)
    m['peer_keysT'] = np.ascontiguousarray(kk.transpose(0, 3, 1, 2).reshape(L, 128, 2048))
    return m


_CONST = {}


def consts():
    if _CONST:
        return _CONST
    f = np.float32
    c = {}
    c['ident'] = np.eye(128, dtype=f)
    s = np.arange(128)
    c['trilT'] = (s[:, None] <= s[None, :]).astype(f)
    p = np.arange(128)
    c['maskq'] = np.stack([np.where((p % 64) < 32, 32 ** -0.5, 0.0), np.where((p % 64) >= 32, 32 ** -0.5, 0.0)], axis=1).astype(f)
    c['iota'] = np.broadcast_to(np.arange(128, dtype=f)[None, :], (128, 128)).copy()
    kj = np.arange(128)[:, None, None]
    qi = np.arange(128)[None, None, :]
    slot = np.arange(2)[None, :, None]
    n = (1 - slot) * 128 + qi - kj
    nn = np.maximum(n, 0)
    ratio = np.log(np.maximum(nn, 1).astype(np.float32) / 16) / np.float32(math.log(128 / 16))
    large = np.minimum(16 + (ratio * 16).astype(np.int32), 31)
    bucket = np.where(nn < 16, nn, large)
    c['bk'] = bucket.astype(f).reshape(128, 256)
    c['mk'] = np.where(n >= 0, 0.0, NEG).astype(f).reshape(128, 256)
    _CONST.update(c)
    return c


_SHARED = {}


def kernel(**inputs):
    T, L = 2048, 4
    inp = {k: np.asarray(v) for k, v in inputs.items()}
    nc = build(T, L)
    cst = consts()
    in_maps = []
    shared = None
    for b in range(8):
        m = prep_inputs(inp, T, L, b) if shared is None else None
        if shared is None:
            shared = {k: v for k, v in m.items() if k not in ('x', 'c_fm')}
            shared.update(cst)
            shared.update(prep_peer_tables(inp, L))
        mm = dict(shared)
        mm['x'] = np.ascontiguousarray(inp['x'][b, :T])
        mm['c_fm'] = np.ascontiguousarray(inp['c'][b].reshape(8, 128).T)
        in_maps.append(mm)
    res = run_bass_kernel_spmd(nc, in_maps, core_ids=list(range(8)))
    return np.stack([r['out'] for r in res.results], axis=0).astype(np.float32)


def prep_peer_tables(inp, L):
    u = inp['peer_u'][:L].reshape(L, 128, 128, 8, 128)
    v = inp['peer_v'][:L].reshape(L, 128, 128, 1024)
    return {
        'peer_uT': np.ascontiguousarray(u.transpose(0, 2, 4, 3, 1).reshape(L, 128, 128, 1024)),
        'peer_vr': np.ascontiguousarray(v.transpose(0, 2, 1, 3)),
    }
```

```python
import contextlib
import math
import numpy as np
import concourse.bass as bass
import concourse.mybir as mybir
from concourse.bass_utils import run_bass_kernel_spmd

F32 = mybir.dt.float32
BF16 = mybir.dt.bfloat16
U32 = mybir.dt.uint32
AF = mybir.ActivationFunctionType
ALU = mybir.AluOpType
AX = mybir.AxisListType

D = 1024
EPS = 1e-6
NEG = -1.0e30
NDMASEM = 8


class Sched:
    ENG = ('pe', 'dve', 'act', 'pool', 'sp')

    def __init__(self, nc, st):
        self.nc = nc
        self.e = {'pe': nc.tensor, 'dve': nc.vector, 'act': nc.scalar, 'pool': nc.gpsimd, 'sp': nc.sync}
        self.sem = {}
        for e in ('pe', 'dve', 'act', 'pool'):
            self.sem[e] = st.enter_context(nc.semaphore('s_' + e))
        for q in ('sp', 'pool'):
            for j in range(NDMASEM):
                self.sem[('dma', q, j)] = st.enter_context(nc.semaphore('d_%s%d' % (q, j)))
        self.cnt = {e: 0 for e in self.ENG}
        self.dcnt = {'sp': 0, 'pool': 0}
        self.waited = {e: {} for e in self.ENG}
        self.last_w = {}
        self.readers = {}
        self.nops = 0
        self.psi = 0

    def _deps(self, reads, writes):
        deps = set()
        for r in reads:
            lw = self.last_w.get(r)
            if lw is not None:
                deps.add(lw)
        for w in writes:
            lw = self.last_w.get(w)
            if lw is not None:
                deps.add(lw)
            rd = self.readers.get(w)
            if rd:
                deps.update(rd.values())
        return deps

    def _emit_waits(self, eng, deps):
        w = self.waited[eng]
        eo = self.e[eng]
        for (k, v) in deps:
            if k == 'pe' and eng == 'pe':
                continue
            if w.get(k, 0) >= v:
                continue
            w[k] = v
            eo.wait_ge(self.sem[k], v)

    def _record(self, tok, reads, writes):
        k = tok[0]
        for r in reads:
            self.readers.setdefault(r, {})[k] = tok
        for wr in writes:
            self.last_w[wr] = tok
            self.readers[wr] = {}

    def op(self, eng, fn, reads=(), writes=()):
        deps = self._deps(reads, writes)
        self._emit_waits(eng, deps)
        self.cnt[eng] += 1
        tok = (eng, self.cnt[eng])
        fn(self.e[eng]).then_inc(self.sem[eng], 1)
        self._record(tok, reads, writes)
        self.nops += 1
        return tok

    def dma(self, out, in_, reads=(), writes=(), q='sp', **kw):
        deps = self._deps(reads, writes)
        i = self.dcnt[q]
        self.dcnt[q] += 1
        key = ('dma', q, i % NDMASEM)
        val = 16 * (i // NDMASEM + 1)
        if i >= NDMASEM:
            deps.add((key, val - 16))
        self._emit_waits(q, deps)
        self.e[q].dma_start(out=out, in_=in_, **kw).then_inc(self.sem[key], 16)
        tok = (key, val)
        self._record(tok, reads, writes)
        self.nops += 1
        return tok

    def barrier(self):
        toks = set(self.last_w.values())
        for rd in self.readers.values():
            toks.update(rd.values())
        mx = {}
        for (k, v) in toks:
            mx[k] = max(mx.get(k, 0), v)
        for eng in self.ENG:
            self._emit_waits(eng, set(mx.items()))
        self.last_w = {}
        self.readers = {}

    def wait_all(self, eng, toks):
        self._emit_waits(eng, set(toks))


def build(T, L, with_peer=True, dbg=False):
    nc = bass.Bass("TRN2", target_bir_lowering=False)
    NT = T // 128
    NB = T // 512

    def din(name, shape, dt=F32):
        return nc.dram_tensor(name, list(shape), dt, kind="ExternalInput").ap()

    x_d = din('x', [T, D])
    c_fm = din('c_fm', [128, 8])
    relb = din('rel_bias', [1, 128])
    w_mod = din('w_mod', [L, D, 6 * D])
    b_mod = din('b_mod_fm', [L, 128, 48])
    n1g = din('n1g_fm', [L, 128, 8])
    n2g = din('n2g_fm', [L, 128, 8])
    fing = din('fing_fm', [128, 8])
    w_in = din('w_in', [L, D, 2560])
    w_out = din('w_out', [L, D, D])
    dlam = din('diff_lambda', [L, 128])
    sublg = din('subln_g', [L, 64])
    cdw = din('conf_dw_fm', [L, 128, 2 * 31])
    clng = din('conf_lng_fm', [L, 128, 2])
    clnb = din('conf_lnb_fm', [L, 128, 2])
    scw = din('sconv_fm', [L, 128, 2 * 3])
    slng = din('sgu_ln_g', [L, 256])
    slnb = din('sgu_ln_b', [L, 256])
    sguwT = din('sgu_wT', [L, 128, 4 * 128])
    sgub = din('sgu_b', [L, 512])
    wq = din('peer_wq', [L, D, 2048])
    keysT = din('peer_keysT', [L, 128, 16 * 128])
    uT = din('peer_uT', [L, 128, 128, 1024])
    vr = din('peer_vr', [L, 128, 128, 1024])
    ident_d = din('ident', [128, 128])
    trilT_d = din('trilT', [128, 128])
    bk_d = din('bk', [128, 256])
    mk_d = din('mk', [128, 256])
    maskq_d = din('maskq', [128, 2])
    iota_d = din('iota', [128, 128])
    out_d = nc.dram_tensor('out', [T, D], F32, kind="ExternalOutput").ap()
    dbg_d = nc.dram_tensor('dbg', [T, D], F32, kind="ExternalOutput").ap() if dbg else None

    wind = nc.dram_tensor('wind', [L, 128, 8 * 2560], BF16, kind="Internal").ap()
    woutd = nc.dram_tensor('woutd', [L, 128, 8 * 1024], BF16, kind="Internal").ap()
    h2d = nc.dram_tensor('h2d', [128, 8 * T], BF16, kind="Internal").ap()
    Gd = nc.dram_tensor('Gd', [NT, 128, 128 * 128], BF16, kind="Internal").ap()

    with contextlib.ExitStack() as st:
        S = Sched(nc, st)

        sbn = [0]

        def sb(name, shape, dt=F32, stack=None):
            sbn[0] += 1
            return (stack or st).enter_context(nc.sbuf_tensor('sb%d_%s' % (sbn[0], name), list(shape), dt))

        xT = sb('xT', [128, 8, T])
        ident_f = sb('ident_f', [128, 128])
        ident_b = sb('ident_b', [128, 128], BF16)
        ones_f = sb('ones_f', [128, 128])
        ones_b = sb('ones_b', [128, 128], BF16)
        eps_t = sb('eps_t', [128, 1])
        modv = sb('modv', [128, L, 48])
        gs1 = sb('gs1', [128, L, 8])
        gs2 = sb('gs2', [128, L, 8])
        TB = sb('TB', [128, 4, 256])
        rb_bc = sb('rb_bc', [128, 128])
        trilT = sb('trilT', [128, 128])
        iota_f = sb('iota_f', [128, 128])
        fing_t = sb('fing_t', [128, 8])
        maskq = sb('maskq', [128, 2])
        iota_rep = sb('iota_rep', [128, 128, 8], BF16)
        ps = [st.enter_context(nc.psum_tensor('ps%d' % i, [128, 512], F32)) for i in range(8)]

        def xk(k, b):
            return ('xT', k, b)

        def psn(pool=(0, 1, 2, 3, 4, 5)):
            S.psi += 1
            return pool[S.psi % len(pool)]

        def pk(i):
            return ('ps', i)

        with contextlib.ExitStack() as ph:
            S.dma(ident_f[:], ident_d, writes=['ident_f'])
            S.dma(trilT[:], trilT_d, writes=['trilT'])
            S.dma(iota_f[:], iota_d, writes=['iota_f'])
            S.dma(fing_t[:], fing, writes=['fing'])
            S.dma(maskq[:], maskq_d, writes=['maskq'])
            S.dma(rb_bc[:].unsqueeze(1), relb[0:1, :].partition_broadcast(128), writes=['rb_bc'])
            S.op('dve', lambda e: e.tensor_copy(out=ident_b[:], in_=ident_f[:]), reads=['ident_f'], writes=['ident_b'])
            S.op('pool', lambda e: e.memset(ones_f[:], 1.0), writes=['ones_f'])
            S.op('dve', lambda e: e.tensor_copy(out=iota_rep[:], in_=iota_f[:].unsqueeze(2).to_broadcast([128, 128, 8])), reads=['iota_f'], writes=['iota_rep'])
            S.op('pool', lambda e: e.memset(ones_b[:], 1.0), writes=['ones_b'])
            S.op('pool', lambda e: e.memset(eps_t[:], EPS), writes=['eps'])
            xin = [sb('xin%d' % i, [128, D], stack=ph) for i in range(2)]
            for tt in range(NT):
                xi = xin[tt % 2]
                S.dma(xi[:], x_d[tt * 128:(tt + 1) * 128, :], writes=[('xin', tt % 2)])
                for half in range(2):
                    bnk = psn()
                    for j in range(4):
                        k = half * 4 + j
                        S.op('pe', lambda e, bnk=bnk, j=j, k=k, xi=xi: e.transpose(
                            out=ps[bnk][:, j * 128:(j + 1) * 128], in_=xi[:, k * 128:(k + 1) * 128], identity=ident_f[:]),
                            reads=[('xin', tt % 2), 'ident_f'], writes=[pk(bnk)])
                    eng = 'act' if half == 0 else 'dve'
                    dst = xT[:, half * 4:half * 4 + 4, tt * 128:(tt + 1) * 128]
                    src = ps[bnk][:, :].rearrange("p (j t) -> p j t", j=4)
                    if eng == 'act':
                        S.op('act', lambda e, dst=dst, src=src: e.copy(out=dst, in_=src), reads=[pk(bnk)],
                             writes=[xk(k, tt // 4) for k in range(half * 4, half * 4 + 4)])
                    else:
                        S.op('dve', lambda e, dst=dst, src=src: e.tensor_copy(out=dst, in_=src), reads=[pk(bnk)],
                             writes=[xk(k, tt // 4) for k in range(half * 4, half * 4 + 4)])
            cond = sb('cond', [128, 8], stack=ph)
            bmt = sb('bmt', [128, L, 48], stack=ph)
            ngt = sb('ngt', [128, 2, L, 8], stack=ph)
            S.dma(cond[:], c_fm, writes=['cond'])
            S.dma(bmt[:], b_mod.rearrange("l p j -> p l j"), writes=['bmt'])
            S.dma(ngt[:, 0], n1g.rearrange("l p j -> p l j"), writes=['ngt0'])
            S.dma(ngt[:, 1], n2g.rearrange("l p j -> p l j"), writes=['ngt1'])
            S.op('act', lambda e: e.activation(out=cond[:], in_=cond[:], func=AF.Silu), reads=['cond'], writes=['cond'])
            wmt = [sb('wmt%d' % i, [128, 8, 512], stack=ph) for i in range(2)]
            wi = 0
            for l in range(L):
                mb = psn()
                for pc in range(12):
                    wt = wmt[wi % 2]
                    wkey = ('wmt', wi % 2)
                    wi += 1
                    S.dma(wt[:], w_mod[l, :, pc * 512:(pc + 1) * 512].rearrange("(k p) n -> p k n", p=128), writes=[wkey])
                    for oc in range(4):
                        col = pc * 4 + oc
                        for k in range(8):
                            S.op('pe', lambda e, wt=wt, oc=oc, k=k, col=col, mb=mb: e.matmul(
                                ps[mb][:, col:col + 1], lhsT=wt[:, k, oc * 128:(oc + 1) * 128], rhs=cond[:, k:k + 1],
                                start=(k == 0), stop=(k == 7)), reads=[wkey, 'cond'], writes=[pk(mb)])
                S.op('dve', lambda e, l=l, mb=mb: e.tensor_tensor(out=modv[:, l, :], in0=ps[mb][:, 0:48], in1=bmt[:, l, :], op=ALU.add),
                     reads=[pk(mb), 'bmt'], writes=[('modv', l)])
                S.op('dve', lambda e, l=l: e.scalar_tensor_tensor(out=gs1[:, l, :], in0=modv[:, l, 8:16], scalar=1.0, in1=ngt[:, 0, l, :],
                                                                  op0=ALU.add, op1=ALU.mult), reads=[('modv', l), 'ngt0'], writes=[('gs1', l)])
                S.op('dve', lambda e, l=l: e.scalar_tensor_tensor(out=gs2[:, l, :], in0=modv[:, l, 32:40], scalar=1.0, in1=ngt[:, 1, l, :],
                                                                  op0=ALU.add, op1=ALU.mult), reads=[('modv', l), 'ngt1'], writes=[('gs2', l)])
            bkt = sb('bkt', [128, 256], stack=ph)
            mkt = sb('mkt', [128, 256], stack=ph)
            tbt = sb('tbt', [128, 256], stack=ph)
            S.dma(bkt[:], bk_d, writes=['bkt'])
            S.dma(mkt[:], mk_d, writes=['mkt'])
            for h in range(4):
                S.op('pool', lambda e, h=h: e.tensor_copy(out=TB[:, h, :], in_=mkt[:]), reads=['mkt'], writes=[('TB', h)])
                for bq in range(32):
                    S.op('dve', lambda e, h=h, bq=bq: e.tensor_scalar(out=tbt[:], in0=bkt[:], scalar1=float(bq), scalar2=rb_bc[:, bq * 4 + h:bq * 4 + h + 1],
                                                                    op0=ALU.is_equal, op1=ALU.mult), reads=['bkt', 'rb_bc'], writes=['tbt'])
                    S.op('dve', lambda e, h=h: e.tensor_tensor(out=TB[:, h, :], in0=TB[:, h, :], in1=tbt[:], op=ALU.add),
                         reads=['tbt', ('TB', h)], writes=[('TB', h)])
            stf = [sb('stf%d' % i, [128, 8, 256], stack=ph) for i in range(2)]
            stb = [sb('stb%d' % i, [128, 8, 256], BF16, stack=ph) for i in range(2)]
            ci = 0
            for l in range(L):
                for (src, dstd, ncol) in ((w_in, wind, 2560), (w_out, woutd, 1024)):
                    for pc in range(ncol // 256):
                        sf, sbf = stf[ci % 2], stb[ci % 2]
                        kf, kb_ = ('stf', ci % 2), ('stb', ci % 2)
                        S.dma(sf[:], src[l, :, pc * 256:(pc + 1) * 256].rearrange("(k p) n -> p k n", p=128), writes=[kf])
                        eng = ('dve', 'pool')[ci % 2]
                        S.op(eng, lambda e, sf=sf, sbf=sbf: e.tensor_copy(out=sbf[:], in_=sf[:]), reads=[kf], writes=[kb_])
                        S.dma(dstd[l].rearrange("p (k n) -> p k n", k=8)[:, :, pc * 256:(pc + 1) * 256], sbf[:], reads=[kb_],
                              writes=[('wd', id(dstd), l, pc)])
                        ci += 1
            S.barrier()

        for l in range(L):
            lam_init = 0.8 - 0.6 * math.exp(-0.3 * l)
            with contextlib.ExitStack() as ph:
                dlt = sb('dlt', [128, 128], stack=ph)
                sgb = sb('sgb', [128, 64], stack=ph)
                cdwt = sb('cdwt', [128, 62], stack=ph)
                clg = sb('clg', [128, 2], stack=ph)
                clb = sb('clb', [128, 2], stack=ph)
                scwt = sb('scwt', [128, 6], stack=ph)
                slg = sb('slg', [128, 256], stack=ph)
                slb = sb('slb', [128, 256], stack=ph)
                wtf = sb('wtf', [128, 512], stack=ph)
                WTm = sb('WTm', [128, 512], BF16, stack=ph)
                sbf_ = sb('sbf_', [1, 512], stack=ph)
                sbb = sb('sbb', [1, 512], BF16, stack=ph)
                lamv = sb('lamv', [128, 8], stack=ph)
                junk = sb('junk', [128, 64], stack=ph)
                S.dma(dlt[:].unsqueeze(1), dlam[l:l + 1, :].partition_broadcast(128), writes=['dlt'])
                S.dma(sgb[:].unsqueeze(1), sublg[l:l + 1, :].partition_broadcast(128), writes=['sgb'])
                S.dma(cdwt[:], cdw[l], writes=['cdwt'])
                S.dma(clg[:], clng[l], writes=['clg'])
                S.dma(clb[:], clnb[l], writes=['clb'])
                S.dma(scwt[:], scw[l], writes=['scwt'])
                S.dma(slg[:].unsqueeze(1), slng[l:l + 1, :].partition_broadcast(128), writes=['slg'])
                S.dma(slb[:].unsqueeze(1), slnb[l:l + 1, :].partition_broadcast(128), writes=['slb'])
                S.dma(wtf[:], sguwT[l], writes=['wtf'])
                S.dma(sbf_[:], sgub[l:l + 1, :], writes=['sbf'])
                S.op('dve', lambda e: e.tensor_tensor(out=WTm[:].rearrange("p (h t) -> p h t", h=4), in0=wtf[:].rearrange("p (h t) -> p h t", h=4),
                                                      in1=trilT[:].unsqueeze(1).to_broadcast([128, 4, 128]), op=ALU.mult),
                     reads=['wtf', 'trilT'], writes=['WTm'])
                S.op('dve', lambda e: e.tensor_copy(out=sbb[:], in_=sbf_[:]), reads=['sbf'], writes=['sbb'])
                S.op('dve', lambda e: e.tensor_scalar(out=sgb[:], in0=sgb[:], scalar1=float(1.0 - lam_init), scalar2=None, op0=ALU.mult),
                     reads=['sgb'], writes=['sgb'])
                S.op('dve', lambda e: e.tensor_tensor(out=junk[:, 0:32], in0=dlt[:, 0:32], in1=dlt[:, 32:64], op=ALU.mult), reads=['dlt'], writes=['junk'])
                S.op('dve', lambda e: e.tensor_reduce(out=lamv[:, 0:1], in_=junk[:, 0:32], axis=AX.X, op=ALU.add), reads=['junk'], writes=['lam0'])
                S.op('dve', lambda e: e.tensor_tensor(out=junk[:, 32:64], in0=dlt[:, 64:96], in1=dlt[:, 96:128], op=ALU.mult), reads=['dlt'], writes=['junk'])
                S.op('dve', lambda e: e.tensor_reduce(out=lamv[:, 1:2], in_=junk[:, 32:64], axis=AX.X, op=ALU.add), reads=['junk'], writes=['lam1'])
                S.op('act', lambda e: e.activation(out=lamv[:, 2:4], in_=lamv[:, 0:2], func=AF.Exp), reads=['lam0', 'lam1'], writes=['lam2'])
                S.op('dve', lambda e: e.tensor_tensor(out=lamv[:, 4:5], in0=lamv[:, 2:3], in1=lamv[:, 3:4], op=ALU.subtract), reads=['lam2'], writes=['lam4'])
                S.op('dve', lambda e: e.tensor_scalar(out=lamv[:, 5:6], in0=lamv[:, 4:5], scalar1=float(lam_init), scalar2=-1.0, op0=ALU.add, op1=ALU.mult),
                     reads=['lam4'], writes=['neglam'])

                hT = sb('hT', [128, 8, 512], BF16, stack=ph)
                yT = sb('yT', [128, 8, 512], BF16, stack=ph)
                kT = sb('kT', [128, 2, T], BF16, stack=ph)
                qT = sb('qT', [128, 2, 2, 512], BF16, stack=ph)
                vaug = sb('vaug', [128, NT, 4, 66], BF16, stack=ph)
                wsl = [sb('wsl%d' % i, [128, 8, 256], BF16, stack=ph) for i in range(3)]
                wol = [sb('wol%d' % i, [128, 8, 128], BF16, stack=ph) for i in range(2)]
                wkf = [sb('wkf%d' % i, [128, 512], stack=ph) for i in range(6)]
                GL = sb('GL', [128, 2, 30 + 512], BF16, stack=ph)
                dgt = sb('dgt', [128, 62, 128], BF16, stack=ph)
                zc = sb('zc', [128, 2, 512], stack=ph)
                MM = sb('MM', [128, 2, 2 + 512], stack=ph)
                uTt = sb('uTt', [128, 2, 512], stack=ph)
                Ebuf = [sb('E%d' % i, [128, 16, 128], BF16, stack=ph) for i in range(2)]
                tmpn = [sb('tmpn%d' % i, [128, 256], stack=ph) for i in range(2)]
                yat = sb('yat', [128, 256], BF16, stack=ph)
                att = sb('att', [128, 4, 80], stack=ph)
                gv = [sb('gv%d' % i, [128, 256], stack=ph) for i in range(4)]
                vn = [sb('vn%d' % i, [128, 256], BF16, stack=ph) for i in range(2)]
                bst = sb('bst', [128, 64], stack=ph)
                rst = sb('rst', [128, 512], stack=ph)
                rkey = 'rst'
                S.op('pool', lambda e: e.memset(vaug[:], 1.0), writes=['vaug_all'])
                for idx_ in range(62):
                    S.op('dve' if idx_ % 2 == 0 else 'pool', lambda e, idx_=idx_: e.tensor_scalar(out=dgt[:, idx_, :], in0=ident_f[:], scalar1=cdwt[:, idx_:idx_ + 1], scalar2=None, op0=ALU.mult),
                         reads=['cdwt', 'ident_f'], writes=['dgt'])
                S.op('pool', lambda e: e.memset(GL[:, :, 0:30], 0.0), writes=['GLh0', 'GLh1'])
                S.op('pool', lambda e: e.memset(MM[:, :, 0:2], 0.0), writes=['MMh0', 'MMh1'])
                wsi = [0]
                wki = [0]

                def wk():
                    wki[0] += 1
                    i = wki[0] % 6
                    return wkf[i], ('wkf', i)

                def load_slice(s):
                    i = wsi[0] % 3
                    wsi[0] += 1
                    S.dma(wsl[i][:], wind[l].rearrange("p (k n) -> p k n", k=8)[:, :, s * 256:(s + 1) * 256], writes=[('wsl', i)],
                          reads=[('wd', id(wind), l, s)])
                    return wsl[i], ('wsl', i)

                def fm_chunk(w, wkey, c, bnk):
                    for k in range(8):
                        S.op('pe', lambda e, k=k: e.matmul(ps[bnk][:, :], lhsT=w[:, k, c * 128:(c + 1) * 128], rhs=hT[:, k, :],
                                                           start=(k == 0), stop=(k == 7)), reads=[wkey, 'hT'], writes=[pk(bnk)])

                def tm_tile(w, wkey, tt, bnk):
                    for k in range(8):
                        S.op('pe', lambda e, k=k: e.matmul(ps[bnk][:, 0:256], lhsT=hT[:, k, tt * 128:(tt + 1) * 128], rhs=w[:, k, :],
                                                           start=(k == 0), stop=(k == 7)), reads=[wkey, 'hT'], writes=[pk(bnk)])

                for b in range(NB):
                    bs = slice(b * 512, (b + 1) * 512)
                    sbank = psn()
                    for k in range(8):
                        w_, wkey_ = wk()
                        S.op('act', lambda e, k=k, w_=w_: e.activation(out=w_[:], in_=xT[:, k, bs], func=AF.Square), reads=[xk(k, b)], writes=[wkey_])
                        S.op('pe', lambda e, k=k, w_=w_: e.matmul(ps[sbank][:, :], lhsT=ones_f[:], rhs=w_[:], start=(k == 0), stop=(k == 7)),
                             reads=[wkey_, 'ones_f'], writes=[pk(sbank)])
                    S.op('act', lambda e: e.activation(out=rst[:], in_=ps[sbank][:, :], func=AF.Ln, bias=eps_t[:], scale=1.0 / D),
                         reads=[pk(sbank), 'eps'], writes=[rkey])
                    S.op('act', lambda e: e.activation(out=rst[:], in_=rst[:], func=AF.Exp, scale=-0.5), reads=[rkey], writes=[rkey])
                    for k in range(8):
                        w_, wkey_ = wk()
                        S.op('dve', lambda e, k=k, w_=w_: e.scalar_tensor_tensor(out=w_[:], in0=xT[:, k, bs], scalar=gs1[:, l, k:k + 1], in1=rst[:],
                                                                                 op0=ALU.mult, op1=ALU.mult), reads=[xk(k, b), rkey, ('gs1', l)], writes=[wkey_])
                        S.op('act', lambda e, k=k, w_=w_: e.activation(out=hT[:, k, :], in_=w_[:], func=AF.Identity, bias=modv[:, l, k:k + 1], scale=1.0),
                             reads=[wkey_, ('modv', l)], writes=['hT'])
                    w, wkey = load_slice(1)
                    for c in range(2):
                        bnk = psn()
                        fm_chunk(w, wkey, c, bnk)
                        S.op('act', lambda e, c=c, bnk=bnk: e.copy(out=kT[:, c, bs], in_=ps[bnk][:, :]), reads=[pk(bnk)], writes=[('kT', c, b)])
                    w, wkey = load_slice(2)
                    for i in range(4):
                        gi = b * 4 + i
                        bnk = psn()
                        tm_tile(w, wkey, i, bnk)
                        S.op('dve', lambda e, gi=gi, bnk=bnk: e.tensor_copy(out=vaug[:, gi, :, 0:64], in_=ps[bnk][:, 0:256].rearrange("p (h d) -> p h d", h=4)),
                             reads=[pk(bnk), 'vaug_all'], writes=[('vaug', gi)])
                    w, wkey = load_slice(0)
                    for c in range(2):
                        bnk = psn()
                        fm_chunk(w, wkey, c, bnk)
                        for m in range(2):
                            S.op('act', lambda e, c=c, bnk=bnk, m=m: e.activation(out=qT[:, m, c, :], in_=ps[bnk][:, :], func=AF.Copy, scale=maskq[:, m:m + 1]),
                                 reads=[pk(bnk), 'maskq'], writes=[('qT', c)])
                    PA, PR = (0, 1, 2, 6), (3, 4, 5)

                    def gen_attn():
                        for i in range(4):
                            gi = b * 4 + i
                            for h in range(4):
                                c = h // 2
                                acc = 7
                                ao = ((gi * 4 + h) % 2) * 256
                                for m in range(2):
                                    E = Ebuf[m]
                                    ekey = ('E', m)
                                    pb = (h % 2) * 64
                                    bnk = psn(PA)
                                    nears = [gi] if gi == 0 else [gi - 1, gi]
                                    for kb in nears:
                                        slot = kb - (gi - 1)
                                        S.op('pe', lambda e, kb=kb, slot=slot, bnk=bnk, pb=pb: e.matmul(
                                            ps[bnk][:, slot * 128:(slot + 1) * 128], lhsT=kT[pb:pb + 64, c, kb * 128:(kb + 1) * 128],
                                            rhs=qT[pb:pb + 64, m, c, i * 128:(i + 1) * 128], start=True, stop=True),
                                            reads=[('kT', c, kb // 4), ('qT', c)], writes=[pk(bnk)])
                                    s0 = 1 if gi == 0 else 0
                                    tn = tmpn[m]
                                    S.op('dve', lambda e, bnk=bnk, s0=s0, tn=tn: e.tensor_tensor(out=tn[:, s0 * 128:256], in0=ps[bnk][:, s0 * 128:256],
                                                                                                 in1=TB[:, h, s0 * 128:256], op=ALU.add),
                                         reads=[pk(bnk), ('TB', h)], writes=[('tmpn', m)])
                                    S.op('act', lambda e, s0=s0, tn=tn, E=E: e.activation(
                                        out=E[:, gi - 1 + s0:gi + 1, :], in_=tn[:, s0 * 128:256].rearrange("p (s q) -> p s q", q=128), func=AF.Exp),
                                        reads=[('tmpn', m)], writes=[ekey])
                                    for g0 in range(0, gi - 1, 4):
                                        g1 = min(g0 + 4, gi - 1)
                                        bnk = psn(PA)
                                        for kb in range(g0, g1):
                                            S.op('pe', lambda e, kb=kb, bnk=bnk, pb=pb: e.matmul(
                                                ps[bnk][:, (kb - g0) * 128:(kb - g0 + 1) * 128], lhsT=kT[pb:pb + 64, c, kb * 128:(kb + 1) * 128],
                                                rhs=qT[pb:pb + 64, m, c, i * 128:(i + 1) * 128], start=True, stop=True),
                                                reads=[('kT', c, kb // 4), ('qT', c)], writes=[pk(bnk)])
                                        S.op('act', lambda e, g0=g0, g1=g1, bnk=bnk, E=E: e.activation(
                                            out=E[:, g0:g1, :], in_=ps[bnk][:, 0:(g1 - g0) * 128].rearrange("p (s q) -> p s q", q=128), func=AF.Exp,
                                            bias=rb_bc[:, 124 + h:125 + h], scale=1.0), reads=[pk(bnk), 'rb_bc'], writes=[ekey])
                                    for kb in range(gi + 1):
                                        S.op('pe', lambda e, kb=kb, E=E: e.matmul(ps[acc][:, ao + m * 128:ao + m * 128 + 65], lhsT=E[:, kb, :], rhs=vaug[:, kb, h, 0:65],
                                                                               start=(kb == 0), stop=(kb == gi)),
                                             reads=[ekey, ('vaug', kb)], writes=[('acc', ao)])
                                    yield
                                a = att[:, h, :]
                                akey = ('att', h)
                                S.op('dve', lambda e, a=a, acc=acc: e.reciprocal(out=a[:, 0:2], in_=ps[acc][:, ao:ao + 256].rearrange("p (m x) -> p m x", m=2)[:, :, 64]),
                                     reads=[('acc', ao)], writes=[akey])
                                S.op('dve', lambda e, a=a: e.tensor_tensor(out=a[:, 2:3], in0=a[:, 1:2], in1=lamv[:, 5:6], op=ALU.mult),
                                     reads=[akey, 'neglam'], writes=[akey])
                                S.op('dve', lambda e, a=a, acc=acc: e.tensor_scalar(out=a[:, 8:72], in0=ps[acc][:, ao:ao + 64], scalar1=a[:, 0:1], scalar2=None, op0=ALU.mult),
                                     reads=[('acc', ao), akey], writes=[akey])
                                S.op('dve', lambda e, a=a, acc=acc: e.scalar_tensor_tensor(out=a[:, 8:72], in0=ps[acc][:, ao + 128:ao + 192], scalar=a[:, 2:3], in1=a[:, 8:72],
                                                                                           op0=ALU.mult, op1=ALU.add), reads=[('acc', ao), akey], writes=[akey])
                                S.op('act', lambda e, a=a: e.activation(out=junk[:, 0:64], in_=a[:, 8:72], func=AF.Square, accum_out=a[:, 3:4]),
                                     reads=[akey], writes=[akey, 'junk'])
                                S.op('act', lambda e, a=a: e.activation(out=a[:, 4:5], in_=a[:, 3:4], func=AF.Ln, bias=eps_t[:], scale=1.0 / 64),
                                     reads=[akey, 'eps'], writes=[akey])
                                S.op('act', lambda e, a=a: e.activation(out=a[:, 4:5], in_=a[:, 4:5], func=AF.Exp, scale=-0.5), reads=[akey], writes=[akey])
                                S.op('dve', lambda e, a=a, h=h: e.scalar_tensor_tensor(out=yat[:, h * 64:(h + 1) * 64], in0=a[:, 8:72], scalar=a[:, 4:5], in1=sgb[:],
                                                                                       op0=ALU.mult, op1=ALU.mult), reads=[akey, 'sgb'], writes=[('yat', h)])
                                yield
                            bnk = psn(PA)
                            pbf = ps[bnk][:, 0:128].bitcast(BF16)
                            for c in range(2):
                                S.op('pe', lambda e, c=c, pbf=pbf: e.transpose(out=pbf[:, c * 128:(c + 1) * 128], in_=yat[:, c * 128:(c + 1) * 128], identity=ident_b[:]),
                                     reads=[('yat', 2 * c), ('yat', 2 * c + 1), 'ident_b'], writes=[pk(bnk)])
                            S.op('act', lambda e, pbf=pbf, i=i: e.copy(out=yT[:, 0:2, i * 128:(i + 1) * 128], in_=pbf.rearrange("p (c t) -> p c t", c=2)),
                                 reads=[pk(bnk)], writes=[('yT', 0, i), ('yT', 1, i)])

                    def gen_rest():
                        wgb, kgb = load_slice(4)
                        wga, kga = load_slice(3)
                        for c in range(2):
                            bnk = psn(PR)
                            yield
                            fm_chunk(wgb, kgb, c, bnk)
                            sg, sgk = wk()
                            S.op('act', lambda e, bnk=bnk, sg=sg: e.activation(out=sg[:], in_=ps[bnk][:, :], func=AF.Sigmoid), reads=[pk(bnk)], writes=[sgk])
                            bnk2 = psn(PR)
                            yield
                            fm_chunk(wga, kga, c, bnk2)
                            S.op('dve', lambda e, bnk2=bnk2, sg=sg, c=c: e.tensor_tensor(out=GL[:, c, 30:542], in0=ps[bnk2][:, :], in1=sg[:], op=ALU.mult),
                                 reads=[pk(bnk2), sgk], writes=[('GL', c)])
                            cbk = psn(PR)
                            for j in range(31):
                                if j % 8 == 7:
                                    yield
                                S.op('pe', lambda e, c=c, j=j, cbk=cbk: e.matmul(ps[cbk][:, :], lhsT=dgt[:, c * 31 + j, :], rhs=GL[:, c, j:j + 512],
                                                                               start=(j == 0), stop=(j == 30)), reads=[('GL', c), 'GLh%d' % c, 'dgt'], writes=[pk(cbk)])
                            S.op('act', lambda e, c=c, cbk=cbk: e.copy(out=zc[:, c, :], in_=ps[cbk][:, :]), reads=[pk(cbk)], writes=[('zc', c)])
                            S.op('pool', lambda e, c=c: e.tensor_copy(out=GL[:, c, 0:30], in_=GL[:, c, 512:542]), reads=[('GL', c)], writes=['GLh%d' % c])
                        yield
                        mub, msb = psn(PR), psn(PR)
                        for c in range(2):
                            S.op('pe', lambda e, c=c: e.matmul(ps[mub][:, :], lhsT=ones_f[:], rhs=zc[:, c, :], start=(c == 0), stop=(c == 1)),
                                 reads=[('zc', c), 'ones_f'], writes=[pk(mub)])
                        for c in range(2):
                            w_, wkey_ = wk()
                            S.op('act', lambda e, c=c, w_=w_: e.activation(out=w_[:], in_=zc[:, c, :], func=AF.Square), reads=[('zc', c)], writes=[wkey_])
                            S.op('pe', lambda e, c=c, w_=w_: e.matmul(ps[msb][:, :], lhsT=ones_f[:], rhs=w_[:], start=(c == 0), stop=(c == 1)),
                                 reads=[wkey_, 'ones_f'], writes=[pk(msb)])
                        mu, muk = wk()
                        var, vark = wk()
                        S.op('act', lambda e: e.activation(out=mu[:], in_=ps[mub][:, :], func=AF.Copy, scale=1.0 / 256), reads=[pk(mub)], writes=[muk])
                        S.op('dve', lambda e: e.tensor_tensor(out=var[:], in0=mu[:], in1=mu[:], op=ALU.mult), reads=[muk], writes=[vark])
                        S.op('dve', lambda e: e.scalar_tensor_tensor(out=var[:], in0=ps[msb][:, :], scalar=1.0 / 256, in1=var[:], op0=ALU.mult, op1=ALU.subtract),
                             reads=[pk(msb), vark], writes=[vark])
                        S.op('dve', lambda e: e.tensor_scalar(out=var[:], in0=var[:], scalar1=0.0, scalar2=None, op0=ALU.max), reads=[vark], writes=[vark])
                        S.op('act', lambda e: e.activation(out=var[:], in_=var[:], func=AF.Ln, bias=eps_t[:], scale=1.0), reads=[vark, 'eps'], writes=[vark])
                        S.op('act', lambda e: e.activation(out=var[:], in_=var[:], func=AF.Exp, scale=-0.5), reads=[vark], writes=[vark])
                        for c in range(2):
                            w_, wkey_ = wk()
                            S.op('dve', lambda e, c=c, w_=w_: e.tensor_tensor(out=w_[:], in0=zc[:, c, :], in1=mu[:], op=ALU.subtract), reads=[('zc', c), muk], writes=[wkey_])
                            S.op('dve', lambda e, w_=w_: e.tensor_tensor(out=w_[:], in0=w_[:], in1=var[:], op=ALU.mult), reads=[wkey_, vark], writes=[wkey_])
                            S.op('act', lambda e, c=c, w_=w_: e.activation(out=yT[:, 2 + c, :], in_=w_[:], func=AF.Silu, bias=clb[:, c:c + 1], scale=clg[:, c:c + 1]),
                                 reads=[wkey_, 'clg', 'clb'], writes=[('yT', 2 + c, i) for i in range(4)])
                        wcc, kcc = load_slice(6)
                        wch, kch = load_slice(7)
                        for c in range(2):
                            bnk = psn(PR)
                            yield
                            fm_chunk(wcc, kcc, c, bnk)
                            w_, wkey_ = wk()
                            S.op('act', lambda e, bnk=bnk, w_=w_: e.copy(out=w_[:], in_=ps[bnk][:, :]), reads=[pk(bnk)], writes=[wkey_])
                            bnk2 = psn(PR)
                            yield
                            fm_chunk(wch, kch, c, bnk2)
                            S.op('dve', lambda e, bnk2=bnk2, w_=w_, c=c: e.tensor_tensor(out=MM[:, c, 2:514], in0=ps[bnk2][:, :], in1=w_[:], op=ALU.mult),
                                 reads=[pk(bnk2), wkey_], writes=[('MM', c)])
                        wcb, kcb = load_slice(5)
                        for c in range(2):
                            cv, cvk = wk()
                            S.op('dve', lambda e, c=c, cv=cv: e.tensor_scalar(out=cv[:], in0=MM[:, c, 0:512], scalar1=scwt[:, c * 3:c * 3 + 1], scalar2=None, op0=ALU.mult),
                                 reads=[('MM', c), 'MMh%d' % c, 'scwt'], writes=[cvk])
                            for j in (1, 2):
                                S.op('dve', lambda e, c=c, j=j, cv=cv: e.scalar_tensor_tensor(out=cv[:], in0=MM[:, c, j:j + 512], scalar=scwt[:, c * 3 + j:c * 3 + j + 1], in1=cv[:],
                                                                                              op0=ALU.mult, op1=ALU.add), reads=[('MM', c), 'MMh%d' % c, cvk], writes=[cvk])
                            S.op('pool', lambda e, c=c: e.tensor_copy(out=MM[:, c, 0:2], in_=MM[:, c, 512:514]), reads=[('MM', c)], writes=['MMh%d' % c])
                            bnk = psn(PR)
                            yield
                            fm_chunk(wcb, kcb, c, bnk)
                            S.op('dve', lambda e, c=c, cv=cv, bnk=bnk: e.tensor_tensor(out=yT[:, 4 + c, :], in0=ps[bnk][:, :], in1=cv[:], op=ALU.mult),
                                 reads=[pk(bnk), cvk], writes=[('yT', 4 + c, i) for i in range(4)])
                        wsu, ksu = load_slice(8)
                        for c in range(2):
                            bnk = psn(PR)
                            yield
                            fm_chunk(wsu, ksu, c, bnk)
                            S.op('act', lambda e, c=c, bnk=bnk: e.activation(out=uTt[:, c, :], in_=ps[bnk][:, :], func=AF.Gelu), reads=[pk(bnk)], writes=[('uTt', c)])
                        wsv, ksv = load_slice(9)
                        zb = [3, 4]
                        for i in range(4):
                            bnk = 5
                            yield
                            tm_tile(wsv, ksv, i, bnk)
                            S.op('act', lambda e, bnk=bnk, i=i: e.activation(out=gv[i][:], in_=ps[bnk][:, 0:256], func=AF.Gelu), reads=[pk(bnk)], writes=[('gv', i)])
                        for i in range(4):
                            yield
                            g_ = gv[i]
                            gk = ('gv', i)
                            v_ = vn[i % 2]
                            vk_ = ('vn', i % 2)
                            bs_ = bst[:, i * 16:(i + 1) * 16]
                            bk_ = ('bst', i)
                            S.op('dve', lambda e, g_=g_, bs_=bs_: e.bn_stats(out=bs_[:, 0:6], in_=g_[:]), reads=[gk], writes=[bk_])
                            S.op('dve', lambda e, bs_=bs_: e.bn_aggr(out=bs_[:, 8:10], in_=bs_[:, 0:6]), reads=[bk_], writes=[bk_])
                            S.op('act', lambda e, bs_=bs_: e.activation(out=bs_[:, 10:11], in_=bs_[:, 9:10], func=AF.Ln, bias=eps_t[:], scale=1.0), reads=[bk_, 'eps'], writes=[bk_])
                            S.op('act', lambda e, bs_=bs_: e.activation(out=bs_[:, 10:11], in_=bs_[:, 10:11], func=AF.Exp, scale=-0.5), reads=[bk_], writes=[bk_])
                            S.op('dve', lambda e, g_=g_, bs_=bs_: e.tensor_scalar(out=g_[:], in0=g_[:], scalar1=bs_[:, 8:9], scalar2=bs_[:, 10:11], op0=ALU.subtract, op1=ALU.mult),
                                 reads=[gk, bk_], writes=[gk])
                            S.op('dve', lambda e, g_=g_: e.tensor_tensor(out=g_[:], in0=g_[:], in1=slg[:], op=ALU.mult), reads=[gk, 'slg'], writes=[gk])
                            S.op('dve', lambda e, g_=g_, v_=v_: e.tensor_tensor(out=v_[:], in0=g_[:], in1=slb[:], op=ALU.add), reads=[gk, 'slb'], writes=[vk_])
                            for h in range(4):
                                c = h // 2
                                po = (h % 2) * 64
                                S.op('pe', lambda e, h=h, c=c, po=po, v_=v_, i=i: e.matmul(ps[zb[c]][po:po + 64, i * 128:(i + 1) * 128], lhsT=v_[:, h * 64:(h + 1) * 64],
                                                                                     rhs=WTm[:, h * 128:(h + 1) * 128], start=True, stop=False),
                                     reads=[vk_, 'WTm'], writes=[pk(zb[c])])
                                S.op('pe', lambda e, h=h, c=c, po=po, i=i: e.matmul(ps[zb[c]][po:po + 64, i * 128:(i + 1) * 128], lhsT=ones_b[0:1, 0:64],
                                                                              rhs=sbb[0:1, h * 128:(h + 1) * 128], start=False, stop=True),
                                     reads=['ones_b', 'sbb'], writes=[pk(zb[c])])
                        for c in range(2):
                            S.op('dve', lambda e, c=c: e.tensor_tensor(out=yT[:, 6 + c, :], in0=ps[zb[c]][:, :], in1=uTt[:, c, :], op=ALU.mult),
                                 reads=[pk(zb[c]), ('uTt', c)], writes=[('yT', 6 + c, i) for i in range(4)])

                    ga_, gr_ = gen_attn(), gen_rest()
                    alive = [True, True]
                    while alive[0] or alive[1]:
                        if alive[0]:
                            try:
                                next(ga_)
                            except StopIteration:
                                alive[0] = False
                        for _ in range(2):
                            if alive[1]:
                                try:
                                    next(gr_)
                                except StopIteration:
                                    alive[1] = False
                    ykeys = [('yT', k, i) for k in range(8) for i in range(4)]
                    for oc in range(8):
                        wo = wol[oc % 2]
                        wok = ('wol', oc % 2)
                        S.dma(wo[:], woutd[l].rearrange("p (k n) -> p k n", k=8)[:, :, oc * 128:(oc + 1) * 128], writes=[wok],
                              reads=[('wd', id(woutd), l, oc // 2)])
                        bnk = psn()
                        for k in range(8):
                            S.op('pe', lambda e, k=k, wo=wo, bnk=bnk: e.matmul(ps[bnk][:, :], lhsT=wo[:, k, :], rhs=yT[:, k, :], start=(k == 0), stop=(k == 7)),
                                 reads=[wok] + ykeys, writes=[pk(bnk)])
                        S.op('dve', lambda e, oc=oc, bnk=bnk: e.scalar_tensor_tensor(out=xT[:, oc, bs], in0=ps[bnk][:, :], scalar=modv[:, l, 16 + oc:17 + oc], in1=xT[:, oc, bs],
                                                                                     op0=ALU.mult, op1=ALU.add), reads=[pk(bnk), xk(oc, b), ('modv', l)], writes=[xk(oc, b)])
                S.barrier()
            if with_peer:
                peer_layer(nc, S, sb, ps, psn, pk, xk, l, T, NT, NB, locals())

        with contextlib.ExitStack() as ph:
            wkf = [sb('fwk%d' % i, [128, 512], stack=ph) for i in range(4)]
            osb = [sb('osb%d' % i, [128, D], stack=ph) for i in range(2)]
            toks = []
            wi = 0
            for b in range(NB):
                bs = slice(b * 512, (b + 1) * 512)
                sbank = psn()
                for k in range(8):
                    w_ = wkf[wi % 2]
                    wkey_ = ('fwk', wi % 2)
                    wi += 1
                    S.op('act', lambda e, k=k, w_=w_: e.activation(out=w_[:], in_=xT[:, k, bs], func=AF.Square), reads=[xk(k, b)], writes=[wkey_])
                    S.op('pe', lambda e, k=k, w_=w_: e.matmul(ps[sbank][:, :], lhsT=ones_f[:], rhs=w_[:], start=(k == 0), stop=(k == 7)),
                         reads=[wkey_, 'ones_f'], writes=[pk(sbank)])
                rst = wkf[2]
                S.op('act', lambda e: e.activation(out=rst[:], in_=ps[sbank][:, :], func=AF.Ln, bias=eps_t[:], scale=1.0 / D),
                     reads=[pk(sbank), 'eps'], writes=['frst'])
                S.op('act', lambda e: e.activation(out=rst[:], in_=rst[:], func=AF.Exp, scale=-0.5), reads=['frst'], writes=['frst'])
                for k in range(8):
                    S.op('dve', lambda e, k=k: e.scalar_tensor_tensor(out=xT[:, k, bs], in0=xT[:, k, bs], scalar=fing_t[:, k:k + 1], in1=rst[:],
                                                                      op0=ALU.mult, op1=ALU.mult), reads=[xk(k, b), 'frst', 'fing'], writes=[xk(k, b)])
                for i in range(4):
                    tt = b * 4 + i
                    ob = osb[tt % 2]
                    okey = ('osb', tt % 2)
                    for half in range(2):
                        bnk = psn()
                        for j in range(4):
                            k = half * 4 + j
                            S.op('pe', lambda e, bnk=bnk, j=j, k=k, tt=tt: e.transpose(out=ps[bnk][:, j * 128:(j + 1) * 128], in_=xT[:, k, tt * 128:(tt + 1) * 128],
                                                                                    identity=ident_f[:]), reads=[xk(k, b), 'ident_f'], writes=[pk(bnk)])
                        if half == 0:
                            S.op('act', lambda e, bnk=bnk, ob=ob: e.copy(out=ob[:, 0:512], in_=ps[bnk][:, :]), reads=[pk(bnk)], writes=[okey + (0,)])
                        else:
                            S.op('dve', lambda e, bnk=bnk, ob=ob: e.tensor_copy(out=ob[:, 512:1024], in_=ps[bnk][:, :]), reads=[pk(bnk)], writes=[okey + (1,)])
                    toks.append(S.dma(out_d[tt * 128:(tt + 1) * 128, :], ob[:], reads=[okey + (0,), okey + (1,)], writes=[('out', tt)]))
            S.wait_all('sp', toks)
    print('instructions emitted:', S.nops)
    return nc


def peer_layer(nc, S, sb, ps, psn, pk, xk, l, T, NT, NB, env):
    xT, modv, gs2, ones_f, eps_t, ident_f, iota_f, iota_rep = (env[k] for k in ('xT', 'modv', 'gs2', 'ones_f', 'eps_t', 'ident_f', 'iota_f', 'iota_rep'))
    wq, keysT, uT, vr, h2d, Gd = (env[k] for k in ('wq', 'keysT', 'uT', 'vr', 'h2d', 'Gd'))
    ALLB = tuple(range(8))
    h2dv = h2d.rearrange("p (k t) -> p k t", k=8)
    with contextlib.ExitStack() as ph:
        keyt = sb('keyt', [128, 16, 128], stack=ph)
        S.dma(keyt[:], keysT[l].rearrange("d (g n) -> d g n", g=16), writes=['keyt'])
        h2f = sb('h2f', [128, 8, 512], stack=ph)
        rst2 = sb('rst2', [128, 512], stack=ph)
        sqw = [sb('sqw%d' % i, [128, 512], stack=ph) for i in range(2)]
        h2b = [sb('h2b%d' % i, [128, 512], BF16, stack=ph) for i in range(2)]
        qpT = [sb('qpT%d' % i, [128, 16, 256], stack=ph) for i in range(1)]
        wqs = [sb('wqs%d' % i, [128, 8, 128], stack=ph) for i in range(2)]
        sc0s = [sb('sc0_%d' % i, [128, 16, 128], stack=ph) for i in range(2)]
        tv = sb('tv', [128, 16, 16], stack=ph)
        ti = sb('ti', [128, 16, 16], U32, stack=ph)
        tif = sb('tif', [128, 16, 16], stack=ph)
        cand = sb('cand', [128, 8, 256], stack=ph)
        tsv = sb('tsv', [128, 8, 16], stack=ph)
        tcv = sb('tcv', [128, 8, 16], U32, stack=ph)
        tab = sb('tab', [128, 2, 128], U32, stack=ph)
        tabf = sb('tabf', [128, 2, 128], stack=ph)
        i12g = sb('i12g', [128, 3, 128], stack=ph)
        zs = sb('zs', [128, 8], stack=ph)
        sT = sb('sT', [128, 3, 128], stack=ph)
        PT = [sb('PT%d' % i, [128, 128, 8], BF16, stack=ph) for i in range(3)]
        QT = [sb('QT%d' % i, [128, 128, 8], BF16, stack=ph) for i in range(3)]
        sTb = sb('sTb', [128, 2, 128], BF16, stack=ph)
        ohi = [0]
        GT = sb('GT', [128, 128, 128], BF16, stack=ph)
        tvv = tv[:].rearrange("p (h w) k -> p h w k", w=2)
        tifv = tif[:].rearrange("p (h w) k -> p h w k", w=2)
        cand4 = cand[:].rearrange("p h (a b) -> p h a b", a=16)
        wqc = [0]

        def emit_n2(b):
            bs = slice(b * 512, (b + 1) * 512)
            sbank = psn(ALLB)
            for k in range(8):
                w_ = sqw[k % 2]
                wkey_ = ('sqw', k % 2)
                S.op('act', lambda e, k=k, w_=w_: e.activation(out=w_[:], in_=xT[:, k, bs], func=AF.Square), reads=[xk(k, b)], writes=[wkey_])
                S.op('pe', lambda e, k=k, w_=w_: e.matmul(ps[sbank][:, :], lhsT=ones_f[:], rhs=w_[:], start=(k == 0), stop=(k == 7)),
                     reads=[wkey_, 'ones_f'], writes=[pk(sbank)])
            S.op('act', lambda e: e.activation(out=rst2[:], in_=ps[sbank][:, :], func=AF.Ln, bias=eps_t[:], scale=1.0 / D),
                 reads=[pk(sbank), 'eps'], writes=['rst2'])
            S.op('act', lambda e: e.activation(out=rst2[:], in_=rst2[:], func=AF.Exp, scale=-0.5), reads=['rst2'], writes=['rst2'])
            for k in range(8):
                w_ = sqw[k % 2]
                wkey_ = ('sqw', k % 2)
                hb_ = h2b[k % 2]
                hkey = ('h2b', k % 2)
                S.op('dve', lambda e, k=k, w_=w_: e.scalar_tensor_tensor(out=w_[:], in0=xT[:, k, bs], scalar=gs2[:, l, k:k + 1], in1=rst2[:],
                                                                         op0=ALU.mult, op1=ALU.mult), reads=[xk(k, b), 'rst2', ('gs2', l)], writes=[wkey_])
                S.op('act', lambda e, k=k, w_=w_: e.activation(out=h2f[:, k, :], in_=w_[:], func=AF.Identity, bias=modv[:, l, 24 + k:25 + k], scale=1.0),
                     reads=[wkey_, ('modv', l)], writes=[('h2f', k)])
                S.op('pool', lambda e, k=k, hb_=hb_: e.tensor_copy(out=hb_[:], in_=h2f[:, k, :]), reads=[('h2f', k)], writes=[hkey])
                S.dma(h2dv[:, k, bs], hb_[:], reads=[hkey], writes=[('h2d', k, b)])

        def emit_qproj(b, half, qi, glist=None):
            hs = slice(half * 256, (half + 1) * 256)
            for g in (glist if glist is not None else range(16)):
                wt = wqs[wqc[0] % 2]
                wkey = ('wqs', wqc[0] % 2)
                wqc[0] += 1
                S.dma(wt[:], wq[l, :, g * 128:(g + 1) * 128].rearrange("(k p) n -> p k n", p=128), writes=[wkey])
                bnk = psn(ALLB)
                for k in range(8):
                    S.op('pe', lambda e, k=k, wt=wt, bnk=bnk: e.matmul(ps[bnk][:, 0:256], lhsT=wt[:, k, :], rhs=h2f[:, k, hs], start=(k == 0), stop=(k == 7)),
                         reads=[wkey, ('h2f', k)], writes=[pk(bnk)])
                S.op('act', lambda e, g=g, bnk=bnk: e.copy(out=qpT[qi][:, g, :], in_=ps[bnk][:, 0:256]), reads=[pk(bnk)], writes=[('qpT', qi, g)])

        def emit_scores(tl, qi):
            tsl = slice(tl * 128, (tl + 1) * 128)
            for gq in range(4):
                bnk = psn(ALLB)
                for gg in range(4):
                    g = gq * 4 + gg
                    S.op('pe', lambda e, g=g, gg=gg, bnk=bnk: e.matmul(ps[bnk][:, gg * 128:(gg + 1) * 128], lhsT=qpT[qi][:, g, tsl], rhs=keyt[:, g, :],
                                                                       start=True, stop=True), reads=[('qpT', qi, g), 'keyt'], writes=[pk(bnk)])
                S.op('act', lambda e, gq=gq, bnk=bnk: e.copy(out=sc0s[tl][:, gq * 4:(gq + 1) * 4, :], in_=ps[bnk][:, :].rearrange("p (g n) -> p g n", g=4)),
                     reads=[pk(bnk)], writes=[('sc0', tl, gq * 4 + j) for j in range(4)])

        def part1a(tl):
            yield
            for g in range(16):
                S.op('dve', lambda e, g=g: e.max(out=tv[:, g, 0:8], in_=sc0s[tl][:, g, :]), reads=[('sc0', tl, g)], writes=[('tv', g)])
            yield
            for g in range(16):
                S.op('dve', lambda e, g=g: e.max_index(out=ti[:, g, 0:8], in_max=tv[:, g, 0:8], in_values=sc0s[tl][:, g, :]),
                     reads=[('sc0', tl, g), ('tv', g)], writes=[('ti', g)])
            yield
            for g in range(16):
                S.op('dve', lambda e, g=g: e.match_replace(out=sc0s[tl][:, g, :], in_to_replace=tv[:, g, 0:8], in_values=sc0s[tl][:, g, :], imm_value=NEG),
                     reads=[('sc0', tl, g), ('tv', g)], writes=[('sc0', tl, g)])
            yield
            for g in range(16):
                S.op('dve', lambda e, g=g: e.max(out=tv[:, g, 8:16], in_=sc0s[tl][:, g, :]), reads=[('sc0', tl, g)], writes=[('tv', g)])
            yield
            for g in range(16):
                S.op('dve', lambda e, g=g: e.max_index(out=ti[:, g, 8:16], in_max=tv[:, g, 8:16], in_values=sc0s[tl][:, g, :]),
                     reads=[('sc0', tl, g), ('tv', g)], writes=[('ti', g)])
            tvk = [('tv', g) for g in range(16)]
            tik = [('ti', g) for g in range(16)]
            S.op('dve', lambda e: e.tensor_tensor(out=cand4, in0=tvv[:, :, 0, :].unsqueeze(3).to_broadcast([128, 8, 16, 16]),
                                                  in1=tvv[:, :, 1, :].unsqueeze(2).to_broadcast([128, 8, 16, 16]), op=ALU.add),
                 reads=tvk, writes=[('cand', h) for h in range(8)])
            yield
            for h in range(8):
                S.op('dve', lambda e, h=h: e.max(out=tsv[:, h, 0:8], in_=cand[:, h, :]), reads=[('cand', h)], writes=[('tsv', h)])
            yield
            for h in range(8):
                S.op('dve', lambda e, h=h: e.max_index(out=tcv[:, h, 0:8], in_max=tsv[:, h, 0:8], in_values=cand[:, h, :]),
                     reads=[('cand', h), ('tsv', h)], writes=[('tcv', h)])
            yield
            for h in range(8):
                S.op('dve', lambda e, h=h: e.match_replace(out=cand[:, h, :], in_to_replace=tsv[:, h, 0:8], in_values=cand[:, h, :], imm_value=NEG),
                     reads=[('cand', h), ('tsv', h)], writes=[('cand', h)])
            yield
            for h in range(8):
                S.op('dve', lambda e, h=h: e.max(out=tsv[:, h, 8:16], in_=cand[:, h, :]), reads=[('cand', h)], writes=[('tsv', h)])
            yield
            for h in range(8):
                S.op('dve', lambda e, h=h: e.max_index(out=tcv[:, h, 8:16], in_max=tsv[:, h, 8:16], in_values=cand[:, h, :]),
                     reads=[('cand', h), ('tsv', h)], writes=[('tcv', h)])
            tsk = [('tsv', h) for h in range(8)]
            tck = [('tcv', h) for h in range(8)]
            ck = [('cand', h) for h in range(8)]
            g3 = i12g[:, 2, :].rearrange("p (h k) -> p h k", h=8)
            S.op('dve', lambda e: e.tensor_tensor(out=g3, in0=tsv[:], in1=tsv[:, :, 0:1].to_broadcast([128, 8, 16]), op=ALU.subtract),
                 reads=tsk, writes=['gate'])
            S.op('act', lambda e: e.activation(out=g3, in_=g3, func=AF.Exp), reads=['gate'], writes=['gate'])
            S.op('dve', lambda e: e.tensor_reduce(out=zs[:], in_=g3, axis=AX.X, op=ALU.add), reads=['gate'], writes=['zs'])
            S.op('dve', lambda e: e.reciprocal(out=zs[:], in_=zs[:]), reads=['zs'], writes=['zs'])
            S.op('dve', lambda e: e.tensor_tensor(out=g3, in0=g3, in1=zs[:].unsqueeze(2).to_broadcast([128, 8, 16]), op=ALU.mult),
                 reads=['gate', 'zs'], writes=['gate'])
            tcf = tcv[:].rearrange("p h k -> p (h k)")
            S.op('dve', lambda e: e.tensor_single_scalar(out=tab[:, 0, :], in_=tcf, scalar=4, op=ALU.logical_shift_right), reads=tck, writes=['tab0'])
            S.op('dve', lambda e: e.tensor_single_scalar(out=tab[:, 1, :], in_=tcf, scalar=15, op=ALU.bitwise_and), reads=tck, writes=['tab1'])
            S.op('dve', lambda e: e.tensor_copy(out=tabf[:], in_=tab[:]), reads=['tab0', 'tab1'], writes=['tabf'])
            S.op('dve', lambda e: e.tensor_copy(out=tif[:], in_=ti[:]), reads=tik, writes=['tif'])
            yield
            for w in range(2):
                af = tabf[:, w, :].rearrange("p (h k) -> p h k", h=8)
                S.op('dve', lambda e, af=af: e.tensor_tensor(out=cand4, in0=af.unsqueeze(3).to_broadcast([128, 8, 16, 16]),
                                                            in1=iota_f[:, 0:16].unsqueeze(1).unsqueeze(1).to_broadcast([128, 8, 16, 16]), op=ALU.is_equal),
                     reads=['tabf', 'iota_f'], writes=ck)
                S.op('dve', lambda e, w=w: e.tensor_tensor(out=cand4, in0=cand4, in1=tifv[:, :, w, :].unsqueeze(2).to_broadcast([128, 8, 16, 16]), op=ALU.mult),
                     reads=ck + ['tif'], writes=ck)
                S.op('dve', lambda e, w=w: e.tensor_reduce(out=i12g[:, w, :].rearrange("p (h k) -> p h k", h=8), in_=cand4, axis=AX.X, op=ALU.add),
                     reads=ck, writes=['i12_%d' % w])
            yield

        def part1b():
            bnk = psn(ALLB)
            for w in range(3):
                S.op('pe', lambda e, w=w, bnk=bnk: e.transpose(out=ps[bnk][:, w * 128:(w + 1) * 128], in_=i12g[:, w, :], identity=ident_f[:]),
                     reads=['i12_0', 'i12_1', 'gate', 'ident_f'], writes=[pk(bnk)])
            S.op('act', lambda e, bnk=bnk: e.copy(out=sT[:], in_=ps[bnk][:, 0:384].rearrange("p (w t) -> p w t", w=3)), reads=[pk(bnk)], writes=['sT'])
            S.op('act', lambda e, bnk=bnk: e.copy(out=sTb[:], in_=ps[bnk][:, 0:256].rearrange("p (w t) -> p w t", w=2)), reads=[pk(bnk)], writes=['sTb'])

        def part2(tt, after_group=None):
            iota_bc = iota_f[:].unsqueeze(1).to_broadcast([128, 8, 128])
            iota_bc4 = iota_f[:].unsqueeze(1).to_broadcast([128, 4, 128])

            def onehots(t8):
                t0 = t8 * 8
                bf = ohi[0] % 3
                ohi[0] += 1
                pkey, qkey = ('PT', bf), ('QT', bf)
                S.op('dve', lambda e: e.tensor_tensor(out=PT[bf][:], in0=iota_rep[:], in1=sTb[:, 0, t0:t0 + 8].unsqueeze(1).to_broadcast([128, 128, 8]),
                                                      op=ALU.is_equal), reads=['sTb', 'iota_rep'], writes=[pkey])
                S.op('pool', lambda e: e.tensor_tensor(out=PT[bf][:], in0=PT[bf][:], in1=sT[:, 2, t0:t0 + 8].unsqueeze(1).to_broadcast([128, 128, 8]),
                                                       op=ALU.mult), reads=['sT', pkey], writes=[pkey])
                S.op('dve', lambda e: e.tensor_tensor(out=QT[bf][:], in0=iota_rep[:], in1=sTb[:, 1, t0:t0 + 8].unsqueeze(1).to_broadcast([128, 128, 8]),
                                                      op=ALU.is_equal), reads=['sTb', 'iota_rep'], writes=[(qkey, 0), (qkey, 1)])
                return bf

            bfs = {0: onehots(0)}
            for t8 in range(16):
                t0 = t8 * 8
                if t8 + 1 < 16:
                    bfs[t8 + 1] = onehots(t8 + 1)
                bf = bfs[t8]
                pkey, qkey = ('PT', bf), ('QT', bf)
                for hq in range(2):
                    bnk = psn(ALLB)
                    for q in range(4):
                        sl = hq * 4 + q
                        S.op('pe', lambda e, q=q, bf=bf, sl=sl, bnk=bnk: e.matmul(ps[bnk][:, :].rearrange("p (j q) -> p q j", q=4)[:, q, :], lhsT=PT[bf][:, :, sl],
                                                                               rhs=QT[bf][:, :, sl], start=True, stop=True), reads=[pkey, (qkey, hq)], writes=[pk(bnk)])
                    tq = t0 + hq * 4
                    dst = GT[:, :, tq:tq + 4]
                    src = ps[bnk][:, :].rearrange("p (j q) -> p j q", q=4)
                    S.op('act', lambda e, dst=dst, src=src: e.copy(out=dst, in_=src), reads=[pk(bnk)], writes=[('GT', tq // 4)])
                if after_group is not None:
                    after_group(t8)
            S.dma(Gd[tt], GT[:].rearrange("p j t -> p (j t)"), reads=[('GT', q) for q in range(32)], writes=[('Gd', tt)])

        def exhaust(g):
            if g is not None:
                for _ in g:
                    pass

        def qproj_gen(nb_, nh_):
            for g in range(16):
                emit_qproj(nb_, nh_, 0, glist=[g])
                yield

        def chain(*gens):
            for g in gens:
                if g is not None:
                    for _ in g:
                        yield

        def stepper(g, n):
            def f(t8):
                for _ in range(n):
                    try:
                        next(g)
                    except StopIteration:
                        return
            return f

        seq = [(b, half) for b in range(NB) for half in range(2)]
        emit_n2(0)
        emit_qproj(0, 0, 0)
        emit_scores(0, 0)
        emit_scores(1, 0)
        exhaust(part1a(0))
        for idx, (b, half) in enumerate(seq):
            nxt = seq[idx + 1] if idx + 1 < len(seq) else None
            part1b()
            if nxt is not None and nxt[1] == 0:
                emit_n2(nxt[0])
            bg = chain(part1a(1), qproj_gen(*nxt) if nxt is not None else None)
            part2(b * 4 + half * 2 + 0, stepper(bg, 2))
            exhaust(bg)
            part1b()
            bg = None
            if nxt is not None:
                emit_scores(0, 0)
                emit_scores(1, 0)
                bg = part1a(0)
            part2(b * 4 + half * 2 + 1, stepper(bg, 1) if bg is not None else None)
            exhaust(bg)
        S.barrier()
    with contextlib.ExitStack() as ph:
        h2a = sb('h2a', [128, 8, T], BF16, stack=ph)
        for k in range(8):
            S.dma(h2a[:, k, :], h2dv[:, k, :], writes=[('h2a', k)])
        h2k = [('h2a', k) for k in range(8)]
        US = [sb('US%d' % i, [128, 8, 1024], BF16, stack=ph) for i in range(2)]
        VS = [sb('VS%d' % i, [128, 8, 1024], BF16, stack=ph) for i in range(2)]
        stg = [sb('stg%d' % i, [128, 1024], stack=ph) for i in range(4)]
        Gt = [sb('Gt%d' % i, [128, 8, 2, 128], BF16, stack=ph) for i in range(3)]
        Wt = [sb('Wt%d' % i, [128, 8, 256], BF16, stack=ph) for i in range(2)]
        ge = [sb('ge%d' % i, [128, 256], BF16, stack=ph) for i in range(3)]
        NS = 16
        NG = T // 256
        sti = [0]

        def load_uv(Sx, jj):
            slot = Sx % 2
            j = Sx * 8 + jj
            for which, src, dstb, eng in (('U', uT, US, 'act'), ('V', vr, VS, 'pool')):
                i = sti[0] % 4
                sti[0] += 1
                S.dma(stg[i][:], src[l, j], writes=[('stg', i)])
                if eng == 'act':
                    S.op('act', lambda e, i=i, dstb=dstb: e.copy(out=dstb[slot][:, jj, :], in_=stg[i][:]), reads=[('stg', i)], writes=[(which, slot, jj)])
                else:
                    S.op('pool', lambda e, i=i, dstb=dstb: e.tensor_copy(out=dstb[slot][:, jj, :], in_=stg[i][:]), reads=[('stg', i)], writes=[(which, slot, jj)])

        def load_g(it):
            Sx, tg = divmod(it, NG)
            gt = Gt[it % 3]
            for hh in range(2):
                tt = tg * 2 + hh
                S.dma(gt[:, :, hh, :], Gd[tt].rearrange("p (j t) -> p j t", j=128)[:, Sx * 8:(Sx + 1) * 8, :], writes=[('Gt', it % 3, hh)])

        for jj in range(8):
            load_uv(0, jj)
        load_g(0)
        if NS * NG > 1:
            load_g(1)
        for it in range(NS * NG):
            Sx, tg = divmod(it, NG)
            slot = Sx % 2
            if it + 2 < NS * NG:
                load_g(it + 2)
            gt = Gt[it % 3]
            wt = Wt[it % 2]
            tsl = slice(tg * 256, (tg + 1) * 256)
            for jj in range(8):
                bnk = psn(ALLB)
                for dk in range(8):
                    S.op('pe', lambda e, dk=dk, jj=jj, bnk=bnk: e.matmul(ps[bnk][:, 0:256], lhsT=US[slot][:, jj, dk * 128:(dk + 1) * 128], rhs=h2a[:, dk, tsl],
                                                                       start=(dk == 0), stop=(dk == 7)), reads=[('U', slot, jj), ('h2a', dk)], writes=[pk(bnk)])
                gi_ = (it * 8 + jj) % 3
                S.op('act', lambda e, bnk=bnk, gi_=gi_: e.activation(out=ge[gi_][:], in_=ps[bnk][:, 0:256], func=AF.Gelu), reads=[pk(bnk)], writes=[('ge', gi_)])
                eng = 'dve' if jj % 2 == 0 else 'pool'
                S.op(eng, lambda e, jj=jj, gi_=gi_, gt=gt, wt=wt: e.tensor_tensor(out=wt[:, jj, :], in0=ge[gi_][:], in1=gt[:, jj].rearrange("p h t -> p (h t)"), op=ALU.mult),
                     reads=[('ge', gi_), ('Gt', it % 3, 0), ('Gt', it % 3, 1)], writes=[('Wt', it % 2, jj)])
            for dk in range(8):
                bnk = psn(ALLB)
                for jj in range(8):
                    S.op('pe', lambda e, dk=dk, jj=jj, bnk=bnk, wt=wt: e.matmul(ps[bnk][:, 0:256], lhsT=VS[slot][:, jj, dk * 128:(dk + 1) * 128], rhs=wt[:, jj, :],
                                                                              start=(jj == 0), stop=(jj == 7)), reads=[('V', slot, jj), ('Wt', it % 2, jj)], writes=[pk(bnk)])
                S.op('dve', lambda e, dk=dk, bnk=bnk: e.scalar_tensor_tensor(out=xT[:, dk, tsl], in0=ps[bnk][:, 0:256], scalar=modv[:, l, 40 + dk:41 + dk], in1=xT[:, dk, tsl],
                                                                             op0=ALU.mult, op1=ALU.add), reads=[pk(bnk), xk(dk, tg // 2), ('modv', l)], writes=[xk(dk, tg // 2)])
            if Sx + 1 < NS and tg < 8:
                load_uv(Sx + 1, tg)
            if Sx + 1 < NS and NG < 8 and tg == NG - 1:
                for jj in range(NG, 8):
                    load_uv(Sx + 1, jj)
        S.barrier()


def prep_inputs(inp, T, L, b):
    f = np.float32

    def fm(v):
        return np.ascontiguousarray(v.reshape(v.shape[:-1] + (8, 128)).swapaxes(-1, -2))

    m = {}
    m['x'] = np.ascontiguousarray(inp['x'][b, :T])
    m['c_fm'] = fm(inp['c'][b])
    m['rel_bias'] = np.ascontiguousarray(inp['rel_bias'].reshape(1, 128))
    m['w_mod'] = inp['w_mod'][:L]
    bm = inp['b_mod'][:L].reshape(L, 6, 8, 128)
    m['b_mod_fm'] = np.ascontiguousarray(bm.transpose(0, 3, 1, 2).reshape(L, 128, 48))
    m['n1g_fm'] = fm(inp['norm1_g'][:L])
    m['n2g_fm'] = fm(inp['norm2_g'][:L])
    m['fing_fm'] = fm(inp['final_g'])
    m['w_in'] = inp['w_in'][:L]
    m['w_out'] = inp['w_out'][:L]
    m['diff_lambda'] = np.ascontiguousarray(inp['diff_lambda'][:L].reshape(L, 128))
    m['subln_g'] = inp['subln_g'][:L]
    cd = inp['conf_dw'][:L].reshape(L, 31, 2, 128)
    m['conf_dw_fm'] = np.ascontiguousarray(cd.transpose(0, 3, 2, 1).reshape(L, 128, 62))
    m['conf_lng_fm'] = np.ascontiguousarray(inp['conf_ln_g'][:L].reshape(L, 2, 128).transpose(0, 2, 1))
    m['conf_lnb_fm'] = np.ascontiguousarray(inp['conf_ln_b'][:L].reshape(L, 2, 128).transpose(0, 2, 1))
    sc = inp['sconv_w'][:L].reshape(L, 3, 2, 128)
    m['sconv_fm'] = np.ascontiguousarray(sc.transpose(0, 3, 2, 1).reshape(L, 128, 6))
    m['sgu_ln_g'] = inp['sgu_ln_g'][:L]
    m['sgu_ln_b'] = inp['sgu_ln_b'][:L]
    m['sgu_wT'] = np.ascontiguousarray(inp['sgu_w'][:L].transpose(0, 3, 1, 2).reshape(L, 128, 512))
    m['sgu_b'] = np.ascontiguousarray(inp['sgu_b'][:L].reshape(L, 512))
    m['peer_wq'] = inp['peer_wq'][:L]
    kk = inp['peer_keys'][:L].reshape(L, 16, 128, 128)
    m['peer_keysT'] = np.ascontiguousarray(kk.transpose(0, 3, 1, 2).reshape(L, 128, 2048))
    return m


_CONST = {}


def consts():
    if _CONST:
        return _CONST
    f = np.float32
    c = {}
    c['ident'] = np.eye(128, dtype=f)
    s = np.arange(128)
    c['trilT'] = (s[:, None] <= s[None, :]).astype(f)
    p = np.arange(128)
    c['maskq'] = np.stack([np.where((p % 64) < 32, 32 ** -0.5, 0.0), np.where((p % 64) >= 32, 32 ** -0.5, 0.0)], axis=1).astype(f)
    c['iota'] = np.broadcast_to(np.arange(128, dtype=f)[None, :], (128, 128)).copy()
    kj = np.arange(128)[:, None, None]
    qi = np.arange(128)[None, None, :]
    slot = np.arange(2)[None, :, None]
    n = (1 - slot) * 128 + qi - kj
    nn = np.maximum(n, 0)
    ratio = np.log(np.maximum(nn, 1).astype(np.float32) / 16) / np.float32(math.log(128 / 16))
    large = np.minimum(16 + (ratio * 16).astype(np.int32), 31)
    bucket = np.where(nn < 16, nn, large)
    c['bk'] = bucket.astype(f).reshape(128, 256)
    c['mk'] = np.where(n >= 0, 0.0, NEG).astype(f).reshape(128, 256)
    _CONST.update(c)
    return c


_SHARED = {}


def kernel(**inputs):
    T, L = 2048, 4
    inp = {k: np.asarray(v) for k, v in inputs.items()}
    nc = build(T, L)
    cst = consts()
    in_maps = []
    shared = None
    for b in range(8):
        m = prep_inputs(inp, T, L, b) if shared is None else None
        if shared is None:
            shared = {k: v for k, v in m.items() if k not in ('x', 'c_fm')}
            shared.update(cst)
            shared.update(prep_peer_tables(inp, L))
        mm = dict(shared)
        mm['x'] = np.ascontiguousarray(inp['x'][b, :T])
        mm['c_fm'] = np.ascontiguousarray(inp['c'][b].reshape(8, 128).T)
        in_maps.append(mm)
    res = run_bass_kernel_spmd(nc, in_maps, core_ids=list(range(8)))
    return np.stack([r['out'] for r in res.results], axis=0).astype(np.float32)


def prep_peer_tables(inp, L):
    u = inp['peer_u'][:L].reshape(L, 128, 128, 8, 128)
    v = inp['peer_v'][:L].reshape(L, 128, 128, 1024)
    return {
        'peer_uT': np.ascontiguousarray(u.transpose(0, 2, 4, 3, 1).reshape(L, 128, 128, 1024)),
        'peer_vr': np.ascontiguousarray(v.transpose(0, 2, 1, 3)),
    }
```

```python
import contextlib
import math
import numpy as np
import concourse.bass as bass
import concourse.mybir as mybir
from concourse.bass_utils import run_bass_kernel_spmd

F32 = mybir.dt.float32
BF16 = mybir.dt.bfloat16
U32 = mybir.dt.uint32
AF = mybir.ActivationFunctionType
ALU = mybir.AluOpType
AX = mybir.AxisListType

D = 1024
EPS = 1e-6
NEG = -1.0e30
NDMASEM = 8


class Sched:
    ENG = ('pe', 'dve', 'act', 'pool', 'sp')

    def __init__(self, nc, st):
        self.nc = nc
        self.e = {'pe': nc.tensor, 'dve': nc.vector, 'act': nc.scalar, 'pool': nc.gpsimd, 'sp': nc.sync}
        self.sem = {}
        for e in ('pe', 'dve', 'act', 'pool'):
            self.sem[e] = st.enter_context(nc.semaphore('s_' + e))
        for q in ('sp', 'pool'):
            for j in range(NDMASEM):
                self.sem[('dma', q, j)] = st.enter_context(nc.semaphore('d_%s%d' % (q, j)))
        self.cnt = {e: 0 for e in self.ENG}
        self.dcnt = {'sp': 0, 'pool': 0}
        self.waited = {e: {} for e in self.ENG}
        self.last_w = {}
        self.readers = {}
        self.nops = 0
        self.psi = 0

    def _deps(self, reads, writes):
        deps = set()
        for r in reads:
            lw = self.last_w.get(r)
            if lw is not None:
                deps.add(lw)
        for w in writes:
            lw = self.last_w.get(w)
            if lw is not None:
                deps.add(lw)
            rd = self.readers.get(w)
            if rd:
                deps.update(rd.values())
        return deps

    def _emit_waits(self, eng, deps):
        w = self.waited[eng]
        eo = self.e[eng]
        for (k, v) in deps:
            if k == 'pe' and eng == 'pe':
                continue
            if w.get(k, 0) >= v:
                continue
            w[k] = v
            eo.wait_ge(self.sem[k], v)

    def _record(self, tok, reads, writes):
        k = tok[0]
        for r in reads:
            self.readers.setdefault(r, {})[k] = tok
        for wr in writes:
            self.last_w[wr] = tok
            self.readers[wr] = {}

    def op(self, eng, fn, reads=(), writes=()):
        deps = self._deps(reads, writes)
        self._emit_waits(eng, deps)
        self.cnt[eng] += 1
        tok = (eng, self.cnt[eng])
        fn(self.e[eng]).then_inc(self.sem[eng], 1)
        self._record(tok, reads, writes)
        self.nops += 1
        return tok

    def dma(self, out, in_, reads=(), writes=(), q='sp', **kw):
        deps = self._deps(reads, writes)
        i = self.dcnt[q]
        self.dcnt[q] += 1
        key = ('dma', q, i % NDMASEM)
        val = 16 * (i // NDMASEM + 1)
        if i >= NDMASEM:
            deps.add((key, val - 16))
        self._emit_waits(q, deps)
        self.e[q].dma_start(out=out, in_=in_, **kw).then_inc(self.sem[key], 16)
        tok = (key, val)
        self._record(tok, reads, writes)
        self.nops += 1
        return tok

    def barrier(self):
        toks = set(self.last_w.values())
        for rd in self.readers.values():
            toks.update(rd.values())
        mx = {}
        for (k, v) in toks:
            mx[k] = max(mx.get(k, 0), v)
        for eng in self.ENG:
            self._emit_waits(eng, set(mx.items()))
        self.last_w = {}
        self.readers = {}

    def wait_all(self, eng, toks):
        self._emit_waits(eng, set(toks))


def build(T, L, with_peer=True, dbg=False):
    nc = bass.Bass("TRN2", target_bir_lowering=False)
    NT = T // 128
    NB = T // 512

    def din(name, shape, dt=F32):
        return nc.dram_tensor(name, list(shape), dt, kind="ExternalInput").ap()

    x_d = din('x', [T, D])
    c_fm = din('c_fm', [128, 8])
    relb = din('rel_bias', [1, 128])
    w_mod = din('w_mod', [L, D, 6 * D])
    b_mod = din('b_mod_fm', [L, 128, 48])
    n1g = din('n1g_fm', [L, 128, 8])
    n2g = din('n2g_fm', [L, 128, 8])
    fing = din('fing_fm', [128, 8])
    w_in = din('w_in', [L, D, 2560])
    w_out = din('w_out', [L, D, D])
    dlam = din('diff_lambda', [L, 128])
    sublg = din('subln_g', [L, 64])
    cdw = din('conf_dw_fm', [L, 128, 2 * 31])
    clng = din('conf_lng_fm', [L, 128, 2])
    clnb = din('conf_lnb_fm', [L, 128, 2])
    scw = din('sconv_fm', [L, 128, 2 * 3])
    slng = din('sgu_ln_g', [L, 256])
    slnb = din('sgu_ln_b', [L, 256])
    sguwT = din('sgu_wT', [L, 128, 4 * 128])
    sgub = din('sgu_b', [L, 512])
    wq = din('peer_wq', [L, D, 2048])
    keysT = din('peer_keysT', [L, 128, 16 * 128])
    uT = din('peer_uT', [L, 128, 128, 1024])
    vr = din('peer_vr', [L, 128, 128, 1024])
    ident_d = din('ident', [128, 128])
    trilT_d = din('trilT', [128, 128])
    bk_d = din('bk', [128, 256])
    mk_d = din('mk', [128, 256])
    maskq_d = din('maskq', [128, 2])
    iota_d = din('iota', [128, 128])
    out_d = nc.dram_tensor('out', [T, D], F32, kind="ExternalOutput").ap()
    dbg_d = nc.dram_tensor('dbg', [T, D], F32, kind="ExternalOutput").ap() if dbg else None

    wind = nc.dram_tensor('wind', [L, 128, 8 * 2560], BF16, kind="Internal").ap()
    woutd = nc.dram_tensor('woutd', [L, 128, 8 * 1024], BF16, kind="Internal").ap()
    h2d = nc.dram_tensor('h2d', [128, 8 * T], BF16, kind="Internal").ap()
    Gd = nc.dram_tensor('Gd', [NT, 128, 128 * 128], BF16, kind="Internal").ap()

    with contextlib.ExitStack() as st:
        S = Sched(nc, st)

        sbn = [0]

        def sb(name, shape, dt=F32, stack=None):
            sbn[0] += 1
            return (stack or st).enter_context(nc.sbuf_tensor('sb%d_%s' % (sbn[0], name), list(shape), dt))

        xT = sb('xT', [128, 8, T])
        ident_f = sb('ident_f', [128, 128])
        ident_b = sb('ident_b', [128, 128], BF16)
        ones_f = sb('ones_f', [128, 128])
        ones_b = sb('ones_b', [128, 128], BF16)
        eps_t = sb('eps_t', [128, 1])
        modv = sb('modv', [128, L, 48])
        gs1 = sb('gs1', [128, L, 8])
        gs2 = sb('gs2', [128, L, 8])
        TB = sb('TB', [128, 4, 256])
        rb_bc = sb('rb_bc', [128, 128])
        trilT = sb('trilT', [128, 128])
        iota_f = sb('iota_f', [128, 128])
        fing_t = sb('fing_t', [128, 8])
        maskq = sb('maskq', [128, 2])
        iota_rep = sb('iota_rep', [128, 128, 8], BF16)
        ps = [st.enter_context(nc.psum_tensor('ps%d' % i, [128, 512], F32)) for i in range(8)]

        def xk(k, b):
            return ('xT', k, b)

        def psn(pool=(0, 1, 2, 3, 4, 5)):
            S.psi += 1
            return pool[S.psi % len(pool)]

        def pk(i):
            return ('ps', i)

        with contextlib.ExitStack() as ph:
            S.dma(ident_f[:], ident_d, writes=['ident_f'])
            S.dma(trilT[:], trilT_d, writes=['trilT'])
            S.dma(iota_f[:], iota_d, writes=['iota_f'])
            S.dma(fing_t[:], fing, writes=['fing'])
            S.dma(maskq[:], maskq_d, writes=['maskq'])
            S.dma(rb_bc[:].unsqueeze(1), relb[0:1, :].partition_broadcast(128), writes=['rb_bc'])
            S.op('dve', lambda e: e.tensor_copy(out=ident_b[:], in_=ident_f[:]), reads=['ident_f'], writes=['ident_b'])
            S.op('pool', lambda e: e.memset(ones_f[:], 1.0), writes=['ones_f'])
            S.op('dve', lambda e: e.tensor_copy(out=iota_rep[:], in_=iota_f[:].unsqueeze(2).to_broadcast([128, 128, 8])), reads=['iota_f'], writes=['iota_rep'])
            S.op('pool', lambda e: e.memset(ones_b[:], 1.0), writes=['ones_b'])
            S.op('pool', lambda e: e.memset(eps_t[:], EPS), writes=['eps'])
            xin = [sb('xin%d' % i, [128, D], stack=ph) for i in range(2)]
            for tt in range(NT):
                xi = xin[tt % 2]
                S.dma(xi[:], x_d[tt * 128:(tt + 1) * 128, :], writes=[('xin', tt % 2)])
                for half in range(2):
                    bnk = psn()
                    for j in range(4):
                        k = half * 4 + j
                        S.op('pe', lambda e, bnk=bnk, j=j, k=k, xi=xi: e.transpose(
                            out=ps[bnk][:, j * 128:(j + 1) * 128], in_=xi[:, k * 128:(k + 1) * 128], identity=ident_f[:]),
                            reads=[('xin', tt % 2), 'ident_f'], writes=[pk(bnk)])
                    eng = 'act' if half == 0 else 'dve'
                    dst = xT[:, half * 4:half * 4 + 4, tt * 128:(tt + 1) * 128]
                    src = ps[bnk][:, :].rearrange("p (j t) -> p j t", j=4)
                    if eng == 'act':
                        S.op('act', lambda e, dst=dst, src=src: e.copy(out=dst, in_=src), reads=[pk(bnk)],
                             writes=[xk(k, tt // 4) for k in range(half * 4, half * 4 + 4)])
                    else:
                        S.op('dve', lambda e, dst=dst, src=src: e.tensor_copy(out=dst, in_=src), reads=[pk(bnk)],
                             writes=[xk(k, tt // 4) for k in range(half * 4, half * 4 + 4)])
            cond = sb('cond', [128, 8], stack=ph)
            bmt = sb('bmt', [128, L, 48], stack=ph)
            ngt = sb('ngt', [128, 2, L, 8], stack=ph)
            S.dma(cond[:], c_fm, writes=['cond'])
            S.dma(bmt[:], b_mod.rearrange("l p j -> p l j"), writes=['bmt'])
            S.dma(ngt[:, 0], n1g.rearrange("l p j -> p l j"), writes=['ngt0'])
            S.dma(ngt[:, 1], n2g.rearrange("l p j -> p l j"), writes=['ngt1'])
            S.op('act', lambda e: e.activation(out=cond[:], in_=cond[:], func=AF.Silu), reads=['cond'], writes=['cond'])
            wmt = [sb('wmt%d' % i, [128, 8, 512], stack=ph) for i in range(2)]
            wi = 0
            for l in range(L):
                mb = psn()
                for pc in range(12):
                    wt = wmt[wi % 2]
                    wkey = ('wmt', wi % 2)
                    wi += 1
                    S.dma(wt[:], w_mod[l, :, pc * 512:(pc + 1) * 512].rearrange("(k p) n -> p k n", p=128), writes=[wkey])
                    for oc in range(4):
                        col = pc * 4 + oc
                        for k in range(8):
                            S.op('pe', lambda e, wt=wt, oc=oc, k=k, col=col, mb=mb: e.matmul(
                                ps[mb][:, col:col + 1], lhsT=wt[:, k, oc * 128:(oc + 1) * 128], rhs=cond[:, k:k + 1],
                                start=(k == 0), stop=(k == 7)), reads=[wkey, 'cond'], writes=[pk(mb)])
                S.op('dve', lambda e, l=l, mb=mb: e.tensor_tensor(out=modv[:, l, :], in0=ps[mb][:, 0:48], in1=bmt[:, l, :], op=ALU.add),
                     reads=[pk(mb), 'bmt'], writes=[('modv', l)])
                S.op('dve', lambda e, l=l: e.scalar_tensor_tensor(out=gs1[:, l, :], in0=modv[:, l, 8:16], scalar=1.0, in1=ngt[:, 0, l, :],
                                                                  op0=ALU.add, op1=ALU.mult), reads=[('modv', l), 'ngt0'], writes=[('gs1', l)])
                S.op('dve', lambda e, l=l: e.scalar_tensor_tensor(out=gs2[:, l, :], in0=modv[:, l, 32:40], scalar=1.0, in1=ngt[:, 1, l, :],
                                                                  op0=ALU.add, op1=ALU.mult), reads=[('modv', l), 'ngt1'], writes=[('gs2', l)])
            bkt = sb('bkt', [128, 256], stack=ph)
            mkt = sb('mkt', [128, 256], stack=ph)
            tbt = sb('tbt', [128, 256], stack=ph)
            S.dma(bkt[:], bk_d, writes=['bkt'])
            S.dma(mkt[:], mk_d, writes=['mkt'])
            for h in range(4):
                S.op('pool', lambda e, h=h: e.tensor_copy(out=TB[:, h, :], in_=mkt[:]), reads=['mkt'], writes=[('TB', h)])
                for bq in range(32):
                    S.op('dve', lambda e, h=h, bq=bq: e.tensor_scalar(out=tbt[:], in0=bkt[:], scalar1=float(bq), scalar2=rb_bc[:, bq * 4 + h:bq * 4 + h + 1],
                                                                    op0=ALU.is_equal, op1=ALU.mult), reads=['bkt', 'rb_bc'], writes=['tbt'])
                    S.op('dve', lambda e, h=h: e.tensor_tensor(out=TB[:, h, :], in0=TB[:, h, :], in1=tbt[:], op=ALU.add),
                         reads=['tbt', ('TB', h)], writes=[('TB', h)])
            stf = [sb('stf%d' % i, [128, 8, 256], stack=ph) for i in range(2)]
            stb = [sb('stb%d' % i, [128, 8, 256], BF16, stack=ph) for i in range(2)]
            ci = 0
            for l in range(L):
                for (src, dstd, ncol) in ((w_in, wind, 2560), (w_out, woutd, 1024)):
                    for pc in range(ncol // 256):
                        sf, sbf = stf[ci % 2], stb[ci % 2]
                        kf, kb_ = ('stf', ci % 2), ('stb', ci % 2)
                        S.dma(sf[:], src[l, :, pc * 256:(pc + 1) * 256].rearrange("(k p) n -> p k n", p=128), writes=[kf])
                        eng = ('dve', 'pool')[ci % 2]
                        S.op(eng, lambda e, sf=sf, sbf=sbf: e.tensor_copy(out=sbf[:], in_=sf[:]), reads=[kf], writes=[kb_])
                        S.dma(dstd[l].rearrange("p (k n) -> p k n", k=8)[:, :, pc * 256:(pc + 1) * 256], sbf[:], reads=[kb_],
                              writes=[('wd', id(dstd), l, pc)])
                        ci += 1
            S.barrier()

        for l in range(L):
            lam_init = 0.8 - 0.6 * math.exp(-0.3 * l)
            with contextlib.ExitStack() as ph:
                dlt = sb('dlt', [128, 128], stack=ph)
                sgb = sb('sgb', [128, 64], stack=ph)
                cdwt = sb('cdwt', [128, 62], stack=ph)
                clg = sb('clg', [128, 2], stack=ph)
                clb = sb('clb', [128, 2], stack=ph)
                scwt = sb('scwt', [128, 6], stack=ph)
                slg = sb('slg', [128, 256], stack=ph)
                slb = sb('slb', [128, 256], stack=ph)
                wtf = sb('wtf', [128, 512], stack=ph)
                WTm = sb('WTm', [128, 512], BF16, stack=ph)
                sbf_ = sb('sbf_', [1, 512], stack=ph)
                sbb = sb('sbb', [1, 512], BF16, stack=ph)
                lamv = sb('lamv', [128, 8], stack=ph)
                junk = sb('junk', [128, 64], stack=ph)
                S.dma(dlt[:].unsqueeze(1), dlam[l:l + 1, :].partition_broadcast(128), writes=['dlt'])
                S.dma(sgb[:].unsqueeze(1), sublg[l:l + 1, :].partition_broadcast(128), writes=['sgb'])
                S.dma(cdwt[:], cdw[l], writes=['cdwt'])
                S.dma(clg[:], clng[l], writes=['clg'])
                S.dma(clb[:], clnb[l], writes=['clb'])
                S.dma(scwt[:], scw[l], writes=['scwt'])
                S.dma(slg[:].unsqueeze(1), slng[l:l + 1, :].partition_broadcast(128), writes=['slg'])
                S.dma(slb[:].unsqueeze(1), slnb[l:l + 1, :].partition_broadcast(128), writes=['slb'])
                S.dma(wtf[:], sguwT[l], writes=['wtf'])
                S.dma(sbf_[:], sgub[l:l + 1, :], writes=['sbf'])
                S.op('dve', lambda e: e.tensor_tensor(out=WTm[:].rearrange("p (h t) -> p h t", h=4), in0=wtf[:].rearrange("p (h t) -> p h t", h=4),
                                                      in1=trilT[:].unsqueeze(1).to_broadcast([128, 4, 128]), op=ALU.mult),
                     reads=['wtf', 'trilT'], writes=['WTm'])
                S.op('dve', lambda e: e.tensor_copy(out=sbb[:], in_=sbf_[:]), reads=['sbf'], writes=['sbb'])
                S.op('dve', lambda e: e.tensor_scalar(out=sgb[:], in0=sgb[:], scalar1=float(1.0 - lam_init), scalar2=None, op0=ALU.mult),
                     reads=['sgb'], writes=['sgb'])
                S.op('dve', lambda e: e.tensor_tensor(out=junk[:, 0:32], in0=dlt[:, 0:32], in1=dlt[:, 32:64], op=ALU.mult), reads=['dlt'], writes=['junk'])
                S.op('dve', lambda e: e.tensor_reduce(out=lamv[:, 0:1], in_=junk[:, 0:32], axis=AX.X, op=ALU.add), reads=['junk'], writes=['lam0'])
                S.op('dve', lambda e: e.tensor_tensor(out=junk[:, 32:64], in0=dlt[:, 64:96], in1=dlt[:, 96:128], op=ALU.mult), reads=['dlt'], writes=['junk'])
                S.op('dve', lambda e: e.tensor_reduce(out=lamv[:, 1:2], in_=junk[:, 32:64], axis=AX.X, op=ALU.add), reads=['junk'], writes=['lam1'])
                S.op('act', lambda e: e.activation(out=lamv[:, 2:4], in_=lamv[:, 0:2], func=AF.Exp), reads=['lam0', 'lam1'], writes=['lam2'])
                S.op('dve', lambda e: e.tensor_tensor(out=lamv[:, 4:5], in0=lamv[:, 2:3], in1=lamv[:, 3:4], op=ALU.subtract), reads=['lam2'], writes=['lam4'])
                S.op('dve', lambda e: e.tensor_scalar(out=lamv[:, 5:6], in0=lamv[:, 4:5], scalar1=float(lam_init), scalar2=-1.0, op0=ALU.add, op1=ALU.mult),
                     reads=['lam4'], writes=['neglam'])

                hT = sb('hT', [128, 8, 512], BF16, stack=ph)
                yT = sb('yT', [128, 8, 512], BF16, stack=ph)
                kT = sb('kT', [128, 2, T], BF16, stack=ph)
                qT = sb('qT', [128, 2, 2, 512], BF16, stack=ph)
                vaug = sb('vaug', [128, NT, 4, 66], BF16, stack=ph)
                wsl = [sb('wsl%d' % i, [128, 8, 256], BF16, stack=ph) for i in range(3)]
                wol = [sb('wol%d' % i, [128, 8, 128], BF16, stack=ph) for i in range(2)]
                wkf = [sb('wkf%d' % i, [128, 512], stack=ph) for i in range(6)]
                GL = sb('GL', [128, 2, 30 + 512], BF16, stack=ph)
                dgt = sb('dgt', [128, 62, 128], BF16, stack=ph)
                zc = sb('zc', [128, 2, 512], stack=ph)
                MM = sb('MM', [128, 2, 2 + 512], stack=ph)
                uTt = sb('uTt', [128, 2, 512], stack=ph)
                Ebuf = [sb('E%d' % i, [128, 16, 128], BF16, stack=ph) for i in range(4)]
                tmpn = [sb('tmpn%d' % i, [128, 256], stack=ph) for i in range(4)]
                junk2 = sb('junk2', [128, 128], stack=ph)
                yat = sb('yat', [128, 256], BF16, stack=ph)
                att = sb('att', [128, 4, 80], stack=ph)
                gv = [sb('gv%d' % i, [128, 256], stack=ph) for i in range(4)]
                vn = [sb('vn%d' % i, [128, 256], BF16, stack=ph) for i in range(2)]
                bst = sb('bst', [128, 64], stack=ph)
                rst = sb('rst', [128, 512], stack=ph)
                rkey = 'rst'
                S.op('pool', lambda e: e.memset(vaug[:], 1.0), writes=['vaug_all'])
                for idx_ in range(62):
                    S.op('dve' if idx_ % 2 == 0 else 'pool', lambda e, idx_=idx_: e.tensor_scalar(out=dgt[:, idx_, :], in0=ident_f[:], scalar1=cdwt[:, idx_:idx_ + 1], scalar2=None, op0=ALU.mult),
                         reads=['cdwt', 'ident_f'], writes=['dgt'])
                S.op('pool', lambda e: e.memset(GL[:, :, 0:30], 0.0), writes=['GLh0', 'GLh1'])
                S.op('pool', lambda e: e.memset(MM[:, :, 0:2], 0.0), writes=['MMh0', 'MMh1'])
                wsi = [0]
                wki = [0]

                def wk():
                    wki[0] += 1
                    i = wki[0] % 6
                    return wkf[i], ('wkf', i)

                def load_slice(s):
                    i = wsi[0] % 3
                    wsi[0] += 1
                    S.dma(wsl[i][:], wind[l].rearrange("p (k n) -> p k n", k=8)[:, :, s * 256:(s + 1) * 256], writes=[('wsl', i)],
                          reads=[('wd', id(wind), l, s)])
                    return wsl[i], ('wsl', i)

                def fm_chunk(w, wkey, c, bnk):
                    for k in range(8):
                        S.op('pe', lambda e, k=k: e.matmul(ps[bnk][:, :], lhsT=w[:, k, c * 128:(c + 1) * 128], rhs=hT[:, k, :],
                                                           start=(k == 0), stop=(k == 7)), reads=[wkey, 'hT'], writes=[pk(bnk)])

                def tm_tile(w, wkey, tt, bnk):
                    for k in range(8):
                        S.op('pe', lambda e, k=k: e.matmul(ps[bnk][:, 0:256], lhsT=hT[:, k, tt * 128:(tt + 1) * 128], rhs=w[:, k, :],
                                                           start=(k == 0), stop=(k == 7)), reads=[wkey, 'hT'], writes=[pk(bnk)])

                for b in range(NB):
                    bs = slice(b * 512, (b + 1) * 512)
                    sbank = psn()
                    for k in range(8):
                        w_, wkey_ = wk()
                        S.op('act', lambda e, k=k, w_=w_: e.activation(out=w_[:], in_=xT[:, k, bs], func=AF.Square), reads=[xk(k, b)], writes=[wkey_])
                        S.op('pe', lambda e, k=k, w_=w_: e.matmul(ps[sbank][:, :], lhsT=ones_f[:], rhs=w_[:], start=(k == 0), stop=(k == 7)),
                             reads=[wkey_, 'ones_f'], writes=[pk(sbank)])
                    S.op('act', lambda e: e.activation(out=rst[:], in_=ps[sbank][:, :], func=AF.Ln, bias=eps_t[:], scale=1.0 / D),
                         reads=[pk(sbank), 'eps'], writes=[rkey])
                    S.op('act', lambda e: e.activation(out=rst[:], in_=rst[:], func=AF.Exp, scale=-0.5), reads=[rkey], writes=[rkey])
                    for k in range(8):
                        w_, wkey_ = wk()
                        S.op('dve', lambda e, k=k, w_=w_: e.scalar_tensor_tensor(out=w_[:], in0=xT[:, k, bs], scalar=gs1[:, l, k:k + 1], in1=rst[:],
                                                                                 op0=ALU.mult, op1=ALU.mult), reads=[xk(k, b), rkey, ('gs1', l)], writes=[wkey_])
                        S.op('act', lambda e, k=k, w_=w_: e.activation(out=hT[:, k, :], in_=w_[:], func=AF.Identity, bias=modv[:, l, k:k + 1], scale=1.0),
                             reads=[wkey_, ('modv', l)], writes=['hT'])
                    w, wkey = load_slice(1)
                    for c in range(2):
                        bnk = psn()
                        fm_chunk(w, wkey, c, bnk)
                        S.op('act', lambda e, c=c, bnk=bnk: e.copy(out=kT[:, c, bs], in_=ps[bnk][:, :]), reads=[pk(bnk)], writes=[('kT', c, b)])
                    w, wkey = load_slice(2)
                    for i in range(4):
                        gi = b * 4 + i
                        bnk = psn()
                        tm_tile(w, wkey, i, bnk)
                        S.op('dve', lambda e, gi=gi, bnk=bnk: e.tensor_copy(out=vaug[:, gi, :, 0:64], in_=ps[bnk][:, 0:256].rearrange("p (h d) -> p h d", h=4)),
                             reads=[pk(bnk), 'vaug_all'], writes=[('vaug', gi)])
                    w, wkey = load_slice(0)
                    for c in range(2):
                        bnk = psn()
                        fm_chunk(w, wkey, c, bnk)
                        for m in range(2):
                            S.op('act', lambda e, c=c, bnk=bnk, m=m: e.activation(out=qT[:, m, c, :], in_=ps[bnk][:, :], func=AF.Copy, scale=maskq[:, m:m + 1]),
                                 reads=[pk(bnk), 'maskq'], writes=[('qT', c)])
                    PA, PR = (0, 1, 2), (3, 4, 5)

                    def gen_attn(ch):
                        for i in range(4):
                            gi = b * 4 + i
                            for h in (2 * ch, 2 * ch + 1):
                                c = h // 2
                                acc = 6 + ch
                                ao = (h % 2) * 256
                                for m in range(2):
                                    E = Ebuf[ch * 2 + m]
                                    ekey = ('E', ch, m)
                                    pb = (h % 2) * 64
                                    bnk = psn(PA)
                                    nears = [gi] if gi == 0 else [gi - 1, gi]
                                    for kb in nears:
                                        slot = kb - (gi - 1)
                                        S.op('pe', lambda e, kb=kb, slot=slot, bnk=bnk, pb=pb: e.matmul(
                                            ps[bnk][:, slot * 128:(slot + 1) * 128], lhsT=kT[pb:pb + 64, c, kb * 128:(kb + 1) * 128],
                                            rhs=qT[pb:pb + 64, m, c, i * 128:(i + 1) * 128], start=True, stop=True),
                                            reads=[('kT', c, kb // 4), ('qT', c)], writes=[pk(bnk)])
                                    s0 = 1 if gi == 0 else 0
                                    tn = tmpn[ch * 2 + m]
                                    S.op('dve', lambda e, bnk=bnk, s0=s0, tn=tn: e.tensor_tensor(out=tn[:, s0 * 128:256], in0=ps[bnk][:, s0 * 128:256],
                                                                                                 in1=TB[:, h, s0 * 128:256], op=ALU.add),
                                         reads=[pk(bnk), ('TB', h)], writes=[('tmpn', ch, m)])
                                    S.op('act', lambda e, s0=s0, tn=tn, E=E: e.activation(
                                        out=E[:, gi - 1 + s0:gi + 1, :], in_=tn[:, s0 * 128:256].rearrange("p (s q) -> p s q", q=128), func=AF.Exp),
                                        reads=[('tmpn', ch, m)], writes=[ekey])
                                    for g0 in range(0, gi - 1, 4):
                                        g1 = min(g0 + 4, gi - 1)
                                        bnk = psn(PA)
                                        for kb in range(g0, g1):
                                            S.op('pe', lambda e, kb=kb, bnk=bnk, pb=pb: e.matmul(
                                                ps[bnk][:, (kb - g0) * 128:(kb - g0 + 1) * 128], lhsT=kT[pb:pb + 64, c, kb * 128:(kb + 1) * 128],
                                                rhs=qT[pb:pb + 64, m, c, i * 128:(i + 1) * 128], start=True, stop=True),
                                                reads=[('kT', c, kb // 4), ('qT', c)], writes=[pk(bnk)])
                                        S.op('act', lambda e, g0=g0, g1=g1, bnk=bnk, E=E: e.activation(
                                            out=E[:, g0:g1, :], in_=ps[bnk][:, 0:(g1 - g0) * 128].rearrange("p (s q) -> p s q", q=128), func=AF.Exp,
                                            bias=rb_bc[:, 124 + h:125 + h], scale=1.0), reads=[pk(bnk), 'rb_bc'], writes=[ekey])
                                    for kb in range(gi + 1):
                                        S.op('pe', lambda e, kb=kb, E=E: e.matmul(ps[acc][:, ao + m * 128:ao + m * 128 + 65], lhsT=E[:, kb, :], rhs=vaug[:, kb, h, 0:65],
                                                                               start=(kb == 0), stop=(kb == gi)),
                                             reads=[ekey, ('vaug', kb)], writes=[('acc', ch, ao)])
                                    yield
                                a = att[:, h, :]
                                akey = ('att', h)
                                S.op('dve', lambda e, a=a, acc=acc: e.reciprocal(out=a[:, 0:2], in_=ps[acc][:, ao:ao + 256].rearrange("p (m x) -> p m x", m=2)[:, :, 64]),
                                     reads=[('acc', ch, ao)], writes=[akey])
                                S.op('dve', lambda e, a=a: e.tensor_tensor(out=a[:, 2:3], in0=a[:, 1:2], in1=lamv[:, 5:6], op=ALU.mult),
                                     reads=[akey, 'neglam'], writes=[akey])
                                S.op('dve', lambda e, a=a, acc=acc: e.tensor_scalar(out=a[:, 8:72], in0=ps[acc][:, ao:ao + 64], scalar1=a[:, 0:1], scalar2=None, op0=ALU.mult),
                                     reads=[('acc', ch, ao), akey], writes=[akey])
                                S.op('dve', lambda e, a=a, acc=acc: e.scalar_tensor_tensor(out=a[:, 8:72], in0=ps[acc][:, ao + 128:ao + 192], scalar=a[:, 2:3], in1=a[:, 8:72],
                                                                                           op0=ALU.mult, op1=ALU.add), reads=[('acc', ch, ao), akey], writes=[akey])
                                S.op('act', lambda e, a=a: e.activation(out=junk2[:, ch * 64:(ch + 1) * 64], in_=a[:, 8:72], func=AF.Square, accum_out=a[:, 3:4]),
                                     reads=[akey], writes=[akey, ('junk2', ch)])
                                S.op('act', lambda e, a=a: e.activation(out=a[:, 4:5], in_=a[:, 3:4], func=AF.Ln, bias=eps_t[:], scale=1.0 / 64),
                                     reads=[akey, 'eps'], writes=[akey])
                                S.op('act', lambda e, a=a: e.activation(out=a[:, 4:5], in_=a[:, 4:5], func=AF.Exp, scale=-0.5), reads=[akey], writes=[akey])
                                S.op('dve', lambda e, a=a, h=h: e.scalar_tensor_tensor(out=yat[:, h * 64:(h + 1) * 64], in0=a[:, 8:72], scalar=a[:, 4:5], in1=sgb[:],
                                                                                       op0=ALU.mult, op1=ALU.mult), reads=[akey, 'sgb'], writes=[('yat', h)])
                                yield
                            bnk = psn(PA)
                            pbf = ps[bnk][:, 0:64].bitcast(BF16)
                            S.op('pe', lambda e, pbf=pbf: e.transpose(out=pbf, in_=yat[:, ch * 128:(ch + 1) * 128], identity=ident_b[:]),
                                 reads=[('yat', 2 * ch), ('yat', 2 * ch + 1), 'ident_b'], writes=[pk(bnk)])
                            S.op('act', lambda e, pbf=pbf, i=i: e.copy(out=yT[:, ch, i * 128:(i + 1) * 128], in_=pbf),
                                 reads=[pk(bnk)], writes=[('yT', ch, i)])
                            yield

                    def gen_rest():
                        wgb, kgb = load_slice(4)
                        wga, kga = load_slice(3)
                        for c in range(2):
                            bnk = psn(PR)
                            yield
                            fm_chunk(wgb, kgb, c, bnk)
                            sg, sgk = wk()
                            S.op('act', lambda e, bnk=bnk, sg=sg: e.activation(out=sg[:], in_=ps[bnk][:, :], func=AF.Sigmoid), reads=[pk(bnk)], writes=[sgk])
                            bnk2 = psn(PR)
                            yield
                            fm_chunk(wga, kga, c, bnk2)
                            S.op('dve', lambda e, bnk2=bnk2, sg=sg, c=c: e.tensor_tensor(out=GL[:, c, 30:542], in0=ps[bnk2][:, :], in1=sg[:], op=ALU.mult),
                                 reads=[pk(bnk2), sgk], writes=[('GL', c)])
                            cbk = psn(PR)
                            for j in range(31):
                                if j % 8 == 7:
                                    yield
                                S.op('pe', lambda e, c=c, j=j, cbk=cbk: e.matmul(ps[cbk][:, :], lhsT=dgt[:, c * 31 + j, :], rhs=GL[:, c, j:j + 512],
                                                                               start=(j == 0), stop=(j == 30)), reads=[('GL', c), 'GLh%d' % c, 'dgt'], writes=[pk(cbk)])
                            S.op('act', lambda e, c=c, cbk=cbk: e.copy(out=zc[:, c, :], in_=ps[cbk][:, :]), reads=[pk(cbk)], writes=[('zc', c)])
                            S.op('pool', lambda e, c=c: e.tensor_copy(out=GL[:, c, 0:30], in_=GL[:, c, 512:542]), reads=[('GL', c)], writes=['GLh%d' % c])
                        yield
                        mub, msb = psn(PR), psn(PR)
                        for c in range(2):
                            S.op('pe', lambda e, c=c: e.matmul(ps[mub][:, :], lhsT=ones_f[:], rhs=zc[:, c, :], start=(c == 0), stop=(c == 1)),
                                 reads=[('zc', c), 'ones_f'], writes=[pk(mub)])
                        for c in range(2):
                            w_, wkey_ = wk()
                            S.op('act', lambda e, c=c, w_=w_: e.activation(out=w_[:], in_=zc[:, c, :], func=AF.Square), reads=[('zc', c)], writes=[wkey_])
                            S.op('pe', lambda e, c=c, w_=w_: e.matmul(ps[msb][:, :], lhsT=ones_f[:], rhs=w_[:], start=(c == 0), stop=(c == 1)),
                                 reads=[wkey_, 'ones_f'], writes=[pk(msb)])
                        mu, muk = wk()
                        var, vark = wk()
                        S.op('act', lambda e: e.activation(out=mu[:], in_=ps[mub][:, :], func=AF.Copy, scale=1.0 / 256), reads=[pk(mub)], writes=[muk])
                        S.op('dve', lambda e: e.tensor_tensor(out=var[:], in0=mu[:], in1=mu[:], op=ALU.mult), reads=[muk], writes=[vark])
                        S.op('dve', lambda e: e.scalar_tensor_tensor(out=var[:], in0=ps[msb][:, :], scalar=1.0 / 256, in1=var[:], op0=ALU.mult, op1=ALU.subtract),
                             reads=[pk(msb), vark], writes=[vark])
                        S.op('dve', lambda e: e.tensor_scalar(out=var[:], in0=var[:], scalar1=0.0, scalar2=None, op0=ALU.max), reads=[vark], writes=[vark])
                        S.op('act', lambda e: e.activation(out=var[:], in_=var[:], func=AF.Ln, bias=eps_t[:], scale=1.0), reads=[vark, 'eps'], writes=[vark])
                        S.op('act', lambda e: e.activation(out=var[:], in_=var[:], func=AF.Exp, scale=-0.5), reads=[vark], writes=[vark])
                        for c in range(2):
                            w_, wkey_ = wk()
                            S.op('dve', lambda e, c=c, w_=w_: e.tensor_tensor(out=w_[:], in0=zc[:, c, :], in1=mu[:], op=ALU.subtract), reads=[('zc', c), muk], writes=[wkey_])
                            S.op('dve', lambda e, w_=w_: e.tensor_tensor(out=w_[:], in0=w_[:], in1=var[:], op=ALU.mult), reads=[wkey_, vark], writes=[wkey_])
                            S.op('act', lambda e, c=c, w_=w_: e.activation(out=yT[:, 2 + c, :], in_=w_[:], func=AF.Silu, bias=clb[:, c:c + 1], scale=clg[:, c:c + 1]),
                                 reads=[wkey_, 'clg', 'clb'], writes=[('yT', 2 + c, i) for i in range(4)])
                        wcc, kcc = load_slice(6)
                        wch, kch = load_slice(7)
                        for c in range(2):
                            bnk = psn(PR)
                            yield
                            fm_chunk(wcc, kcc, c, bnk)
                            w_, wkey_ = wk()
                            S.op('act', lambda e, bnk=bnk, w_=w_: e.copy(out=w_[:], in_=ps[bnk][:, :]), reads=[pk(bnk)], writes=[wkey_])
                            bnk2 = psn(PR)
                            yield
                            fm_chunk(wch, kch, c, bnk2)
                            S.op('dve', lambda e, bnk2=bnk2, w_=w_, c=c: e.tensor_tensor(out=MM[:, c, 2:514], in0=ps[bnk2][:, :], in1=w_[:], op=ALU.mult),
                                 reads=[pk(bnk2), wkey_], writes=[('MM', c)])
                        wcb, kcb = load_slice(5)
                        for c in range(2):
                            cv, cvk = wk()
                            S.op('dve', lambda e, c=c, cv=cv: e.tensor_scalar(out=cv[:], in0=MM[:, c, 0:512], scalar1=scwt[:, c * 3:c * 3 + 1], scalar2=None, op0=ALU.mult),
                                 reads=[('MM', c), 'MMh%d' % c, 'scwt'], writes=[cvk])
                            for j in (1, 2):
                                S.op('dve', lambda e, c=c, j=j, cv=cv: e.scalar_tensor_tensor(out=cv[:], in0=MM[:, c, j:j + 512], scalar=scwt[:, c * 3 + j:c * 3 + j + 1], in1=cv[:],
                                                                                              op0=ALU.mult, op1=ALU.add), reads=[('MM', c), 'MMh%d' % c, cvk], writes=[cvk])
                            S.op('pool', lambda e, c=c: e.tensor_copy(out=MM[:, c, 0:2], in_=MM[:, c, 512:514]), reads=[('MM', c)], writes=['MMh%d' % c])
                            bnk = psn(PR)
                            yield
                            fm_chunk(wcb, kcb, c, bnk)
                            S.op('dve', lambda e, c=c, cv=cv, bnk=bnk: e.tensor_tensor(out=yT[:, 4 + c, :], in0=ps[bnk][:, :], in1=cv[:], op=ALU.mult),
                                 reads=[pk(bnk), cvk], writes=[('yT', 4 + c, i) for i in range(4)])
                        wsu, ksu = load_slice(8)
                        for c in range(2):
                            bnk = psn(PR)
                            yield
                            fm_chunk(wsu, ksu, c, bnk)
                            S.op('act', lambda e, c=c, bnk=bnk: e.activation(out=uTt[:, c, :], in_=ps[bnk][:, :], func=AF.Gelu), reads=[pk(bnk)], writes=[('uTt', c)])
                        wsv, ksv = load_slice(9)
                        zb = [3, 4]
                        for i in range(4):
                            bnk = 5
                            yield
                            tm_tile(wsv, ksv, i, bnk)
                            S.op('act', lambda e, bnk=bnk, i=i: e.activation(out=gv[i][:], in_=ps[bnk][:, 0:256], func=AF.Gelu), reads=[pk(bnk)], writes=[('gv', i)])
                        for i in range(4):
                            yield
                            g_ = gv[i]
                            gk = ('gv', i)
                            v_ = vn[i % 2]
                            vk_ = ('vn', i % 2)
                            bs_ = bst[:, i * 16:(i + 1) * 16]
                            bk_ = ('bst', i)
                            S.op('dve', lambda e, g_=g_, bs_=bs_: e.bn_stats(out=bs_[:, 0:6], in_=g_[:]), reads=[gk], writes=[bk_])
                            S.op('dve', lambda e, bs_=bs_: e.bn_aggr(out=bs_[:, 8:10], in_=bs_[:, 0:6]), reads=[bk_], writes=[bk_])
                            S.op('act', lambda e, bs_=bs_: e.activation(out=bs_[:, 10:11], in_=bs_[:, 9:10], func=AF.Ln, bias=eps_t[:], scale=1.0), reads=[bk_, 'eps'], writes=[bk_])
                            S.op('act', lambda e, bs_=bs_: e.activation(out=bs_[:, 10:11], in_=bs_[:, 10:11], func=AF.Exp, scale=-0.5), reads=[bk_], writes=[bk_])
                            S.op('dve', lambda e, g_=g_, bs_=bs_: e.tensor_scalar(out=g_[:], in0=g_[:], scalar1=bs_[:, 8:9], scalar2=bs_[:, 10:11], op0=ALU.subtract, op1=ALU.mult),
                                 reads=[gk, bk_], writes=[gk])
                            S.op('dve', lambda e, g_=g_: e.tensor_tensor(out=g_[:], in0=g_[:], in1=slg[:], op=ALU.mult), reads=[gk, 'slg'], writes=[gk])
                            S.op('dve', lambda e, g_=g_, v_=v_: e.tensor_tensor(out=v_[:], in0=g_[:], in1=slb[:], op=ALU.add), reads=[gk, 'slb'], writes=[vk_])
                            for h in range(4):
                                c = h // 2
                                po = (h % 2) * 64
                                S.op('pe', lambda e, h=h, c=c, po=po, v_=v_, i=i: e.matmul(ps[zb[c]][po:po + 64, i * 128:(i + 1) * 128], lhsT=v_[:, h * 64:(h + 1) * 64],
                                                                                     rhs=WTm[:, h * 128:(h + 1) * 128], start=True, stop=False),
                                     reads=[vk_, 'WTm'], writes=[pk(zb[c])])
                                S.op('pe', lambda e, h=h, c=c, po=po, i=i: e.matmul(ps[zb[c]][po:po + 64, i * 128:(i + 1) * 128], lhsT=ones_b[0:1, 0:64],
                                                                              rhs=sbb[0:1, h * 128:(h + 1) * 128], start=False, stop=True),
                                     reads=['ones_b', 'sbb'], writes=[pk(zb[c])])
                        for c in range(2):
                            S.op('dve', lambda e, c=c: e.tensor_tensor(out=yT[:, 6 + c, :], in0=ps[zb[c]][:, :], in1=uTt[:, c, :], op=ALU.mult),
                                 reads=[pk(zb[c]), ('uTt', c)], writes=[('yT', 6 + c, i) for i in range(4)])

                    gens = [gen_attn(0), gen_attn(1), gen_rest()]
                    reps = [1, 1, 2]
                    alive = [True, True, True]
                    while any(alive):
                        for gi_, g_ in enumerate(gens):
                            for _ in range(reps[gi_]):
                                if alive[gi_]:
                                    try:
                                        next(g_)
                                    except StopIteration:
                                        alive[gi_] = False
                    ykeys = [('yT', k, i) for k in range(8) for i in range(4)]
                    for oc in range(8):
                        wo = wol[oc % 2]
                        wok = ('wol', oc % 2)
                        S.dma(wo[:], woutd[l].rearrange("p (k n) -> p k n", k=8)[:, :, oc * 128:(oc + 1) * 128], writes=[wok],
                              reads=[('wd', id(woutd), l, oc // 2)])
                        bnk = psn()
                        for k in range(8):
                            S.op('pe', lambda e, k=k, wo=wo, bnk=bnk: e.matmul(ps[bnk][:, :], lhsT=wo[:, k, :], rhs=yT[:, k, :], start=(k == 0), stop=(k == 7)),
                                 reads=[wok] + ykeys, writes=[pk(bnk)])
                        S.op('dve', lambda e, oc=oc, bnk=bnk: e.scalar_tensor_tensor(out=xT[:, oc, bs], in0=ps[bnk][:, :], scalar=modv[:, l, 16 + oc:17 + oc], in1=xT[:, oc, bs],
                                                                                     op0=ALU.mult, op1=ALU.add), reads=[pk(bnk), xk(oc, b), ('modv', l)], writes=[xk(oc, b)])
                S.barrier()
            if with_peer:
                peer_layer(nc, S, sb, ps, psn, pk, xk, l, T, NT, NB, locals())

        with contextlib.ExitStack() as ph:
            wkf = [sb('fwk%d' % i, [128, 512], stack=ph) for i in range(4)]
            osb = [sb('osb%d' % i, [128, D], stack=ph) for i in range(2)]
            toks = []
            wi = 0
            for b in range(NB):
                bs = slice(b * 512, (b + 1) * 512)
                sbank = psn()
                for k in range(8):
                    w_ = wkf[wi % 2]
                    wkey_ = ('fwk', wi % 2)
                    wi += 1
                    S.op('act', lambda e, k=k, w_=w_: e.activation(out=w_[:], in_=xT[:, k, bs], func=AF.Square), reads=[xk(k, b)], writes=[wkey_])
                    S.op('pe', lambda e, k=k, w_=w_: e.matmul(ps[sbank][:, :], lhsT=ones_f[:], rhs=w_[:], start=(k == 0), stop=(k == 7)),
                         reads=[wkey_, 'ones_f'], writes=[pk(sbank)])
                rst = wkf[2]
                S.op('act', lambda e: e.activation(out=rst[:], in_=ps[sbank][:, :], func=AF.Ln, bias=eps_t[:], scale=1.0 / D),
                     reads=[pk(sbank), 'eps'], writes=['frst'])
                S.op('act', lambda e: e.activation(out=rst[:], in_=rst[:], func=AF.Exp, scale=-0.5), reads=['frst'], writes=['frst'])
                for k in range(8):
                    S.op('dve', lambda e, k=k: e.scalar_tensor_tensor(out=xT[:, k, bs], in0=xT[:, k, bs], scalar=fing_t[:, k:k + 1], in1=rst[:],
                                                                      op0=ALU.mult, op1=ALU.mult), reads=[xk(k, b), 'frst', 'fing'], writes=[xk(k, b)])
                for i in range(4):
                    tt = b * 4 + i
                    ob = osb[tt % 2]
                    okey = ('osb', tt % 2)
                    for half in range(2):
                        bnk = psn()
                        for j in range(4):
                            k = half * 4 + j
                            S.op('pe', lambda e, bnk=bnk, j=j, k=k, tt=tt: e.transpose(out=ps[bnk][:, j * 128:(j + 1) * 128], in_=xT[:, k, tt * 128:(tt + 1) * 128],
                                                                                    identity=ident_f[:]), reads=[xk(k, b), 'ident_f'], writes=[pk(bnk)])
                        if half == 0:
                            S.op('act', lambda e, bnk=bnk, ob=ob: e.copy(out=ob[:, 0:512], in_=ps[bnk][:, :]), reads=[pk(bnk)], writes=[okey + (0,)])
                        else:
                            S.op('dve', lambda e, bnk=bnk, ob=ob: e.tensor_copy(out=ob[:, 512:1024], in_=ps[bnk][:, :]), reads=[pk(bnk)], writes=[okey + (1,)])
                    toks.append(S.dma(out_d[tt * 128:(tt + 1) * 128, :], ob[:], reads=[okey + (0,), okey + (1,)], writes=[('out', tt)]))
            S.wait_all('sp', toks)
    print('instructions emitted:', S.nops)
    return nc


def peer_layer(nc, S, sb, ps, psn, pk, xk, l, T, NT, NB, env):
    xT, modv, gs2, ones_f, eps_t, ident_f, iota_f, iota_rep = (env[k] for k in ('xT', 'modv', 'gs2', 'ones_f', 'eps_t', 'ident_f', 'iota_f', 'iota_rep'))
    wq, keysT, uT, vr, h2d, Gd = (env[k] for k in ('wq', 'keysT', 'uT', 'vr', 'h2d', 'Gd'))
    ALLB = tuple(range(8))
    h2dv = h2d.rearrange("p (k t) -> p k t", k=8)
    with contextlib.ExitStack() as ph:
        keyt = sb('keyt', [128, 16, 128], stack=ph)
        S.dma(keyt[:], keysT[l].rearrange("d (g n) -> d g n", g=16), writes=['keyt'])
        h2f = sb('h2f', [128, 8, 512], stack=ph)
        rst2 = sb('rst2', [128, 512], stack=ph)
        sqw = [sb('sqw%d' % i, [128, 512], stack=ph) for i in range(2)]
        h2b = [sb('h2b%d' % i, [128, 512], BF16, stack=ph) for i in range(2)]
        qpT = [sb('qpT%d' % i, [128, 16, 256], stack=ph) for i in range(1)]
        wqs = [sb('wqs%d' % i, [128, 8, 128], stack=ph) for i in range(2)]
        sc0s = [sb('sc0_%d' % i, [128, 16, 128], stack=ph) for i in range(2)]
        tv = sb('tv', [128, 16, 16], stack=ph)
        ti = sb('ti', [128, 16, 16], U32, stack=ph)
        tif = sb('tif', [128, 16, 16], stack=ph)
        cand = sb('cand', [128, 8, 256], stack=ph)
        tsv = sb('tsv', [128, 8, 16], stack=ph)
        tcv = sb('tcv', [128, 8, 16], U32, stack=ph)
        tab = sb('tab', [128, 2, 128], U32, stack=ph)
        tabf = sb('tabf', [128, 2, 128], stack=ph)
        i12g = sb('i12g', [128, 3, 128], stack=ph)
        zs = sb('zs', [128, 8], stack=ph)
        sT = sb('sT', [128, 3, 128], stack=ph)
        PT = [sb('PT%d' % i, [128, 128, 8], BF16, stack=ph) for i in range(3)]
        QT = [sb('QT%d' % i, [128, 128, 8], BF16, stack=ph) for i in range(3)]
        sTb = sb('sTb', [128, 2, 128], BF16, stack=ph)
        ohi = [0]
        GT = sb('GT', [128, 128, 128], BF16, stack=ph)
        tvv = tv[:].rearrange("p (h w) k -> p h w k", w=2)
        tifv = tif[:].rearrange("p (h w) k -> p h w k", w=2)
        cand4 = cand[:].rearrange("p h (a b) -> p h a b", a=16)
        wqc = [0]

        def emit_n2(b):
            bs = slice(b * 512, (b + 1) * 512)
            sbank = psn(ALLB)
            for k in range(8):
                w_ = sqw[k % 2]
                wkey_ = ('sqw', k % 2)
                S.op('act', lambda e, k=k, w_=w_: e.activation(out=w_[:], in_=xT[:, k, bs], func=AF.Square), reads=[xk(k, b)], writes=[wkey_])
                S.op('pe', lambda e, k=k, w_=w_: e.matmul(ps[sbank][:, :], lhsT=ones_f[:], rhs=w_[:], start=(k == 0), stop=(k == 7)),
                     reads=[wkey_, 'ones_f'], writes=[pk(sbank)])
            S.op('act', lambda e: e.activation(out=rst2[:], in_=ps[sbank][:, :], func=AF.Ln, bias=eps_t[:], scale=1.0 / D),
                 reads=[pk(sbank), 'eps'], writes=['rst2'])
            S.op('act', lambda e: e.activation(out=rst2[:], in_=rst2[:], func=AF.Exp, scale=-0.5), reads=['rst2'], writes=['rst2'])
            for k in range(8):
                w_ = sqw[k % 2]
                wkey_ = ('sqw', k % 2)
                hb_ = h2b[k % 2]
                hkey = ('h2b', k % 2)
                S.op('dve', lambda e, k=k, w_=w_: e.scalar_tensor_tensor(out=w_[:], in0=xT[:, k, bs], scalar=gs2[:, l, k:k + 1], in1=rst2[:],
                                                                         op0=ALU.mult, op1=ALU.mult), reads=[xk(k, b), 'rst2', ('gs2', l)], writes=[wkey_])
                S.op('act', lambda e, k=k, w_=w_: e.activation(out=h2f[:, k, :], in_=w_[:], func=AF.Identity, bias=modv[:, l, 24 + k:25 + k], scale=1.0),
                     reads=[wkey_, ('modv', l)], writes=[('h2f', k)])
                S.op('pool', lambda e, k=k, hb_=hb_: e.tensor_copy(out=hb_[:], in_=h2f[:, k, :]), reads=[('h2f', k)], writes=[hkey])
                S.dma(h2dv[:, k, bs], hb_[:], reads=[hkey], writes=[('h2d', k, b)])

        def emit_qproj(b, half, qi, glist=None):
            hs = slice(half * 256, (half + 1) * 256)
            for g in (glist if glist is not None else range(16)):
                wt = wqs[wqc[0] % 2]
                wkey = ('wqs', wqc[0] % 2)
                wqc[0] += 1
                S.dma(wt[:], wq[l, :, g * 128:(g + 1) * 128].rearrange("(k p) n -> p k n", p=128), writes=[wkey])
                bnk = psn(ALLB)
                for k in range(8):
                    S.op('pe', lambda e, k=k, wt=wt, bnk=bnk: e.matmul(ps[bnk][:, 0:256], lhsT=wt[:, k, :], rhs=h2f[:, k, hs], start=(k == 0), stop=(k == 7)),
                         reads=[wkey, ('h2f', k)], writes=[pk(bnk)])
                S.op('act', lambda e, g=g, bnk=bnk: e.copy(out=qpT[qi][:, g, :], in_=ps[bnk][:, 0:256]), reads=[pk(bnk)], writes=[('qpT', qi, g)])

        def emit_scores(tl, qi):
            tsl = slice(tl * 128, (tl + 1) * 128)
            for gq in range(4):
                bnk = psn(ALLB)
                for gg in range(4):
                    g = gq * 4 + gg
                    S.op('pe', lambda e, g=g, gg=gg, bnk=bnk: e.matmul(ps[bnk][:, gg * 128:(gg + 1) * 128], lhsT=qpT[qi][:, g, tsl], rhs=keyt[:, g, :],
                                                                       start=True, stop=True), reads=[('qpT', qi, g), 'keyt'], writes=[pk(bnk)])
                S.op('act', lambda e, gq=gq, bnk=bnk: e.copy(out=sc0s[tl][:, gq * 4:(gq + 1) * 4, :], in_=ps[bnk][:, :].rearrange("p (g n) -> p g n", g=4)),
                     reads=[pk(bnk)], writes=[('sc0', tl, gq * 4 + j) for j in range(4)])

        def part1a(tl):
            yield
            for g in range(16):
                S.op('dve', lambda e, g=g: e.max(out=tv[:, g, 0:8], in_=sc0s[tl][:, g, :]), reads=[('sc0', tl, g)], writes=[('tv', g)])
            yield
            for g in range(16):
                S.op('dve', lambda e, g=g: e.max_index(out=ti[:, g, 0:8], in_max=tv[:, g, 0:8], in_values=sc0s[tl][:, g, :]),
                     reads=[('sc0', tl, g), ('tv', g)], writes=[('ti', g)])
            yield
            for g in range(16):
                S.op('dve', lambda e, g=g: e.match_replace(out=sc0s[tl][:, g, :], in_to_replace=tv[:, g, 0:8], in_values=sc0s[tl][:, g, :], imm_value=NEG),
                     reads=[('sc0', tl, g), ('tv', g)], writes=[('sc0', tl, g)])
            yield
            for g in range(16):
                S.op('dve', lambda e, g=g: e.max(out=tv[:, g, 8:16], in_=sc0s[tl][:, g, :]), reads=[('sc0', tl, g)], writes=[('tv', g)])
            yield
            for g in range(16):
                S.op('dve', lambda e, g=g: e.max_index(out=ti[:, g, 8:16], in_max=tv[:, g, 8:16], in_values=sc0s[tl][:, g, :]),
                     reads=[('sc0', tl, g), ('tv', g)], writes=[('ti', g)])
            tvk = [('tv', g) for g in range(16)]
            tik = [('ti', g) for g in range(16)]
            S.op('dve', lambda e: e.tensor_tensor(out=cand4, in0=tvv[:, :, 0, :].unsqueeze(3).to_broadcast([128, 8, 16, 16]),
                                                  in1=tvv[:, :, 1, :].unsqueeze(2).to_broadcast([128, 8, 16, 16]), op=ALU.add),
                 reads=tvk, writes=[('cand', h) for h in range(8)])
            yield
            for h in range(8):
                S.op('dve', lambda e, h=h: e.max(out=tsv[:, h, 0:8], in_=cand[:, h, :]), reads=[('cand', h)], writes=[('tsv', h)])
            yield
            for h in range(8):
                S.op('dve', lambda e, h=h: e.max_index(out=tcv[:, h, 0:8], in_max=tsv[:, h, 0:8], in_values=cand[:, h, :]),
                     reads=[('cand', h), ('tsv', h)], writes=[('tcv', h)])
            yield
            for h in range(8):
                S.op('dve', lambda e, h=h: e.match_replace(out=cand[:, h, :], in_to_replace=tsv[:, h, 0:8], in_values=cand[:, h, :], imm_value=NEG),
                     reads=[('cand', h), ('tsv', h)], writes=[('cand', h)])
            yield
            for h in range(8):
                S.op('dve', lambda e, h=h: e.max(out=tsv[:, h, 8:16], in_=cand[:, h, :]), reads=[('cand', h)], writes=[('tsv', h)])
            yield
            for h in range(8):
                S.op('dve', lambda e, h=h: e.max_index(out=tcv[:, h, 8:16], in_max=tsv[:, h, 8:16], in_values=cand[:, h, :]),
                     reads=[('cand', h), ('tsv', h)], writes=[('tcv', h)])
            tsk = [('tsv', h) for h in range(8)]
            tck = [('tcv', h) for h in range(8)]
            ck = [('cand', h) for h in range(8)]
            g3 = i12g[:, 2, :].rearrange("p (h k) -> p h k", h=8)
            S.op('dve', lambda e: e.tensor_tensor(out=g3, in0=tsv[:], in1=tsv[:, :, 0:1].to_broadcast([128, 8, 16]), op=ALU.subtract),
                 reads=tsk, writes=['gate'])
            S.op('act', lambda e: e.activation(out=g3, in_=g3, func=AF.Exp), reads=['gate'], writes=['gate'])
            S.op('dve', lambda e: e.tensor_reduce(out=zs[:], in_=g3, axis=AX.X, op=ALU.add), reads=['gate'], writes=['zs'])
            S.op('dve', lambda e: e.reciprocal(out=zs[:], in_=zs[:]), reads=['zs'], writes=['zs'])
            S.op('dve', lambda e: e.tensor_tensor(out=g3, in0=g3, in1=zs[:].unsqueeze(2).to_broadcast([128, 8, 16]), op=ALU.mult),
                 reads=['gate', 'zs'], writes=['gate'])
            tcf = tcv[:].rearrange("p h k -> p (h k)")
            S.op('dve', lambda e: e.tensor_single_scalar(out=tab[:, 0, :], in_=tcf, scalar=4, op=ALU.logical_shift_right), reads=tck, writes=['tab0'])
            S.op('dve', lambda e: e.tensor_single_scalar(out=tab[:, 1, :], in_=tcf, scalar=15, op=ALU.bitwise_and), reads=tck, writes=['tab1'])
            S.op('dve', lambda e: e.tensor_copy(out=tabf[:], in_=tab[:]), reads=['tab0', 'tab1'], writes=['tabf'])
            S.op('dve', lambda e: e.tensor_copy(out=tif[:], in_=ti[:]), reads=tik, writes=['tif'])
            yield
            for w in range(2):
                af = tabf[:, w, :].rearrange("p (h k) -> p h k", h=8)
                S.op('dve', lambda e, af=af: e.tensor_tensor(out=cand4, in0=af.unsqueeze(3).to_broadcast([128, 8, 16, 16]),
                                                            in1=iota_f[:, 0:16].unsqueeze(1).unsqueeze(1).to_broadcast([128, 8, 16, 16]), op=ALU.is_equal),
                     reads=['tabf', 'iota_f'], writes=ck)
                S.op('dve', lambda e, w=w: e.tensor_tensor(out=cand4, in0=cand4, in1=tifv[:, :, w, :].unsqueeze(2).to_broadcast([128, 8, 16, 16]), op=ALU.mult),
                     reads=ck + ['tif'], writes=ck)
                S.op('dve', lambda e, w=w: e.tensor_reduce(out=i12g[:, w, :].rearrange("p (h k) -> p h k", h=8), in_=cand4, axis=AX.X, op=ALU.add),
                     reads=ck, writes=['i12_%d' % w])
            yield

        def part1b():
            bnk = psn(ALLB)
            for w in range(3):
                S.op('pe', lambda e, w=w, bnk=bnk: e.transpose(out=ps[bnk][:, w * 128:(w + 1) * 128], in_=i12g[:, w, :], identity=ident_f[:]),
                     reads=['i12_0', 'i12_1', 'gate', 'ident_f'], writes=[pk(bnk)])
            S.op('act', lambda e, bnk=bnk: e.copy(out=sT[:], in_=ps[bnk][:, 0:384].rearrange("p (w t) -> p w t", w=3)), reads=[pk(bnk)], writes=['sT'])
            S.op('act', lambda e, bnk=bnk: e.copy(out=sTb[:], in_=ps[bnk][:, 0:256].rearrange("p (w t) -> p w t", w=2)), reads=[pk(bnk)], writes=['sTb'])

        def part2(tt, after_group=None):
            iota_bc = iota_f[:].unsqueeze(1).to_broadcast([128, 8, 128])
            iota_bc4 = iota_f[:].unsqueeze(1).to_broadcast([128, 4, 128])

            def onehots(t8):
                t0 = t8 * 8
                bf = ohi[0] % 3
                ohi[0] += 1
                pkey, qkey = ('PT', bf), ('QT', bf)
                S.op('dve', lambda e: e.tensor_tensor(out=PT[bf][:], in0=iota_rep[:], in1=sTb[:, 0, t0:t0 + 8].unsqueeze(1).to_broadcast([128, 128, 8]),
                                                      op=ALU.is_equal), reads=['sTb', 'iota_rep'], writes=[pkey])
                S.op('pool', lambda e: e.tensor_tensor(out=PT[bf][:], in0=PT[bf][:], in1=sT[:, 2, t0:t0 + 8].unsqueeze(1).to_broadcast([128, 128, 8]),
                                                       op=ALU.mult), reads=['sT', pkey], writes=[pkey])
                S.op('dve', lambda e: e.tensor_tensor(out=QT[bf][:], in0=iota_rep[:], in1=sTb[:, 1, t0:t0 + 8].unsqueeze(1).to_broadcast([128, 128, 8]),
                                                      op=ALU.is_equal), reads=['sTb', 'iota_rep'], writes=[(qkey, 0), (qkey, 1)])
                return bf

            bfs = {0: onehots(0)}
            for t8 in range(16):
                t0 = t8 * 8
                if t8 + 1 < 16:
                    bfs[t8 + 1] = onehots(t8 + 1)
                bf = bfs[t8]
                pkey, qkey = ('PT', bf), ('QT', bf)
                for hq in range(2):
                    bnk = psn(ALLB)
                    for q in range(4):
                        sl = hq * 4 + q
                        S.op('pe', lambda e, q=q, bf=bf, sl=sl, bnk=bnk: e.matmul(ps[bnk][:, :].rearrange("p (j q) -> p q j", q=4)[:, q, :], lhsT=PT[bf][:, :, sl],
                                                                               rhs=QT[bf][:, :, sl], start=True, stop=True), reads=[pkey, (qkey, hq)], writes=[pk(bnk)])
                    tq = t0 + hq * 4
                    dst = GT[:, :, tq:tq + 4]
                    src = ps[bnk][:, :].rearrange("p (j q) -> p j q", q=4)
                    S.op('act', lambda e, dst=dst, src=src: e.copy(out=dst, in_=src), reads=[pk(bnk)], writes=[('GT', tq // 4)])
                if after_group is not None:
                    after_group(t8)
            S.dma(Gd[tt], GT[:].rearrange("p j t -> p (j t)"), reads=[('GT', q) for q in range(32)], writes=[('Gd', tt)])

        def exhaust(g):
            if g is not None:
                for _ in g:
                    pass

        def qproj_gen(nb_, nh_):
            for g in range(16):
                emit_qproj(nb_, nh_, 0, glist=[g])
                yield

        def chain(*gens):
            for g in gens:
                if g is not None:
                    for _ in g:
                        yield

        def stepper(g, n):
            def f(t8):
                for _ in range(n):
                    try:
                        next(g)
                    except StopIteration:
                        return
            return f

        seq = [(b, half) for b in range(NB) for half in range(2)]
        emit_n2(0)
        emit_qproj(0, 0, 0)
        emit_scores(0, 0)
        emit_scores(1, 0)
        exhaust(part1a(0))
        for idx, (b, half) in enumerate(seq):
            nxt = seq[idx + 1] if idx + 1 < len(seq) else None
            part1b()
            if nxt is not None and nxt[1] == 0:
                emit_n2(nxt[0])
            bg = chain(part1a(1), qproj_gen(*nxt) if nxt is not None else None)
            part2(b * 4 + half * 2 + 0, stepper(bg, 2))
            exhaust(bg)
            part1b()
            bg = None
            if nxt is not None:
                emit_scores(0, 0)
                emit_scores(1, 0)
                bg = part1a(0)
            part2(b * 4 + half * 2 + 1, stepper(bg, 1) if bg is not None else None)
            exhaust(bg)
        S.barrier()
    with contextlib.ExitStack() as ph:
        h2a = sb('h2a', [128, 8, T], BF16, stack=ph)
        for k in range(8):
            S.dma(h2a[:, k, :], h2dv[:, k, :], writes=[('h2a', k)])
        h2k = [('h2a', k) for k in range(8)]
        US = [sb('US%d' % i, [128, 8, 1024], BF16, stack=ph) for i in range(2)]
        VS = [sb('VS%d' % i, [128, 8, 1024], BF16, stack=ph) for i in range(2)]
        stg = [sb('stg%d' % i, [128, 1024], stack=ph) for i in range(4)]
        Gt = [sb('Gt%d' % i, [128, 8, 2, 128], BF16, stack=ph) for i in range(3)]
        Wt = [sb('Wt%d' % i, [128, 8, 256], BF16, stack=ph) for i in range(2)]
        ge = [sb('ge%d' % i, [128, 256], BF16, stack=ph) for i in range(3)]
        NS = 16
        NG = T // 256
        sti = [0]

        def load_uv(Sx, jj):
            slot = Sx % 2
            j = Sx * 8 + jj
            for which, src, dstb, eng in (('U', uT, US, 'act'), ('V', vr, VS, 'pool')):
                i = sti[0] % 4
                sti[0] += 1
                S.dma(stg[i][:], src[l, j], writes=[('stg', i)])
                if eng == 'act':
                    S.op('act', lambda e, i=i, dstb=dstb: e.copy(out=dstb[slot][:, jj, :], in_=stg[i][:]), reads=[('stg', i)], writes=[(which, slot, jj)])
                else:
                    S.op('pool', lambda e, i=i, dstb=dstb: e.tensor_copy(out=dstb[slot][:, jj, :], in_=stg[i][:]), reads=[('stg', i)], writes=[(which, slot, jj)])

        def load_g(it):
            Sx, tg = divmod(it, NG)
            gt = Gt[it % 3]
            for hh in range(2):
                tt = tg * 2 + hh
                S.dma(gt[:, :, hh, :], Gd[tt].rearrange("p (j t) -> p j t", j=128)[:, Sx * 8:(Sx + 1) * 8, :], writes=[('Gt', it % 3, hh)])

        for jj in range(8):
            load_uv(0, jj)
        load_g(0)
        if NS * NG > 1:
            load_g(1)
        for it in range(NS * NG):
            Sx, tg = divmod(it, NG)
            slot = Sx % 2
            if it + 2 < NS * NG:
                load_g(it + 2)
            gt = Gt[it % 3]
            wt = Wt[it % 2]
            tsl = slice(tg * 256, (tg + 1) * 256)
            for jj in range(8):
                bnk = psn(ALLB)
                for dk in range(8):
                    S.op('pe', lambda e, dk=dk, jj=jj, bnk=bnk: e.matmul(ps[bnk][:, 0:256], lhsT=US[slot][:, jj, dk * 128:(dk + 1) * 128], rhs=h2a[:, dk, tsl],
                                                                       start=(dk == 0), stop=(dk == 7)), reads=[('U', slot, jj), ('h2a', dk)], writes=[pk(bnk)])
                gi_ = (it * 8 + jj) % 3
                S.op('act', lambda e, bnk=bnk, gi_=gi_: e.activation(out=ge[gi_][:], in_=ps[bnk][:, 0:256], func=AF.Gelu), reads=[pk(bnk)], writes=[('ge', gi_)])
                eng = 'dve' if jj % 2 == 0 else 'pool'
                S.op(eng, lambda e, jj=jj, gi_=gi_, gt=gt, wt=wt: e.tensor_tensor(out=wt[:, jj, :], in0=ge[gi_][:], in1=gt[:, jj].rearrange("p h t -> p (h t)"), op=ALU.mult),
                     reads=[('ge', gi_), ('Gt', it % 3, 0), ('Gt', it % 3, 1)], writes=[('Wt', it % 2, jj)])
            for dk in range(8):
                bnk = psn(ALLB)
                for jj in range(8):
                    S.op('pe', lambda e, dk=dk, jj=jj, bnk=bnk, wt=wt: e.matmul(ps[bnk][:, 0:256], lhsT=VS[slot][:, jj, dk * 128:(dk + 1) * 128], rhs=wt[:, jj, :],
                                                                              start=(jj == 0), stop=(jj == 7)), reads=[('V', slot, jj), ('Wt', it % 2, jj)], writes=[pk(bnk)])
                S.op('dve', lambda e, dk=dk, bnk=bnk: e.scalar_tensor_tensor(out=xT[:, dk, tsl], in0=ps[bnk][:, 0:256], scalar=modv[:, l, 40 + dk:41 + dk], in1=xT[:, dk, tsl],
                                                                             op0=ALU.mult, op1=ALU.add), reads=[pk(bnk), xk(dk, tg // 2), ('modv', l)], writes=[xk(dk, tg // 2)])
            if Sx + 1 < NS and tg < 8:
                load_uv(Sx + 1, tg)
            if Sx + 1 < NS and NG < 8 and tg == NG - 1:
                for jj in range(NG, 8):
                    load_uv(Sx + 1, jj)
        S.barrier()


def prep_inputs(inp, T, L, b):
    f = np.float32

    def fm(v):
        return np.ascontiguousarray(v.reshape(v.shape[:-1] + (8, 128)).swapaxes(-1, -2))

    m = {}
    m['x'] = np.ascontiguousarray(inp['x'][b, :T])
    m['c_fm'] = fm(inp['c'][b])
    m['rel_bias'] = np.ascontiguousarray(inp['rel_bias'].reshape(1, 128))
    m['w_mod'] = inp['w_mod'][:L]
    bm = inp['b_mod'][:L].reshape(L, 6, 8, 128)
    m['b_mod_fm'] = np.ascontiguousarray(bm.transpose(0, 3, 1, 2).reshape(L, 128, 48))
    m['n1g_fm'] = fm(inp['norm1_g'][:L])
    m['n2g_fm'] = fm(inp['norm2_g'][:L])
    m['fing_fm'] = fm(inp['final_g'])
    m['w_in'] = inp['w_in'][:L]
    m['w_out'] = inp['w_out'][:L]
    m['diff_lambda'] = np.ascontiguousarray(inp['diff_lambda'][:L].reshape(L, 128))
    m['subln_g'] = inp['subln_g'][:L]
    cd = inp['conf_dw'][:L].reshape(L, 31, 2, 128)
    m['conf_dw_fm'] = np.ascontiguousarray(cd.transpose(0, 3, 2, 1).reshape(L, 128, 62))
    m['conf_lng_fm'] = np.ascontiguousarray(inp['conf_ln_g'][:L].reshape(L, 2, 128).transpose(0, 2, 1))
    m['conf_lnb_fm'] = np.ascontiguousarray(inp['conf_ln_b'][:L].reshape(L, 2, 128).transpose(0, 2, 1))
    sc = inp['sconv_w'][:L].reshape(L, 3, 2, 128)
    m['sconv_fm'] = np.ascontiguousarray(sc.transpose(0, 3, 2, 1).reshape(L, 128, 6))
    m['sgu_ln_g'] = inp['sgu_ln_g'][:L]
    m['sgu_ln_b'] = inp['sgu_ln_b'][:L]
    m['sgu_wT'] = np.ascontiguousarray(inp['sgu_w'][:L].transpose(0, 3, 1, 2).reshape(L, 128, 512))
    m['sgu_b'] = np.ascontiguousarray(inp['sgu_b'][:L].reshape(L, 512))
    m['peer_wq'] = inp['peer_wq'][:L]
    kk = inp['peer_keys'][:L].reshape(L, 16, 128, 128)
    m['peer_keysT'] = np.ascontiguousarray(kk.transpose(0, 3, 1, 2).reshape(L, 128, 2048))
    return m


_CONST = {}


def consts():
    if _CONST:
        return _CONST
    f = np.float32
    c = {}
    c['ident'] = np.eye(128, dtype=f)
    s = np.arange(128)
    c['trilT'] = (s[:, None] <= s[None, :]).astype(f)
    p = np.arange(128)
    c['maskq'] = np.stack([np.where((p % 64) < 32, 32 ** -0.5, 0.0), np.where((p % 64) >= 32, 32 ** -0.5, 0.0)], axis=1).astype(f)
    c['iota'] = np.broadcast_to(np.arange(128, dtype=f)[None, :], (128, 128)).copy()
    kj = np.arange(128)[:, None, None]
    qi = np.arange(128)[None, None, :]
    slot = np.arange(2)[None, :, None]
    n = (1 - slot) * 128 + qi - kj
    nn = np.maximum(n, 0)
    ratio = np.log(np.maximum(nn, 1).astype(np.float32) / 16) / np.float32(math.log(128 / 16))
    large = np.minimum(16 + (ratio * 16).astype(np.int32), 31)
    bucket = np.where(nn < 16, nn, large)
    c['bk'] = bucket.astype(f).reshape(128, 256)
    c['mk'] = np.where(n >= 0, 0.0, NEG).astype(f).reshape(128, 256)
    _CONST.update(c)
    return c


_SHARED = {}


def kernel(**inputs):
    T, L = 2048, 4
    inp = {k: np.asarray(v) for k, v in inputs.items()}
    nc = build(T, L)
    cst = consts()
    in_maps = []
    shared = None
    for b in range(8):
        m = prep_inputs(inp, T, L, b) if shared is None else None
        if shared is None:
            shared = {k: v for k, v in m.items() if k not in ('x', 'c_fm')}
            shared.update(cst)
            shared.update(prep_peer_tables(inp, L))
        mm = dict(shared)
        mm['x'] = np.ascontiguousarray(inp['x'][b, :T])
        mm['c_fm'] = np.ascontiguousarray(inp['c'][b].reshape(8, 128).T)
        in_maps.append(mm)
    res = run_bass_kernel_spmd(nc, in_maps, core_ids=list(range(8)))
    return np.stack([r['out'] for r in res.results], axis=0).astype(np.float32)


def prep_peer_tables(inp, L):
    u = inp['peer_u'][:L].reshape(L, 128, 128, 8, 128)
    v = inp['peer_v'][:L].reshape(L, 128, 128, 1024)
    return {
        'peer_uT': np.ascontiguousarray(u.transpose(0, 2, 4, 3, 1).reshape(L, 128, 128, 1024)),
        'peer_vr': np.ascontiguousarray(v.transpose(0, 2, 1, 3)),
    }
```
